# Optimizing a Trainium2 kernel written in Bass

```python
import jax, jax.numpy as jnp
from jax import lax
import numpy as np

D_MODEL = 1024
BATCH = 8
SEQ = 2048
DEPTH = 2

GRID_W = 64
CTX_LEN = 256
HEAD_DIM = 64
N_Q_HEADS = (D_MODEL // 2) // HEAD_DIM
N_KV_HEADS = 2
Q_PER_KV = N_Q_HEADS // N_KV_HEADS
Q_DIM = N_Q_HEADS * HEAD_DIM
KV_DIM = N_KV_HEADS * HEAD_DIM
WINDOW = 128
ATT_BLOCK = 128
ROPE_THETA = 10000.0
ROPE_PAIRS_PER_AXIS = HEAD_DIM // 4
CONV_CH = D_MODEL // 2
CONV_WIDTH = 31
EVEN_IN = 2 * CONV_CH + Q_DIM + 2 * KV_DIM
EVEN_MIX = CONV_CH + Q_DIM
KV_COL = 2 * CONV_CH + Q_DIM
CHUNK = 128
GMLP_CH = D_MODEL
GMLP_GROUPS = 8
GMLP_GROUP_CH = GMLP_CH // GMLP_GROUPS
N_EXPERTS = 32
TOP_K = 4
EXPERT_FF = D_MODEL
SWIGLU_LIMIT = 7.0
SWIGLU_ALPHA = 1.702
MOE_BLOCK = 128

EPS = 1e-6
NEG_INF = -1e30

kernel_name = "hybrid_conv_swa_gmlp_moe_dit"


def rms_norm(x, g):
    xf = x.astype(jnp.float32)
    y = xf * lax.rsqrt(jnp.mean(xf * xf, axis=-1, keepdims=True) + EPS)
    return (y * g).astype(x.dtype)


def layer_norm(x, g, b):
    xf = x.astype(jnp.float32)
    mu = jnp.mean(xf, axis=-1, keepdims=True)
    var = jnp.mean(jnp.square(xf - mu), axis=-1, keepdims=True)
    return ((xf - mu) * lax.rsqrt(var + EPS) * g + b).astype(x.dtype)


def ada_modulation(cvec, w, b):
    return jnp.split(jax.nn.silu(cvec) @ w + b, 6, axis=-1)


def modulate(x, g, shift, scale):
    return rms_norm(x, g) * (1.0 + scale) + shift


def axial_rope(rows):
    r = jnp.repeat(jnp.arange(rows), GRID_W).astype(jnp.float32)
    col = jnp.tile(jnp.arange(GRID_W), rows).astype(jnp.float32)
    inv_freq = ROPE_THETA ** (-jnp.arange(ROPE_PAIRS_PER_AXIS, dtype=jnp.float32) / ROPE_PAIRS_PER_AXIS)
    ang = jnp.concatenate([r[:, None] * inv_freq, col[:, None] * inv_freq], axis=-1)
    return jnp.cos(ang), jnp.sin(ang)


def apply_rope(x, cos, sin):
    xf = x.astype(jnp.float32)
    x1, x2 = jnp.split(xf, 2, axis=-1)
    c, s = cos[None, :, None, :], sin[None, :, None, :]
    return jnp.concatenate([x1 * c - x2 * s, x2 * c + x1 * s], axis=-1).astype(x.dtype)


def sink_softmax(logits, sink_logit):
    sink_b = jnp.broadcast_to(sink_logit, logits.shape[:-1] + (1,))
    p = jax.nn.softmax(jnp.concatenate([logits, sink_b], axis=-1), axis=-1)
    return p[..., :-1]


def conformer_conv(a_in, conv_w, conv_b, ln_g, ln_b):
    u, gate = jnp.split(a_in, 2, axis=-1)
    u = u * jax.nn.sigmoid(gate)
    u = lax.conv_general_dilated(
        u, conv_w[:, None, :], window_strides=(1,),
        padding=[(CONV_WIDTH // 2, CONV_WIDTH // 2)],
        dimension_numbers=('NWC', 'WIO', 'NWC'),
        feature_group_count=CONV_CH) + conv_b
    return jax.nn.silu(layer_norm(u, ln_g, ln_b))


def windowed_attention(q, k, v, k_c, v_c, sink):
    bsz, n = q.shape[0], q.shape[1]
    nb = n // ATT_BLOCK
    scale = HEAD_DIM ** -0.5
    qb = q.reshape(bsz, nb, ATT_BLOCK, N_KV_HEADS, Q_PER_KV, HEAD_DIM)

    def band(t):
        tp = jnp.pad(t, ((0, 0), (ATT_BLOCK, ATT_BLOCK), (0, 0), (0, 0)))
        tp = tp.reshape(bsz, nb + 2, ATT_BLOCK, N_KV_HEADS, HEAD_DIM)
        return jnp.concatenate([tp[:, :-2], tp[:, 1:-1], tp[:, 2:]], axis=2)

    kw, vw = band(k), band(v)
    s_win = jnp.einsum('bnqkgd,bnpkd->bnkgqp', qb, kw).astype(jnp.float32) * scale
    s_ctx = jnp.einsum('bnqkgd,bpkd->bnkgqp', qb, k_c).astype(jnp.float32) * scale
    qi = jnp.arange(ATT_BLOCK)[:, None]
    kp = jnp.arange(3 * ATT_BLOCK)[None, :]
    kpos = jnp.arange(nb)[:, None, None] * ATT_BLOCK - ATT_BLOCK + kp
    valid = (jnp.abs(kp - ATT_BLOCK - qi) <= WINDOW)[None] & (kpos >= 0) & (kpos < n)
    s_win = jnp.where(valid[None, :, None, None], s_win, NEG_INF)
    sink_l = sink.reshape(N_KV_HEADS, Q_PER_KV, 1, 1).astype(jnp.float32)
    p = sink_softmax(jnp.concatenate([s_win, s_ctx], axis=-1), sink_l).astype(v.dtype)
    p_win, p_ctx = p[..., :3 * ATT_BLOCK], p[..., 3 * ATT_BLOCK:]
    out = (jnp.einsum('bnkgqp,bnpkd->bnqkgd', p_win, vw)
           + jnp.einsum('bnkgqp,bpkd->bnqkgd', p_ctx, v_c))
    return out.reshape(bsz, n, Q_DIM)


def context_attention(q_c, k_c, v_c, sink):
    bsz, nl = q_c.shape[0], q_c.shape[1]
    qg = q_c.reshape(bsz, nl, N_KV_HEADS, Q_PER_KV, HEAD_DIM)
    s = jnp.einsum('bqkgd,bpkd->bkgqp', qg, k_c).astype(jnp.float32) * (HEAD_DIM ** -0.5)
    p = sink_softmax(s, sink.reshape(N_KV_HEADS, Q_PER_KV, 1, 1).astype(jnp.float32)).astype(v_c.dtype)
    return jnp.einsum('bkgqp,bpkd->bqkgd', p, v_c).reshape(bsz, nl, Q_DIM)


def even_mixer(hm, gm, w_in, b_in, conv_w, conv_b, ln_g, ln_b, qn_g, kn_g, sink, w_out, b_out,
               cos, sin, ctx_out):
    bsz, n, _ = hm.shape
    splits = [2 * CONV_CH, KV_COL, KV_COL + KV_DIM]
    a_in, q, k, v = jnp.split(hm @ w_in + b_in, splits, axis=-1)
    conv_h = conformer_conv(a_in, conv_w, conv_b, ln_g, ln_b)
    q = apply_rope(rms_norm(q.reshape(bsz, n, N_Q_HEADS, HEAD_DIM), qn_g), cos, sin)
    k = apply_rope(rms_norm(k.reshape(bsz, n, N_KV_HEADS, HEAD_DIM), kn_g), cos, sin)
    v = v.reshape(bsz, n, N_KV_HEADS, HEAD_DIM)
    nl = gm.shape[1]
    if ctx_out:
        a_c, q_c, k_c, v_c = jnp.split(gm @ w_in + b_in, splits, axis=-1)
    else:
        k_c, v_c = jnp.split(gm @ w_in[:, KV_COL:] + b_in[KV_COL:], 2, axis=-1)
    k_c = rms_norm(k_c.reshape(bsz, nl, N_KV_HEADS, HEAD_DIM), kn_g)
    v_c = v_c.reshape(bsz, nl, N_KV_HEADS, HEAD_DIM)
    att_h = windowed_attention(q, k, v, k_c, v_c, sink)
    out_h = jnp.concatenate([conv_h, att_h], axis=-1) @ w_out + b_out
    if not ctx_out:
        return out_h, None
    q_c = rms_norm(q_c.reshape(bsz, nl, N_Q_HEADS, HEAD_DIM), qn_g)
    att_c = context_attention(q_c, k_c, v_c, sink)
    conv_c = conformer_conv(a_c, conv_w, conv_b, ln_g, ln_b)
    out_c = jnp.concatenate([conv_c, att_c], axis=-1) @ w_out + b_out
    return out_h, out_c


def chunk_gmlp(xm, w_in, b_in, ln_g, ln_b, w_s, b_s, w_out, b_out):
    bsz, n, _ = xm.shape
    z = jax.nn.gelu(xm @ w_in + b_in, approximate=False)
    u, v = jnp.split(z, 2, axis=-1)
    v = layer_norm(v, ln_g, ln_b).reshape(bsz, n // CHUNK, CHUNK, GMLP_GROUPS, GMLP_GROUP_CH)
    sv = jnp.einsum('gpq,bnqgc->bnpgc', w_s, v) + b_s.T[:, :, None]
    return (u * sv.reshape(bsz, n, GMLP_CH)) @ w_out + b_out


def moe_ffn(xf, r_w, r_b, w1, b1, w2, b2):
    n, d = xf.shape
    logits = (xf @ r_w + r_b).astype(jnp.float32)
    top_val, top_idx = lax.top_k(logits, TOP_K)
    gates = jax.nn.softmax(top_val, axis=-1)
    n_assign = n * TOP_K
    flat_e = top_idx.reshape(-1)
    order = jnp.argsort(flat_e)
    e_sorted = flat_e[order]
    tok_sorted = order // TOP_K
    counts = jnp.zeros((N_EXPERTS,), jnp.int32).at[flat_e].add(1)
    padded = (counts + MOE_BLOCK - 1) // MOE_BLOCK * MOE_BLOCK
    start = jnp.cumsum(counts) - counts
    pend = jnp.cumsum(padded)
    pstart = pend - padded
    dest = pstart[e_sorted] + jnp.arange(n_assign) - start[e_sorted]
    n_blocks = (n_assign + N_EXPERTS * (MOE_BLOCK - 1) + MOE_BLOCK - 1) // MOE_BLOCK
    rows = jnp.zeros((n_blocks * MOE_BLOCK, d), xf.dtype).at[dest].set(xf[tok_sorted])
    block_expert = jnp.minimum(
        jnp.searchsorted(pend, jnp.arange(n_blocks) * MOE_BLOCK, side='right'), N_EXPERTS - 1)

    def expert_block(args):
        xb, e = args
        gate, up = jnp.split(xb @ w1[e] + b1[e], 2, axis=-1)
        gate = jnp.minimum(gate, SWIGLU_LIMIT)
        up = jnp.clip(up, -SWIGLU_LIMIT, SWIGLU_LIMIT)
        act = (up + 1.0) * gate * jax.nn.sigmoid(gate * SWIGLU_ALPHA)
        return act @ w2[e] + b2[e]

    y_rows = lax.map(expert_block, (rows.reshape(n_blocks, MOE_BLOCK, d), block_expert)).reshape(-1, d)
    y_assign = y_rows[dest] * gates.reshape(-1)[order][:, None].astype(xf.dtype)
    return jax.ops.segment_sum(y_assign, tok_sorted, num_segments=n)


def setup_inputs(seed: int = 0) -> dict:
    key = jax.random.key(seed)
    ks = iter(jax.random.split(key, 40))
    ne, no = (DEPTH + 1) // 2, DEPTH // 2
    d = D_MODEL

    def nrm(shape, scale):
        return jax.random.normal(next(ks), shape, jnp.float32) * scale

    return {
        'x': nrm((BATCH, SEQ, d), 1.0),
        'c': nrm((BATCH, d), 1.0),
        'ctx': nrm((BATCH, CTX_LEN, d), 1.0),
        'c_ctx': nrm((d,), 1.0),
        'ada_w': nrm((DEPTH, d, 6 * d), 0.5 * d ** -0.5),
        'ada_b': nrm((DEPTH, 6 * d), 0.02),
        'norm_mix_g': 1.0 + nrm((DEPTH, d), 0.1),
        'norm_ffn_g': 1.0 + nrm((DEPTH, d), 0.1),
        'ev_w_in': nrm((ne, d, EVEN_IN), d ** -0.5),
        'ev_b_in': nrm((ne, EVEN_IN), 0.02),
        'ev_conv_w': nrm((ne, CONV_WIDTH, CONV_CH), CONV_WIDTH ** -0.5),
        'ev_conv_b': nrm((ne, CONV_CH), 0.02),
        'ev_conv_ln_g': 1.0 + nrm((ne, CONV_CH), 0.1),
        'ev_conv_ln_b': nrm((ne, CONV_CH), 0.02),
        'ev_q_norm_g': 1.0 + nrm((ne, HEAD_DIM), 0.1),
        'ev_k_norm_g': 1.0 + nrm((ne, HEAD_DIM), 0.1),
        'ev_sink': nrm((ne, N_Q_HEADS), 0.5),
        'ev_w_out': nrm((ne, EVEN_MIX, d), EVEN_MIX ** -0.5),
        'ev_b_out': nrm((ne, d), 0.02),
        'od_w_in': nrm((no, d, 2 * GMLP_CH), d ** -0.5),
        'od_b_in': nrm((no, 2 * GMLP_CH), 0.02),
        'od_v_ln_g': 1.0 + nrm((no, GMLP_CH), 0.1),
        'od_v_ln_b': nrm((no, GMLP_CH), 0.02),
        'od_w_s': nrm((no, GMLP_GROUPS, CHUNK, CHUNK), CHUNK ** -0.5),
        'od_b_s': 1.0 + nrm((no, GMLP_GROUPS, CHUNK), 0.1),
        'od_w_out': nrm((no, GMLP_CH, d), GMLP_CH ** -0.5),
        'od_b_out': nrm((no, d), 0.02),
        'moe_router_w': nrm((DEPTH, d, N_EXPERTS), d ** -0.5),
        'moe_router_b': nrm((DEPTH, N_EXPERTS), 0.01),
        'moe_w1': nrm((DEPTH, N_EXPERTS, d, 2 * EXPERT_FF), d ** -0.5),
        'moe_b1': nrm((DEPTH, N_EXPERTS, 2 * EXPERT_FF), 0.02),
        'moe_w2': nrm((DEPTH, N_EXPERTS, EXPERT_FF, d), EXPERT_FF ** -0.5),
        'moe_b2': nrm((DEPTH, N_EXPERTS, d), 0.02),
    }


def reference(x, c, ctx, c_ctx, ada_w, ada_b, norm_mix_g, norm_ffn_g,
              ev_w_in, ev_b_in, ev_conv_w, ev_conv_b, ev_conv_ln_g, ev_conv_ln_b,
              ev_q_norm_g, ev_k_norm_g, ev_sink, ev_w_out, ev_b_out,
              od_w_in, od_b_in, od_v_ln_g, od_v_ln_b, od_w_s, od_b_s, od_w_out, od_b_out,
              moe_router_w, moe_router_b, moe_w1, moe_b1, moe_w2, moe_b2):
    n_lat = x.shape[1]
    rows = n_lat // GRID_W
    cos, sin = axial_rope(rows)
    h, g = x, ctx
    for l in range(DEPTH):
        i = l // 2
        is_even = l % 2 == 0
        ctx_read_later = any(j % 2 == 0 for j in range(l + 1, DEPTH))
        sh_m, sc_m, gt_m, sh_f, sc_f, gt_f = [m[:, None, :] for m in ada_modulation(c, ada_w[l], ada_b[l])]
        hm = modulate(h, norm_mix_g[l], sh_m, sc_m)
        if is_even or ctx_read_later:
            csh_m, csc_m, cgt_m, csh_f, csc_f, cgt_f = ada_modulation(c_ctx, ada_w[l], ada_b[l])
            gm = modulate(g, norm_mix_g[l], csh_m, csc_m)
        if is_even:
            mix_h, mix_g = even_mixer(hm, gm, ev_w_in[i], ev_b_in[i], ev_conv_w[i], ev_conv_b[i],
                                      ev_conv_ln_g[i], ev_conv_ln_b[i], ev_q_norm_g[i], ev_k_norm_g[i],
                                      ev_sink[i], ev_w_out[i], ev_b_out[i], cos, sin, ctx_read_later)
        else:
            gmlp_args = (od_w_in[i], od_b_in[i], od_v_ln_g[i], od_v_ln_b[i], od_w_s[i], od_b_s[i],
                         od_w_out[i], od_b_out[i])
            mix_h = chunk_gmlp(hm, *gmlp_args)
            mix_g = chunk_gmlp(gm, *gmlp_args) if ctx_read_later else None
        h = h + gt_m * mix_h
        moe_args = (moe_router_w[l], moe_router_b[l], moe_w1[l], moe_b1[l], moe_w2[l], moe_b2[l])
        hf = modulate(h, norm_ffn_g[l], sh_f, sc_f)
        if ctx_read_later:
            g = g + cgt_m * mix_g
            gf = modulate(g, norm_ffn_g[l], csh_f, csc_f)
            tokens = jnp.concatenate([gf, hf], axis=1)
            y = moe_ffn(tokens.reshape(-1, D_MODEL), *moe_args).reshape(tokens.shape)
            g = g + cgt_f * y[:, :g.shape[1]]
            h = h + gt_f * y[:, g.shape[1]:]
        else:
            h = h + gt_f * moe_ffn(hf.reshape(-1, D_MODEL), *moe_args).reshape(h.shape)
    return h
```

```python
import numpy as np
import ml_dtypes
import concourse.bass as bass
import concourse.mybir as mybir
from concourse.bass_utils import run_bass_kernel_spmd
from contextlib import ExitStack

F32, BF16 = mybir.dt.float32, mybir.dt.bfloat16
AF = mybir.ActivationFunctionType
ALU = mybir.AluOpType
AX = mybir.AxisListType

D = 1024; S = 2048; L = 256; NE = 32; EPS = 1e-6
DEBUG = {}

VOFF = {}
def _mk_voff():
    o = 0
    for name, n in [('sc_in', 16), ('ada_b', 96), ('nmg', 16), ('nfg', 16), ('ev_bin_a', 8),
                    ('conv_w', 124), ('conv_b', 4), ('cln_g', 4), ('cln_b', 4), ('ev_bout', 8),
                    ('od_bin_u', 8), ('od_bout', 8)]:
        VOFF[name] = o; o += n
    return o
NV = _mk_voff()
NCONST = 128 + 256
NCS = 1024
NROW0 = 768 + 64 + 64 + 8
NROW1 = 4096


class Res:
    __slots__ = ('name', 'w', 'r')
    def __init__(self, name=''):
        self.name = name; self.w = {}; self.r = {}

class DSem:
    def __init__(self, h):
        self.h = h; self.count = 0

class Ins:
    __slots__ = ('eng', 'fn', 'deps', 'seq', 'need', 'dsem', 'dord', 'phase')

class Prog:
    CE = ('act', 'dve', 'pool', 'pe')
    def __init__(self, nc, es):
        self.nc = nc
        self.esem = {e: es.enter_context(nc.semaphore('s_' + e)) for e in self.CE}
        self.seq = {e: 0 for e in self.CE}
        self.waited = {x: {} for x in self.CE + ('sp',)}
        self.dsems = []
        self.es = es
        self.cur = {x: [] for x in self.CE + ('sp',)}
        self.phase = 0
        self.bar = None
        self.n_ins = 0
        self.dpool = {}; self.dpi = {}

    def dsem(self, name):
        s = DSem(self.es.enter_context(self.nc.semaphore(name)))
        self.dsems.append(s)
        return s

    def op(self, eng, fn, reads=(), writes=(), dsem=None):
        I = Ins(); I.eng = eng; I.fn = fn; I.seq = 0; I.need = False; I.dsem = dsem; I.phase = self.phase
        key = dsem if dsem is not None else eng
        if dsem is not None:
            dsem.count += 1; I.dord = dsem.count
        raw = set()
        deps = set()
        for r in reads:
            for d in r.w.values():
                deps.add(d); raw.add(d)
        for w in writes:
            for d in w.w.values(): deps.add(d)
            for d in w.r.values(): deps.add(d)
        out = []
        for d in deps:
            if d.phase != self.phase: continue
            if d.dsem is None and d.eng == eng and dsem is None:
                if eng == 'pe': continue
            out.append(d)
            if d.dsem is None: d.need = True
        I.deps = out
        for r in reads: r.r[key] = I
        for w in writes: w.w[key] = I
        self.cur[eng].append(I)
        self.n_ins += 1
        return I

    def dma(self, eng, out, in_, dsem=None, reads=(), writes=()):
        auto = dsem is None or getattr(dsem, 'auto', True)
        if auto:
            pool = self.dpool.setdefault(eng, [])
            if not pool:
                for i in range(12 if eng == 'sp' else 8):
                    d = self.dsem(f"dp_{eng}{i}"); d.last = None; pool.append(d)
                self.dpi[eng] = 0
            dsem = pool[self.dpi[eng] % len(pool)]; self.dpi[eng] += 1
        I = self.op(eng, lambda e, o=out, i=in_: e.dma_start(out=o, in_=i), reads, writes, dsem=dsem)
        if auto:
            if dsem.last is not None and dsem.last.phase == self.phase and dsem.last not in I.deps:
                I.deps.append(dsem.last)
            dsem.last = I
        return I

    def flush(self):
        nc = self.nc
        for e in self.CE:
            for I in reversed(self.cur[e]):
                if I.dsem is None:
                    I.need = True; break
        for e in self.CE:
            for I in self.cur[e]:
                if I.need and I.dsem is None:
                    self.seq[e] += 1; I.seq = self.seq[e]
        bar_prev = self.bar
        with nc.Block() as block:
            def emit(X, engobj):
                wt = self.waited[X]
                def wait(key, sem, val):
                    if wt.get(key, 0) < val:
                        engobj.wait_ge(sem, val); wt[key] = val
                if bar_prev is not None:
                    for e, v in bar_prev['seq'].items():
                        if v > 0: wait(e, self.esem[e], v)
                    for s, c in bar_prev['dma']:
                        if c > 0: wait(s, s.h, 16 * c)
                for I in self.cur[X]:
                    for d in I.deps:
                        if d.dsem is not None: wait(d.dsem, d.dsem.h, 16 * d.dord)
                        else: wait(d.eng, self.esem[d.eng], d.seq)
                    bi = I.fn(engobj)
                    if I.dsem is not None: bi.then_inc(I.dsem.h, 16)
                    elif I.need: bi.then_inc(self.esem[X], 1)
            @block.sync
            def _(e): emit('sp', e)
            @block.scalar
            def _(e): emit('act', e)
            @block.vector
            def _(e): emit('dve', e)
            @block.gpsimd
            def _(e): emit('pool', e)
            @block.tensor
            def _(e): emit('pe', e)
        self.bar = {'seq': dict(self.seq), 'dma': [(s, s.count) for s in self.dsems]}
        self.cur = {x: [] for x in self.CE + ('sp',)}
        self.phase += 1

    def final_wait(self):
        nc = self.nc
        bar = self.bar
        with nc.Block() as block:
            @block.sync
            def _(e):
                for en, v in bar['seq'].items():
                    if v > 0: e.wait_ge(self.esem[en], v)
                for s, c in bar['dma']:
                    if c > 0: e.wait_ge(s.h, 16 * c)


class Ring:
    def __init__(self, items):
        self.items = items; self.i = 0
    def next(self):
        it = self.items[self.i % len(self.items)]; self.i += 1
        return it


def build_program(dbg=None):
    dbg = dbg or {}
    nc = bass.Bass("TRN2", target_bir_lowering=False)
    dt_in = lambda name, shape, dt=F32: nc.dram_tensor(name, list(shape), dt, kind="ExternalInput").ap()
    xT = dt_in("xT", [D, S]); ctxT = dt_in("ctxT", [D, L])
    vecs_d = dt_in("vecs", [128, NV]); consts_d = dt_in("consts", [128, NCONST]); cs_d = dt_in("cossin", [128, NCS])
    b1c_d = dt_in("b1cols", [2, 128, 512])
    rows0_d = dt_in("rows0", [1, NROW0]); rows1_d = dt_in("rows1", [1, NROW1]); rowsm_d = dt_in("rowsm", [1, 64])
    ada_w = dt_in("ada_w", [2, D, 6 * D])
    ev_w_in = dt_in("ev_w_in", [D, 1792]); ev_w_out = dt_in("ev_w_out", [D, D])
    od_w_in = dt_in("od_w_in", [D, 2 * D]); od_w_s = dt_in("od_w_s", [8, 128, 128]); od_w_out = dt_in("od_w_out", [D, D])
    r_w = dt_in("r_w", [2, D, NE]); w1 = dt_in("w1", [2, NE, D, 2 * D]); w2 = dt_in("w2", [2, NE, D, D])
    b2 = dt_in("b2", [2, NE, D])
    outT = nc.dram_tensor("outT", [D, S], F32, kind="ExternalOutput").ap()
    gt_scr = nc.dram_tensor("gt_scr", [2, NE, S], F32, kind="Internal").ap()
    mk_scr = nc.dram_tensor("mk_scr", [2, NE, S], BF16, kind="Internal").ap()
    dbg_out = {}
    for k, shp in dbg.items():
        dbg_out[k] = nc.dram_tensor("dbg_" + k, list(shp), F32, kind="ExternalOutput").ap()

    es = ExitStack()
    with es:
        P = Prog(nc, es)
        _sbn = [0]
        def SB(name, shape, dt=F32, st=es):
            _sbn[0] += 1
            return st.enter_context(nc.sbuf_tensor(f"sb{_sbn[0]}_{name}", list(shape), dt))
        hT = SB("hT", [128, 8, S]); R_h = [[Res() for _ in range(4)] for _ in range(8)]
        vecs = SB("vecs", [128, NV]); R_vecs = Res()
        consts = SB("consts", [128, 128]); R_consts = Res()
        mod = SB("mod", [128, 2, 48, 2]); R_mod = Res()
        der = SB("der", [128, 2, 5, 8, 2]); R_der = Res()
        ones_bf = SB("ones_bf", [128, 128], BF16); ones_f = SB("ones_f", [128, 128]); ident_bf = SB("ident_bf", [128, 128], BF16)
        maskb = SB("maskb", [128, 256], BF16)
        R_k = Res()
        out_st = P.dsem("out_st"); out_st.auto = False; dbg_st = P.dsem("dbg_st"); dbg_st.auto = False
        cs_ld = None; wA = None; wB = None
        ident_f = consts[:, 0:128]
        def V(name, i=0, n=1): return vecs[:, VOFF[name] + i: VOFF[name] + i + n]

        def dump(name, ap, res_list):
            if name in dbg_out:
                P.dma('sp', dbg_out[name], ap, dbg_st, reads=res_list)

        scg = SB("scg", [128, 16], F32); R_sc = Res()
        def ada_layer(l, ph, nstage, width, lazy=False):
            PS = [ph.enter_context(nc.psum_tensor(f"L{P.phase}_adaps{l}_{i}", [128, 512], F32)) for i in range(2)]
            R_ps = [Res() for _ in range(2)]
            PST = ph.enter_context(nc.psum_tensor(f"L{P.phase}_adapst{l}", [128, 512], F32)); R_pst0 = Res()
            stage = [SB(f"adast{l}_{i}", [128, 8, width], F32, ph) for i in range(nstage)]
            R_st = [Res() for _ in range(nstage)]
            modrow = SB(f"modrow{l}", [2, 6 * D], F32, ph); R_mr = Res()
            items = []
            def chunk(cb):
                si = cb % nstage; pi = cb % 2
                P.dma('sp', stage[si][:], ada_w[l].rearrange("(k p) n -> p k n", p=128)[:, :, cb * width:(cb + 1) * width],
                      None, writes=[R_st[si]])
                for k in range(8):
                    P.op('pe', lambda e, k=k: e.matmul(
                        PS[pi][0:2, 0:width], scg[:, 2 * k:2 * k + 2], stage[si][:, k, :], start=(k == 0), stop=(k == 7)),
                        reads=[R_st[si], R_sc], writes=[R_ps[pi]])
                P.op('act', lambda e: e.copy(out=modrow[:, cb * width:(cb + 1) * width], in_=PS[pi][0:2, 0:width]),
                     reads=[R_ps[pi]], writes=[R_mr])
            for cb in range(6 * D // width):
                items.append(lambda cb=cb: chunk(cb))
            items.append(lambda: ada_finish(l, PST, R_pst0, modrow, R_mr))
            if lazy:
                return items
            for it in items: it()

        def ada_finish(l, PST, R_pst0, modrow, R_mr):
            for j in range(48):
                P.op('pe', lambda e, j=j: e.transpose(out=PST[:, 2 * j:2 * j + 2], in_=modrow[:, j * 128:(j + 1) * 128], identity=ident_f[0:2, 0:2]),
                     reads=[R_mr, R_consts], writes=[R_pst0])
            P.op('dve', lambda e: e.tensor_tensor(
                mod[:, l, :, :], PST[:, 0:96].rearrange("p (j s) -> p j s", s=2),
                V('ada_b', l * 48, 48).unsqueeze(2).to_broadcast([128, 48, 2]), op=ALU.add),
                reads=[R_pst0, R_vecs], writes=[R_mod])
            P.op('dve', lambda e: e.scalar_tensor_tensor(
                out=der[:, l, 0], in0=mod[:, l, 8:16, :], scalar=1.0,
                in1=V('nmg', l * 8, 8).unsqueeze(2).to_broadcast([128, 8, 2]), op0=ALU.add, op1=ALU.mult),
                reads=[R_mod, R_vecs], writes=[R_der])
            P.op('dve', lambda e: e.scalar_tensor_tensor(
                out=der[:, l, 1], in0=mod[:, l, 32:40, :], scalar=1.0,
                in1=V('nfg', l * 8, 8).unsqueeze(2).to_broadcast([128, 8, 2]), op0=ALU.add, op1=ALU.mult),
                reads=[R_mod, R_vecs], writes=[R_der])
            bo = 'ev_bout' if l == 0 else 'od_bout'
            P.op('dve', lambda e: e.tensor_tensor(
                der[:, l, 2], mod[:, l, 16:24, :], V(bo, 0, 8).unsqueeze(2).to_broadcast([128, 8, 2]), op=ALU.mult),
                reads=[R_mod, R_vecs], writes=[R_der])

        with ExitStack() as ph:
            P.dma('sp', vecs[:], vecs_d[:, :], cs_ld, writes=[R_vecs])
            P.dma('sp', consts[:], consts_d[:, 0:128], cs_ld, writes=[R_consts])
            mtmp = SB("mtmp", [128, 256], F32, ph); R_mtmp = Res()
            P.dma('sp', mtmp[:], consts_d[:, 128:384], cs_ld, writes=[R_mtmp])
            for k in range(8):
                P.dma('sp', hT[:, k, :], xT[k * 128:(k + 1) * 128, :], cs_ld, writes=R_h[k])
            P.op('dve', lambda e: e.memset(ones_f[:], 1.0), writes=[R_k])
            P.op('dve', lambda e: e.memset(ones_bf[:], 1.0), writes=[R_k])
            P.op('dve', lambda e: e.tensor_copy(ident_bf[:], ident_f), reads=[R_consts], writes=[R_k])
            P.op('dve', lambda e: e.tensor_copy(maskb[:], mtmp[:]), reads=[R_mtmp], writes=[R_k])
            P.op('act', lambda e: e.activation(out=scg[:], in_=V('sc_in', 0, 16), func=AF.Silu), reads=[R_vecs], writes=[R_sc])
            ada_layer(0, ph, 3, 512)
            dump('mod', mod[:].rearrange("p l j s -> p (l j s)"), [R_mod])
            P.flush()

        def norm_mod(ph_tiles, src_fn, n_tok, A_fn, sh_fn, dst_fn, R_src, R_dst, ps_ss, R_ps_ss, tmp, also32=None):
            sq, R_sq = tmp['sq'], tmp['R_sq']
            for k in range(8):
                s_i = tmp['sqi'][0] % len(sq); tmp['sqi'][0] += 1
                P.op('act', lambda e, k=k, s_i=s_i: e.activation(out=sq[s_i][:, 0:n_tok], in_=src_fn(k), func=AF.Square),
                     reads=[R_src[k]], writes=[R_sq[s_i]])
                P.op('pe', lambda e, k=k, s_i=s_i: e.matmul(ps_ss[:, 0:n_tok], ones_bf[:], sq[s_i][:, 0:n_tok], start=(k == 0), stop=(k == 7)),
                     reads=[R_sq[s_i], R_k], writes=[R_ps_ss])
            rs, R_rs = tmp['rs'], tmp['R_rs']
            P.op('act', lambda e: e.activation(out=rs[:, 0:n_tok], in_=ps_ss[:, 0:n_tok], func=AF.Sqrt, scale=1.0 / D, bias=tmp['eps'][:, 0:1]),
                 reads=[R_ps_ss], writes=[R_rs])
            P.op('dve', lambda e: e.reciprocal(rs[:, 0:n_tok], rs[:, 0:n_tok]), reads=[R_rs], writes=[R_rs])
            for k in range(8):
                t_i = tmp['ti'][0] % len(tmp['t']); tmp['ti'][0] += 1
                tt_, R_tt = tmp['t'][t_i], tmp['R_t'][t_i]
                P.op('dve', lambda e, k=k, tt_=tt_: e.tensor_tensor(tt_[:, 0:n_tok], src_fn(k), rs[:, 0:n_tok], op=ALU.mult),
                     reads=[R_src[k], R_rs], writes=[R_tt])
                if also32 is None:
                    P.op('act', lambda e, k=k, tt_=tt_: e.activation(out=dst_fn(k), in_=tt_[:, 0:n_tok], func=AF.Identity, scale=A_fn(k), bias=sh_fn(k)),
                         reads=[R_tt, R_der, R_mod], writes=[R_dst[k]])
                else:
                    d32, R_d32 = also32
                    P.op('act', lambda e, k=k, tt_=tt_: e.activation(out=d32(k), in_=tt_[:, 0:n_tok], func=AF.Identity, scale=A_fn(k), bias=sh_fn(k)),
                         reads=[R_tt, R_der, R_mod], writes=[R_d32[k]])
                    P.op('pool', lambda e, k=k: e.tensor_copy(dst_fn(k), d32(k)), reads=[R_d32[k]], writes=[R_dst[k]])

        def mk_norm_tmp(ph, pfx):
            t = {'sq': [SB(f"{pfx}sq{i}", [128, 512], BF16, ph) for i in range(3)], 'R_sq': [Res() for _ in range(3)], 'sqi': [0],
                 'rs': SB(f"{pfx}rs", [128, 512], F32, ph), 'R_rs': Res(),
                 't': [SB(f"{pfx}t{i}", [128, 512], F32, ph) for i in range(2)], 'R_t': [Res() for _ in range(2)], 'ti': [0],
                 'eps': SB(f"{pfx}eps", [128, 1], F32, ph)}
            P.op('dve', lambda e: e.memset(t['eps'][:], EPS), writes=[t['R_rs']])
            return t

        def moe_phase(l):
            gtf = lambda m: mod[:, l, 40 + m, 0:1]
            with ExitStack() as pm:
                hf = SB("hf", [128, 8, S], BF16, pm); R_hf = [[Res() for _ in range(4)] for _ in range(8)]
                with ExitStack() as ph:
                    hf32 = SB("hf32", [128, 8, 512], F32, ph); R_hf32 = [Res() for _ in range(8)]
                    GT = SB("GT", [NE, S], F32, ph); R_GT = [Res() for _ in range(4)]
                    GTs = SB("GTs", [NE, S], F32, ph); R_GTs = Res()
                    rw = SB("rw", [128, 8, NE], F32, ph); R_rw = Res()
                    B2 = SB("B2", [NE, D], F32, ph); R_B2 = Res()
                    rb = SB("rb", [128, NE], F32, ph); R_rb = Res()
                    lg = SB("lg", [128, 16, NE], F32, ph); R_lg = [Res() for _ in range(16)]
                    Gt = SB("Gt", [128, 16, NE], F32, ph)
                    sm = SB("sm", [128, 16, 16], F32, ph)
                    ntmp = mk_norm_tmp(ph, f"m{l}")
                    PSx = ph.enter_context(nc.psum_tensor(f"L{P.phase}_psx", [128, 512], F32)); R_psx = Res()
                    PSy = [ph.enter_context(nc.psum_tensor(f"L{P.phase}_psy{i}", [128, 512], F32)) for i in range(2)]; R_psy = [Res() for _ in range(2)]
                    PSo = [ph.enter_context(nc.psum_tensor(f"L{P.phase}_m1pso{i}", [128, 512], F32)) for i in range(2)]; R_pso = [Res() for _ in range(2)]
                    P.dma('sp', rw[:], r_w[l].rearrange("(k p) n -> p k n", p=128), cs_ld, writes=[R_rw])
                    P.dma('sp', B2[:], b2[l], cs_ld, writes=[R_B2])
                    P.dma('sp', rb[:], rowsm_d[0:1, l * 32:(l + 1) * 32].partition_broadcast(128), cs_ld, writes=[R_rb])
                    ada_items = ada_layer(1, ph, 2, 256, lazy=True) if l == 0 else []
                    for tt in range(4):
                        tok = slice(tt * 512, (tt + 1) * 512)
                        norm_mod(ph, lambda k, tok=tok: hT[:, k, tok], 512,
                                 lambda k: der[:, l, 1, k, 0:1], lambda k: mod[:, l, 24 + k, 0:1],
                                 lambda k, tok=tok: hf[:, k, tok], [R_h[k][tt] for k in range(8)], [R_hf[k][tt] for k in range(8)],
                                 PSx, R_psx, ntmp, also32=(lambda k: hf32[:, k, :], R_hf32))
                        for sub in range(4):
                            i = tt * 4 + sub; yi = i % 2
                            for k in range(8):
                                P.op('pe', lambda e, k=k, sub=sub, yi=yi: e.matmul(PSy[yi][:, 0:NE], hf32[:, k, sub * 128:(sub + 1) * 128], rw[:, k, :],
                                                                                  start=(k == 0), stop=(k == 7)),
                                     reads=[R_hf32[k], R_rw], writes=[R_psy[yi]])
                            P.op('dve', lambda e, i=i, yi=yi: e.tensor_tensor(lg[:, i, :], PSy[yi][:, 0:NE], rb[:], op=ALU.add),
                                 reads=[R_psy[yi], R_rb], writes=[R_lg[i]])
                            P.op('dve', lambda e, i=i: e.max(out=sm[:, i, 0:8], in_=lg[:, i, :]), reads=[R_lg[i]], writes=[R_lg[i]])
                            P.op('dve', lambda e, i=i: e.tensor_scalar(sm[:, i, 8:9], sm[:, i, 0:1], -1.0, None, op0=ALU.mult),
                                 reads=[R_lg[i]], writes=[R_lg[i]])
                            P.op('act', lambda e, i=i: e.activation(out=Gt[:, i, :], in_=lg[:, i, :], func=AF.Exp, bias=sm[:, i, 8:9], scale=1.0),
                                 reads=[R_lg[i]], writes=[R_lg[i]])
                            P.op('dve', lambda e, i=i: e.scalar_tensor_tensor(out=Gt[:, i, :], in0=lg[:, i, :], scalar=sm[:, i, 3:4], in1=Gt[:, i, :],
                                                                                op0=ALU.is_ge, op1=ALU.mult),
                                 reads=[R_lg[i]], writes=[R_lg[i]])
                            P.op('dve', lambda e, i=i: e.reduce_sum(out=sm[:, i, 9:10], in_=Gt[:, i, :], axis=AX.X), reads=[R_lg[i]], writes=[R_lg[i]])
                            P.op('dve', lambda e, i=i: e.reciprocal(sm[:, i, 10:11], sm[:, i, 9:10]), reads=[R_lg[i]], writes=[R_lg[i]])
                            P.op('dve', lambda e, i=i: e.tensor_scalar(Gt[:, i, :], Gt[:, i, :], sm[:, i, 10:11], None, op0=ALU.mult),
                                 reads=[R_lg[i]], writes=[R_lg[i]])
                            P.op('pe', lambda e, i=i, yi=yi: e.transpose(out=PSy[yi][0:NE, 128:256], in_=Gt[:, i, :], identity=ident_f),
                                 reads=[R_lg[i], R_consts], writes=[R_psy[yi]])
                            P.op('act', lambda e, i=i, yi=yi: e.copy(out=GT[:, i * 128:(i + 1) * 128], in_=PSy[yi][0:NE, 128:256]),
                                 reads=[R_psy[yi]], writes=[R_GT[tt]])
                            if ada_items: ada_items.pop(0)()
                    dump(f'hf{l}', hf[:], sum(R_hf, []))
                    dump(f'G{l}', Gt[:].rearrange("p i e -> p (i e)"), R_lg)
                    P.op('dve', lambda e: e.tensor_scalar(GTs[:], GT[:], 1.0 / 1.702, None, op0=ALU.mult), reads=R_GT, writes=[R_GTs])
                    P.dma('sp', gt_scr[l], GTs[:], reads=[R_GTs])
                    GTm = SB("GTm", [NE, S], BF16, ph); R_GTm = Res()
                    P.op('dve', lambda e: e.tensor_scalar(GTm[:], GT[:], 0.0, None, op0=ALU.is_gt), reads=R_GT, writes=[R_GTm])
                    P.dma('sp', mk_scr[l], GTm[:], reads=[R_GTm])
                    while ada_items: ada_items.pop(0)()
                    for tt in range(4):
                        tok = slice(tt * 512, (tt + 1) * 512)
                        for m in range(8):
                            oi = (tt * 8 + m) % 2
                            P.op('pe', lambda e, m=m, oi=oi, tok=tok: e.matmul(PSo[oi][:], B2[:, m * 128:(m + 1) * 128], GT[:, tok], start=True, stop=True),
                                 reads=[R_B2, R_GT[tt]], writes=[R_pso[oi]])
                            P.op('dve', lambda e, m=m, oi=oi, tok=tok: e.scalar_tensor_tensor(out=hT[:, m, tok], in0=PSo[oi][:], scalar=gtf(m), in1=hT[:, m, tok],
                                                                                               op0=ALU.mult, op1=ALU.add),
                                 reads=[R_pso[oi], R_mod, R_h[m][tt]], writes=[R_h[m][tt]])
                    P.flush()
                with ExitStack() as ph:
                    act = SB("actT", [128, 8, S], BF16, ph); R_act = [[Res() for _ in range(4)] for _ in range(8)]
                    W1s = [SB(f"W1s{i}", [128, 8, 2, 512], BF16, ph) for i in range(2)]; R_W1 = [[Res(), Res()] for _ in range(2)]
                    W2q = [SB(f"W2q{i}", [128, 8, 256], BF16, ph) for i in range(3)]; R_W2 = [Res() for _ in range(3)]
                    nq = [0]
                    w1sem = [None] * 2; w2sem = [None] * 2
                    hfm = [SB(f"hfm{i}", [128, 8, 512], BF16, ph) for i in range(2)]; R_hfm = [[Res() for _ in range(8)] for _ in range(2)]
                    maskt = SB("maskt", [128, 512], BF16, ph); R_maskt = Res()
                    sg = [SB(f"sg_{i}", [128, 512], F32, ph) for i in range(2)]; R_sg = [Res() for _ in range(2)]
                    u0 = [SB(f"u0_{i}", [128, 512], F32, ph) for i in range(2)]; R_u0 = [Res() for _ in range(2)]
                    b1e = [SB(f"b1e{i}", [128, 16], F32, ph) for i in range(2)]; R_b1e = [Res() for _ in range(2)]
                    PSg = [ph.enter_context(nc.psum_tensor(f"L{P.phase}_psg{i}", [128, 512], F32)) for i in range(2)]; R_psg = [Res() for _ in range(2)]
                    PSu = [ph.enter_context(nc.psum_tensor(f"L{P.phase}_psu{i}", [128, 512], F32)) for i in range(2)]; R_psu = [Res() for _ in range(2)]
                    PSo = [ph.enter_context(nc.psum_tensor(f"L{P.phase}_pso{i}", [128, 512], F32)) for i in range(4)]; R_pso = [Res() for _ in range(4)]

                    def load_w1(e, half):
                        for two in range(2):
                            src = w1[l, e].rearrange("(k p) (two h j) -> p k two h j", p=128, two=2, h=2)[:, :, two, half, :]
                            P.dma('pool', W1s[half][:, :, two, :], src, w1sem[half], writes=[R_W1[half][two]])
                    def load_w2q(e, q):
                        slot = (e * 4 + q) % 3
                        src = w2[l, e].rearrange("(k p) (q j) -> p k q j", p=128, q=4)[:, :, q, :]
                        P.dma('pool', W2q[slot][:], src, None, writes=[R_W2[slot]])

                    def load_b1(e):
                        eb_ = e % 2
                        P.dma('sp', b1e[eb_][:], b1c_d[l][:, e * 16:(e + 1) * 16], writes=[R_b1e[eb_]])
                        P.op('dve', lambda e_: e_.tensor_scalar(b1e[eb_][:, 8:16], b1e[eb_][:, 8:16], 1.0, None, op0=ALU.add), reads=[R_b1e[eb_]], writes=[R_b1e[eb_]])
                    load_b1(0)
                    load_w1(0, 0); load_w1(0, 1)
                    cnt = {'g': 0, 'o': 0, 'b': 0}
                    n_exp = DEBUG.get('n_exp', NE)
                    Gb = [SB(f"Gb{i}", [128, 512], F32, ph) for i in range(2)]; R_Gb = [Res() for _ in range(2)]
                    pend = [None]
                    def flush_tail():
                        if pend[0] is not None:
                            pend[0](); pend[0] = None
                    groups = [(e_, half, tt) for e_ in range(n_exp) for half in range(2) for tt in range(4)]
                    def prep(n):
                        e_, half, tt = groups[n]; r = n % 2
                        tok = slice(tt * 512, (tt + 1) * 512)
                        P.dma('sp', maskt[:], mk_scr[l, e_:e_ + 1, tok].partition_broadcast(128), writes=[R_maskt])
                        return [lambda k=k: P.op('dve', lambda e: e.tensor_tensor(hfm[r][:, k, :], hf[:, k, tok], maskt[:], op=ALU.mult),
                                                 reads=[R_hf[k][tt], R_maskt], writes=[R_hfm[r][k]]) for k in range(8)]
                    for it in prep(0): it()
                    for n_, (e_, half, tt) in enumerate(groups):
                        if half == 0 and tt == 0:
                            load_w2q(e_, 0); load_w2q(e_, 1); load_w2q(e_, 2)
                            if e_ + 1 < n_exp: load_b1(e_ + 1)
                        pitems = prep(n_ + 1) if n_ + 1 < len(groups) else []
                        hr = n_ % 2
                        if True:
                            if True:
                                tok = slice(tt * 512, (tt + 1) * 512)
                                gi = cnt['b'] % 2; cnt['b'] += 1
                                P.dma('sp', Gb[gi][:], gt_scr[l, e_:e_ + 1, tok].partition_broadcast(128), writes=[R_Gb[gi]])
                                for cc in range(4):
                                    c = half * 4 + cc
                                    pi = cnt['g'] % 2; cnt['g'] += 1
                                    for it in pitems[2 * cc:2 * cc + 2]: it()
                                    for k in range(8):
                                        P.op('pe', lambda e, k=k, cc=cc, pi=pi, half=half, hr=hr: e.matmul(
                                            PSg[pi][:], W1s[half][:, k, 0, cc * 128:(cc + 1) * 128], hfm[hr][:, k, :], start=(k == 0), stop=(k == 7)),
                                            reads=[R_W1[half][0], R_hfm[hr][k]], writes=[R_psg[pi]])
                                    for k in range(8):
                                        P.op('pe', lambda e, k=k, cc=cc, pi=pi, half=half, hr=hr: e.matmul(
                                            PSu[pi][:], W1s[half][:, k, 1, cc * 128:(cc + 1) * 128], hfm[hr][:, k, :], start=(k == 0), stop=(k == 7)),
                                            reads=[R_W1[half][1], R_hfm[hr][k]], writes=[R_psu[pi]])
                                    eb = e_ % 2
                                    bg = b1e[eb][:, c:c + 1]
                                    bu = b1e[eb][:, 8 + c:9 + c]
                                    P.op('dve', lambda e, pi=pi, bg=bg: e.tensor_scalar(sg[pi][:], PSg[pi][:], bg, 7.0, op0=ALU.add, op1=ALU.min),
                                         reads=[R_psg[pi], R_b1e[eb]], writes=[R_sg[pi]])
                                    P.op('act', lambda e, pi=pi: e.activation(out=sg[pi][:], in_=sg[pi][:], func=AF.Silu, scale=1.702),
                                         reads=[R_sg[pi]], writes=[R_sg[pi]])
                                    P.op('act', lambda e, pi=pi, bu=bu: e.activation(out=u0[pi][:], in_=PSu[pi][:], func=AF.Identity, bias=bu, scale=1.0),
                                         reads=[R_psu[pi], R_b1e[eb]], writes=[R_u0[pi]])
                                    P.op('dve', lambda e, pi=pi: e.tensor_scalar(u0[pi][:], u0[pi][:], -6.0, 8.0, op0=ALU.max, op1=ALU.min),
                                         reads=[R_u0[pi]], writes=[R_u0[pi]])
                                    flush_tail()
                                    def tail(pi=pi, gi=gi, c=c, tok=tok, tt=tt):
                                        P.op('dve', lambda e: e.tensor_tensor(u0[pi][:], u0[pi][:], sg[pi][:], op=ALU.mult),
                                             reads=[R_sg[pi], R_u0[pi]], writes=[R_u0[pi]])
                                        P.op('dve', lambda e: e.tensor_tensor(act[:, c, tok], u0[pi][:], Gb[gi][:], op=ALU.mult),
                                             reads=[R_u0[pi], R_Gb[gi]], writes=[R_act[c][tt]])
                                    pend[0] = tail
                        if tt != 3: continue
                        if half == 1: flush_tail()
                        if e_ + 1 < n_exp:
                            load_w1(e_ + 1, half)
                        if half != 1: continue
                        for q in range(4):
                            slot = (e_ * 4 + q) % 3
                            for tt2 in range(4):
                                tok = slice(tt2 * 512, (tt2 + 1) * 512)
                                for mm_ in range(2):
                                    m = q * 2 + mm_
                                    oi = cnt['o'] % 4; cnt['o'] += 1
                                    for k in range(8):
                                        P.op('pe', lambda e, k=k, mm_=mm_, oi=oi, slot=slot, tok=tok: e.matmul(
                                            PSo[oi][:], W2q[slot][:, k, mm_ * 128:(mm_ + 1) * 128], act[:, k, tok], start=(k == 0), stop=(k == 7)),
                                            reads=[R_W2[slot], R_act[k][tt2]], writes=[R_pso[oi]])
                                    P.op('dve', lambda e, m=m, oi=oi, tok=tok: e.scalar_tensor_tensor(out=hT[:, m, tok], in0=PSo[oi][:], scalar=gtf(m), in1=hT[:, m, tok],
                                                                                                       op0=ALU.mult, op1=ALU.add),
                                         reads=[R_pso[oi], R_mod, R_h[m][tt2]], writes=[R_h[m][tt2]])
                            if q == 0: load_w2q(e_, 3)
                    dump(f'h_moe{l}', hT[:], sum(R_h, []))
                    P.flush()

        def mixer0():
            with ExitStack() as pa:
                u_bf = SB("u_bf", [128, 4, S + 30], BF16, pa); R_u = [[Res() for _ in range(4)] for _ in range(4)]; R_upad = Res()
                qT = SB("qT", [128, 16, 512], BF16, pa); R_qT = [Res() for _ in range(16)]
                kT = SB("kT", [128, S + L], BF16, pa); R_kT = [Res() for _ in range(18)]
                vtok = SB("vtok", [128, 18, 128], BF16, pa); R_v = [Res() for _ in range(18)]
                rows0 = SB("rows0", [128, NROW0], F32, pa); R_rows0 = Res()
                es_t = SB("es_t", [128, 4], F32, pa); R_es = Res()
                with ExitStack() as ph:
                    hm = [SB(f"hm{i}", [128, 8, 512], BF16, ph) for i in range(2)]; R_hm = [[Res() for _ in range(8)] for _ in range(2)]
                    cT = SB("cT", [128, 8, L], F32, ph); R_cT = [Res() for _ in range(8)]
                    w_in = SB("w_in", [128, 8, 1792], BF16, ph); R_win = Res()
                    cs_t = SB("cs_t", [128, NCS], F32, ph); R_cs = Res()
                    G10 = SB("G10", [128, 10, 64], F32, ph); R_G10 = Res()
                    cos_t = cs_t[:, 0:512].rearrange("p (i f) -> p i f", f=32)
                    sin_t = cs_t[:, 512:1024].rearrange("p (i f) -> p i f", f=32)
                    ntmp = mk_norm_tmp(ph, "a1")
                    qkv = SB("qkv", [128, 768], F32, ph); R_qkv = Res()
                    sqq = SB("sqq", [128, 640], F32, ph); R_sqq = Res()
                    st10 = SB("st10", [128, 16], F32, ph); R_st10 = Res()
                    qn = SB("qn", [128, 10, 64], F32, ph); R_qn = Res()
                    ra = SB("ra", [128, 10, 32], F32, ph); R_ra = Res()
                    rbb = SB("rbb", [128, 10, 32], F32, ph); R_rbb = Res()
                    qr = SB("qr", [128, 10, 64], F32, ph); R_qr = Res()
                    sgl = [SB(f"sgl{i}", [128, 512], F32, ph) for i in range(2)]; R_sgl = [Res() for _ in range(2)]
                    PSs = ph.enter_context(nc.psum_tensor(f"L{P.phase}_a1pss", [128, 512], F32)); R_pss = Res()
                    PSa = [ph.enter_context(nc.psum_tensor(f"L{P.phase}_a1psa{i}", [128, 512], F32)) for i in range(4)]; R_psa = [Res() for _ in range(4)]
                    PSq = ph.enter_context(nc.psum_tensor(f"L{P.phase}_a1psq", [128, 512], F32)); R_psq = Res()
                    PSk = ph.enter_context(nc.psum_tensor(f"L{P.phase}_a1psk", [128, 512], F32)); R_psk = Res()
                    PSt = ph.enter_context(nc.psum_tensor(f"L{P.phase}_a1pst", [128, 512], F32)); R_pst = Res()
                    for k in range(8):
                        P.dma('sp', cT[:, k, :], ctxT[k * 128:(k + 1) * 128, :], cs_ld, writes=[R_cT[k]])
                    P.dma('sp', rows0[:], rows0_d[0:1, :].partition_broadcast(128), cs_ld, writes=[R_rows0])
                    P.dma('sp', cs_t[:], cs_d[:, :], cs_ld, writes=[R_cs])
                    P.dma('pool', w_in[:], ev_w_in.rearrange("(k p) n -> p k n", p=128), wA, writes=[R_win])
                    P.op('dve', lambda e: e.memset(u_bf[:, :, 0:15], 0.0), writes=[R_upad])
                    P.op('dve', lambda e: e.memset(u_bf[:, :, S + 15:S + 30], 0.0), writes=[R_upad])
                    P.op('dve', lambda e: e.tensor_copy(G10[:, 0:8, :], rows0[:, 768:832].unsqueeze(1).to_broadcast([128, 8, 64])), reads=[R_rows0], writes=[R_G10])
                    P.op('dve', lambda e: e.tensor_copy(G10[:, 8:10, :], rows0[:, 832:896].unsqueeze(1).to_broadcast([128, 2, 64])), reads=[R_rows0], writes=[R_G10])
                    P.op('act', lambda e: e.activation(out=es_t[0:64, :], in_=rows0[0:64, 896:900], func=AF.Exp), reads=[R_rows0], writes=[R_es])
                    P.op('act', lambda e: e.activation(out=es_t[64:128, :], in_=rows0[64:128, 900:904], func=AF.Exp), reads=[R_rows0], writes=[R_es])
                    na = 0
                    A1C = DEBUG.get('a1', 99)
                    for tt in range(5 if A1C >= 9 else (1 if A1C >= 1 else 0)):
                        hb = tt % 2
                        hmt = hm[hb]
                        if tt < 4:
                            tok = slice(tt * 512, (tt + 1) * 512)
                            norm_mod(ph, lambda k, tok=tok: hT[:, k, tok], 512, lambda k: der[:, 0, 0, k, 0:1], lambda k: mod[:, 0, k, 0:1],
                                     lambda k, hmt=hmt: hmt[:, k, :], [R_h[k][tt] for k in range(8)], R_hm[hb], PSs, R_pss, ntmp)
                        else:
                            norm_mod(ph, lambda k: cT[:, k, :], L, lambda k: der[:, 0, 0, k, 1:2], lambda k: mod[:, 0, k, 1:2],
                                     lambda k, hmt=hmt: hmt[:, k, 0:L], R_cT, R_hm[hb], PSs, R_pss, ntmp)
                        if tt == 0: dump('hm0', hmt[:], R_hm[hb])
                        if A1C == 1: break
                        if tt < 4:
                            for c in range(4):
                                pu = na % 4; pg = (na + 1) % 4; si = (na // 2) % 2; na += 2
                                for k in range(8):
                                    P.op('pe', lambda e, k=k, c=c, pu=pu, hmt=hmt: e.matmul(PSa[pu][:], w_in[:, k, c * 128:(c + 1) * 128], hmt[:, k, :], start=(k == 0), stop=(k == 7)),
                                         reads=[R_win, R_hm[hb][k]], writes=[R_psa[pu]])
                                for k in range(8):
                                    P.op('pe', lambda e, k=k, c=c, pg=pg, hmt=hmt: e.matmul(PSa[pg][:], w_in[:, k, 512 + c * 128:512 + (c + 1) * 128], hmt[:, k, :], start=(k == 0), stop=(k == 7)),
                                         reads=[R_win, R_hm[hb][k]], writes=[R_psa[pg]])
                                P.op('act', lambda e, c=c, pg=pg, si=si: e.activation(out=sgl[si][:], in_=PSa[pg][:], func=AF.Sigmoid, bias=V('ev_bin_a', 4 + c, 1), scale=1.0),
                                     reads=[R_psa[pg], R_vecs], writes=[R_sgl[si]])
                                P.op('dve', lambda e, c=c, pu=pu, si=si, tt=tt: e.scalar_tensor_tensor(
                                    out=u_bf[:, c, 15 + tt * 512:15 + (tt + 1) * 512], in0=PSa[pu][:], scalar=V('ev_bin_a', c, 1), in1=sgl[si][:], op0=ALU.add, op1=ALU.mult),
                                    reads=[R_psa[pu], R_sgl[si], R_vecs], writes=[R_u[c][tt]])
                        nsub = 4 if tt < 4 else 2
                        if A1C == 2: break
                        if A1C < 9: nsub = 1
                        for sub in range(nsub):
                            i = tt * 4 + sub
                            tokc = slice(i * 128, (i + 1) * 128)
                            lc = slice(sub * 128, (sub + 1) * 128)
                            main = tt < 4
                            if main:
                                for k in range(8):
                                    P.op('pe', lambda e, k=k, lc=lc, hmt=hmt: e.matmul(PSq[:], hmt[:, k, lc], w_in[:, k, 1024:1536], start=(k == 0), stop=(k == 7)),
                                         reads=[R_win, R_hm[hb][k]], writes=[R_psq])
                            for k in range(8):
                                P.op('pe', lambda e, k=k, lc=lc, hmt=hmt: e.matmul(PSk[:, 0:256], hmt[:, k, lc], w_in[:, k, 1536:1792], start=(k == 0), stop=(k == 7)),
                                     reads=[R_win, R_hm[hb][k]], writes=[R_psk])
                            if main:
                                P.op('dve', lambda e: e.tensor_tensor(qkv[:, 0:512], PSq[:], rows0[:, 0:512], op=ALU.add),
                                     reads=[R_psq, R_rows0], writes=[R_qkv])
                            P.op('dve', lambda e: e.tensor_tensor(qkv[:, 512:768], PSk[:, 0:256], rows0[:, 512:768], op=ALU.add),
                                 reads=[R_psk, R_rows0], writes=[R_qkv])
                            P.op('act', lambda e, i=i: e.copy(out=vtok[:, i, :], in_=qkv[:, 640:768]), reads=[R_qkv], writes=[R_v[i]])
                            h0 = 0 if main else 8
                            nh = 10 - h0
                            c0 = h0 * 64
                            P.op('pool', lambda e, c0=c0: e.tensor_tensor(sqq[:, c0:640], qkv[:, c0:640], qkv[:, c0:640], op=ALU.mult),
                                 reads=[R_qkv], writes=[R_sqq])
                            P.op('dve', lambda e, h0=h0, c0=c0: e.tensor_reduce(out=st10[:, h0:10], in_=sqq[:, c0:640].rearrange("p (h d) -> p h d", d=64),
                                                                                 axis=AX.X, op=ALU.add),
                                 reads=[R_sqq], writes=[R_st10])
                            P.op('act', lambda e, h0=h0: e.activation(out=st10[:, h0:10], in_=st10[:, h0:10], func=AF.Sqrt, scale=1.0 / 64, bias=ntmp['eps'][:, 0:1]),
                                 reads=[R_st10], writes=[R_st10])
                            P.op('dve', lambda e, h0=h0: e.reciprocal(st10[:, h0:10], st10[:, h0:10]), reads=[R_st10], writes=[R_st10])
                            P.op('dve', lambda e, h0=h0, nh=nh, c0=c0: e.tensor_tensor(
                                qn[:, h0:10, :], qkv[:, c0:640].rearrange("p (h d) -> p h d", d=64),
                                st10[:, h0:10].unsqueeze(2).to_broadcast([128, nh, 64]), op=ALU.mult),
                                reads=[R_qkv, R_st10], writes=[R_qn])
                            if A1C == 3: break
                            if main:
                                P.op('pool', lambda e: e.tensor_tensor(qn[:], qn[:], G10[:], op=ALU.mult), reads=[R_qn, R_G10], writes=[R_qn])
                                cb_ = lambda i=i: cos_t[:, i, :].unsqueeze(1).to_broadcast([128, 10, 32])
                                sb_ = lambda i=i: sin_t[:, i, :].unsqueeze(1).to_broadcast([128, 10, 32])
                                P.op('dve', lambda e, cb_=cb_: e.tensor_tensor(ra[:], qn[:, :, 0:32], cb_(), op=ALU.mult), reads=[R_qn, R_cs], writes=[R_ra])
                                P.op('pool', lambda e, sb_=sb_: e.tensor_tensor(rbb[:], qn[:, :, 32:64], sb_(), op=ALU.mult), reads=[R_qn, R_cs], writes=[R_rbb])
                                qperm = lambda lo, hi: qr[:, 0:8, lo:hi].rearrange("p (g kv) d -> p kv g d", kv=2)
                                v4 = lambda t: t[:, 0:8, :].rearrange("p (kv g) d -> p kv g d", kv=2)
                                P.op('dve', lambda e, qperm=qperm, v4=v4: e.tensor_tensor(qperm(0, 32), v4(ra), v4(rbb), op=ALU.subtract), reads=[R_ra, R_rbb], writes=[R_qr])
                                P.op('dve', lambda e: e.tensor_tensor(qr[:, 8:10, 0:32], ra[:, 8:10, :], rbb[:, 8:10, :], op=ALU.subtract), reads=[R_ra, R_rbb], writes=[R_qr])
                                P.op('dve', lambda e, cb_=cb_: e.tensor_tensor(ra[:], qn[:, :, 32:64], cb_(), op=ALU.mult), reads=[R_qn, R_cs], writes=[R_ra])
                                P.op('pool', lambda e, sb_=sb_: e.tensor_tensor(rbb[:], qn[:, :, 0:32], sb_(), op=ALU.mult), reads=[R_qn, R_cs], writes=[R_rbb])
                                P.op('dve', lambda e, qperm=qperm, v4=v4: e.tensor_tensor(qperm(32, 64), v4(ra), v4(rbb), op=ALU.add), reads=[R_ra, R_rbb], writes=[R_qr])
                                P.op('dve', lambda e: e.tensor_tensor(qr[:, 8:10, 32:64], ra[:, 8:10, :], rbb[:, 8:10, :], op=ALU.add), reads=[R_ra, R_rbb], writes=[R_qr])
                                if A1C == 4: break
                                for g in range(4):
                                    P.op('pe', lambda e, g=g: e.transpose(
                                        out=PSt[:, g * 128:(g + 1) * 128],
                                        in_=qr[:, 2 * g:2 * g + 2, :], identity=ident_f),
                                        reads=[R_qr, R_consts], writes=[R_pst])
                                P.op('pe', lambda e: e.transpose(out=PSk[:, 384:512], in_=qr[:, 8:10, :], identity=ident_f),
                                     reads=[R_qr, R_consts], writes=[R_psk])
                                P.op('act', lambda e, i=i: e.copy(out=qT[:, i, :], in_=PSt[:, 0:512]),
                                     reads=[R_pst], writes=[R_qT[i]])
                                P.op('dve', lambda e, tokc=tokc: e.tensor_copy(kT[:, tokc], PSk[:, 384:512]), reads=[R_psk], writes=[R_kT[i]])
                            else:
                                P.op('pool', lambda e: e.tensor_tensor(qr[:, 8:10, :], qn[:, 8:10, :], G10[:, 8:10, :], op=ALU.mult),
                                     reads=[R_qn, R_G10], writes=[R_qr])
                                P.op('pe', lambda e: e.transpose(out=PSk[:, 384:512], in_=qr[:, 8:10, :], identity=ident_f),
                                     reads=[R_qr, R_consts], writes=[R_psk])
                                P.op('dve', lambda e, tokc=tokc: e.tensor_copy(kT[:, tokc], PSk[:, 384:512]), reads=[R_psk], writes=[R_kT[i]])
                    dump('u', u_bf[:], sum(R_u, []))
                    dump('qT', qT[:].rearrange('p i f -> p (i f)'), R_qT); dump('kT', kT[:], R_kT); dump('v', vtok[:], R_v)
                    P.flush()
                if DEBUG.get('sub', 9) <= 1: return
                with ExitStack() as pb:
                    chT = SB("chT", [128, 4, S], BF16, pb); R_ch = [[Res() for _ in range(4)] for _ in range(4)]
                    atT = SB("atT", [128, 4, S], BF16, pb); R_at = [Res() for _ in range(16)]
                    with ExitStack() as ph:
                        acc = SB("acc", [128, 4, 512], F32, ph); R_acc = [Res() for _ in range(4)]
                        sq4 = SB("sq4", [128, 4, 512], F32, ph); R_sq4 = [Res() for _ in range(4)]
                        mean = SB("mean", [128, 512], F32, ph); R_mean = Res()
                        var = SB("var", [128, 512], F32, ph); R_var = Res()
                        m2 = SB("m2", [128, 512], F32, ph); R_m2 = Res()
                        tl = [SB(f"tl{i}", [128, 512], F32, ph) for i in range(2)]; R_tl = [Res() for _ in range(2)]
                        epsb = SB("epsb", [128, 1], F32, ph); R_epsb = Res()
                        Eb = [SB(f"Eb{i}", [128, 512], BF16, ph) for i in range(6)]; R_Eb = [Res() for _ in range(6)]
                        den = SB("den", [128, 512], F32, ph); R_den = Res()
                        PSm = ph.enter_context(nc.psum_tensor(f"L{P.phase}_a2psm", [128, 512], F32)); R_psm = Res()
                        PSv = ph.enter_context(nc.psum_tensor(f"L{P.phase}_a2psv", [128, 512], F32)); R_psv = Res()
                        PSs2 = [ph.enter_context(nc.psum_tensor(f"L{P.phase}_a2pss{i}", [128, 512], F32)) for i in range(2)]; R_pss2 = [Res() for _ in range(2)]
                        PSo2 = [ph.enter_context(nc.psum_tensor(f"L{P.phase}_a2pso{i}", [128, 512], F32)) for i in range(2)]; R_pso2 = [Res() for _ in range(2)]
                        PSd2 = [ph.enter_context(nc.psum_tensor(f"L{P.phase}_a2psd{i}", [128, 512], F32)) for i in range(2)]; R_psd2 = [Res() for _ in range(2)]
                        P.op('dve', lambda e: e.memset(epsb[:], EPS), writes=[R_epsb])
                        jobs = []
                        for i in range(16):
                            blocks = [(jb, m_) for jb, m_ in ((i - 1, 0), (i, None), (i + 1, 1)) if 0 <= jb < 16] + [(16, None), (17, None)]
                            for kv in range(2):
                                for bi_, (jb, m_) in enumerate(blocks):
                                    jobs.append(dict(i=i, kv=kv, jb=jb, m=m_, first=(bi_ == 0), last=(bi_ == len(blocks) - 1), n=len(jobs)))
                        def job_scores(J):
                            i, kv, jb = J['i'], J['kv'], J['jb']; si = J['n'] % 2
                            pr = slice(kv * 64, (kv + 1) * 64)
                            P.op('pe', lambda e: e.matmul(PSs2[si][:], kT[pr, jb * 128:(jb + 1) * 128], qT[pr, i, :], start=True, stop=True),
                                 reads=[R_kT[jb], R_qT[i]], writes=[R_pss2[si]])
                        def job_rest(J):
                            i, kv, jb, m_ = J['i'], J['kv'], J['jb'], J['m']; si = J['n'] % 2; ei = J['n'] % 6; pi = i % 2
                            pr = slice(kv * 64, (kv + 1) * 64)
                            first, last = J['first'], J['last']
                            P.op('act', lambda e: e.activation(out=Eb[ei][:], in_=PSs2[si][:], func=AF.Exp, scale=0.125),
                                 reads=[R_pss2[si]], writes=[R_Eb[ei]])
                            if m_ is not None:
                                P.op('pool', lambda e: e.tensor_tensor(
                                    Eb[ei][:].rearrange("p (g t) -> p g t", g=4), Eb[ei][:].rearrange("p (g t) -> p g t", g=4),
                                    maskb[:, m_ * 128:(m_ + 1) * 128].unsqueeze(1).to_broadcast([128, 4, 128]), op=ALU.mult),
                                    reads=[R_Eb[ei], R_k], writes=[R_Eb[ei]])
                            P.op('pe', lambda e: e.matmul(PSo2[pi][pr, :], vtok[:, jb, kv * 64:(kv + 1) * 64], Eb[ei][:], start=first, stop=last),
                                 reads=[R_v[jb], R_Eb[ei]], writes=[R_pso2[pi]])
                            P.op('pe', lambda e: e.matmul(PSd2[pi][pr, :], ones_bf[:, 0:64], Eb[ei][:], start=first, stop=last),
                                 reads=[R_k, R_Eb[ei]], writes=[R_psd2[pi]])
                            if last and kv == 1:
                                tokq = slice(i * 128, (i + 1) * 128)
                                P.op('dve', lambda e: e.tensor_tensor(den[:].rearrange("p (g t) -> p g t", g=4), PSd2[pi][:].rearrange("p (g t) -> p g t", g=4),
                                                                       es_t[:].unsqueeze(2).to_broadcast([128, 4, 128]), op=ALU.add),
                                     reads=[R_psd2[pi], R_es], writes=[R_den])
                                P.op('dve', lambda e: e.reciprocal(den[:], den[:]), reads=[R_den], writes=[R_den])
                                P.op('dve', lambda e: e.tensor_tensor(atT[:, :, tokq], PSo2[pi][:].rearrange("p (g t) -> p g t", g=4),
                                                                      den[:].rearrange("p (g t) -> p g t", g=4), op=ALU.mult),
                                     reads=[R_pso2[pi], R_den], writes=[R_at[i]])
                                return True
                            return False

                        ptmp = SB("ptmp", [128, 512], F32, ph); R_ptmp = Res()
                        def conv_taps(tt):
                            ops = []
                            for tap in range(31):
                                for c in range(4):
                                    src = u_bf[:, c, tt * 512 + tap: tt * 512 + tap + 512]
                                    wcol = V('conv_w', c * 31 + tap, 1)
                                    rd = [R_u[c][t2] for t2 in range(max(0, tt - 1), min(4, tt + 2))] + [R_upad, R_vecs]
                                    eng = 'dve'
                                    if tap == 0:
                                        ops.append(lambda eng=eng, c=c, src=src, wcol=wcol, rd=rd: P.op(eng, lambda e: e.tensor_scalar(
                                            acc[:, c, :], src, wcol, V('conv_b', c, 1), op0=ALU.mult, op1=ALU.add),
                                            reads=rd, writes=[R_acc[c]]))
                                    elif True:
                                        ops.append(lambda c=c, src=src, wcol=wcol, rd=rd: P.op('dve', lambda e: e.scalar_tensor_tensor(
                                            out=acc[:, c, :], in0=src, scalar=wcol, in1=acc[:, c, :], op0=ALU.mult, op1=ALU.add),
                                            reads=rd + [R_acc[c]], writes=[R_acc[c]]))
                                    else:
                                        def two(c=c, src=src, wcol=wcol, rd=rd):
                                            P.op('pool', lambda e: e.tensor_scalar(ptmp[:], src, wcol, None, op0=ALU.mult), reads=rd, writes=[R_ptmp])
                                            P.op('pool', lambda e: e.tensor_tensor(acc[:, c, :], acc[:, c, :], ptmp[:], op=ALU.add), reads=[R_ptmp, R_acc[c]], writes=[R_acc[c]])
                                        ops.append(two)
                            return ops

                        def conv_tile(tt):
                            for c in range(4):
                                P.op('act', lambda e, c=c: e.activation(out=sq4[:, c, :], in_=acc[:, c, :], func=AF.Square),
                                     reads=[R_acc[c]], writes=[R_sq4[c]])
                            for c in range(4):
                                P.op('pe', lambda e, c=c: e.matmul(PSm[:], ones_f[:], acc[:, c, :], start=(c == 0), stop=(c == 3)),
                                     reads=[R_acc[c], R_k], writes=[R_psm])
                            for c in range(4):
                                P.op('pe', lambda e, c=c: e.matmul(PSv[:], ones_f[:], sq4[:, c, :], start=(c == 0), stop=(c == 3)),
                                     reads=[R_sq4[c], R_k], writes=[R_psv])
                            P.op('act', lambda e: e.activation(out=mean[:], in_=PSm[:], func=AF.Identity, scale=1.0 / 512), reads=[R_psm], writes=[R_mean])
                            P.op('pool', lambda e: e.tensor_tensor(m2[:], mean[:], mean[:], op=ALU.mult), reads=[R_mean], writes=[R_m2])
                            P.op('dve', lambda e: e.scalar_tensor_tensor(out=var[:], in0=PSv[:], scalar=1.0 / 512, in1=m2[:], op0=ALU.mult, op1=ALU.subtract),
                                 reads=[R_psv, R_m2], writes=[R_var])
                            P.op('act', lambda e: e.activation(out=var[:], in_=var[:], func=AF.Sqrt, bias=epsb[:, 0:1], scale=1.0), reads=[R_var, R_epsb], writes=[R_var])
                            P.op('dve', lambda e: e.reciprocal(var[:], var[:]), reads=[R_var], writes=[R_var])
                            for c in range(4):
                                ti = c % 2
                                P.op('dve', lambda e, c=c, ti=ti: e.tensor_tensor(tl[ti][:], acc[:, c, :], mean[:], op=ALU.subtract),
                                     reads=[R_acc[c], R_mean], writes=[R_tl[ti]])
                                P.op('pool', lambda e, ti=ti: e.tensor_tensor(tl[ti][:], tl[ti][:], var[:], op=ALU.mult), reads=[R_tl[ti], R_var], writes=[R_tl[ti]])
                                P.op('act', lambda e, c=c, ti=ti, tt=tt: e.activation(out=chT[:, c, tt * 512:(tt + 1) * 512], in_=tl[ti][:], func=AF.Silu,
                                                                                      scale=V('cln_g', c, 1), bias=V('cln_b', c, 1)),
                                     reads=[R_tl[ti], R_vecs], writes=[R_ch[c][tt]])
                        conv_q = []
                        for tt in range(4):
                            ops = conv_taps(tt)
                            per = (len(ops) + 3) // 4
                            for qi in range(4):
                                conv_q.append((tt, qi, ops[qi * per:(qi + 1) * per]))
                        def run_conv(item):
                            tt, qi, ops = item
                            for o in ops: o()
                            if qi == 3: conv_tile(tt)
                        job_scores(jobs[0])
                        done_tiles = 0
                        for n in range(len(jobs)):
                            if n + 1 < len(jobs): job_scores(jobs[n + 1])
                            if job_rest(jobs[n]):
                                run_conv(conv_q[done_tiles]); done_tiles += 1
                        dump('chT', chT[:], sum(R_ch, [])); dump('atT', atT[:], R_at)
                        P.flush()
                    if DEBUG.get('sub', 9) <= 2: return
                    with ExitStack() as ph:
                        w_out = SB("w_out", [128, 8, D], BF16, ph); R_wo = [Res() for _ in range(3)]
                        tmp = [SB(f"a3t{i}", [128, 512], F32, ph) for i in range(2)]; R_tmp = [Res() for _ in range(2)]
                        PSo3 = [ph.enter_context(nc.psum_tensor(f"L{P.phase}_a3ps{i}", [128, 512], F32)) for i in range(2)]; R_pso3 = [Res() for _ in range(2)]
                        P.dma('pool', w_out[:, 0:4, :], ev_w_out[0:512, :].rearrange("(k p) n -> p k n", p=128), wB, writes=[R_wo[0]])
                        P.dma('pool', w_out[0:64, 4:8, :], ev_w_out[512:768, :].rearrange("(g d) n -> d g n", d=64), wB, writes=[R_wo[1]])
                        P.dma('pool', w_out[64:128, 4:8, :], ev_w_out[768:1024, :].rearrange("(g d) n -> d g n", d=64), wB, writes=[R_wo[2]])
                        n3 = 0
                        for tt in range(4):
                            tok = slice(tt * 512, (tt + 1) * 512)
                            for m in range(8):
                                oi = n3 % 2; n3 += 1
                                for c in range(4):
                                    P.op('pe', lambda e, c=c, m=m, oi=oi, tok=tok: e.matmul(PSo3[oi][:], w_out[:, c, m * 128:(m + 1) * 128], chT[:, c, tok], start=(c == 0), stop=False),
                                         reads=R_wo + [R_ch[c][tt]], writes=[R_pso3[oi]])
                                for g in range(4):
                                    P.op('pe', lambda e, g=g, m=m, oi=oi, tok=tok: e.matmul(PSo3[oi][:], w_out[:, 4 + g, m * 128:(m + 1) * 128], atT[:, g, tok], start=False, stop=(g == 3)),
                                         reads=R_wo + [R_at[tt * 4 + q_] for q_ in range(4)], writes=[R_pso3[oi]])
                                P.op('act', lambda e, m=m, oi=oi: e.activation(out=tmp[oi][:], in_=PSo3[oi][:], func=AF.Identity, scale=mod[:, 0, 16 + m, 0:1], bias=der[:, 0, 2, m, 0:1]),
                                     reads=[R_pso3[oi], R_mod, R_der], writes=[R_tmp[oi]])
                                P.op('dve', lambda e, m=m, oi=oi, tok=tok: e.tensor_tensor(hT[:, m, tok], hT[:, m, tok], tmp[oi][:], op=ALU.add),
                                     reads=[R_tmp[oi], R_h[m][tt]], writes=[R_h[m][tt]])
                        dump('h_mix0', hT[:], sum(R_h, []))
                        P.flush()

        def mixer1():
            with ExitStack() as pa:
                uT = SB("uT", [128, 8, S], BF16, pa); R_uT = [[Res() for _ in range(4)] for _ in range(8)]
                vt = SB("vt", [128, 16, D], BF16, pa); R_vt = [Res() for _ in range(16)]
                wsT = SB("wsT", [128, 8, 128], BF16, pa); R_ws = Res()
                with ExitStack() as ph:
                    wsf = SB("wsf", [128, 8, 128], F32, ph); R_wsf = Res()
                    PSt = [ph.enter_context(nc.psum_tensor(f"L{P.phase}_b0pst{i}", [128, 512], F32)) for i in range(2)]; R_pst = [Res(), Res()]
                    P.dma('sp', wsf[:], od_w_s.rearrange("g p q -> p g q"), cs_ld, writes=[R_wsf])
                    for g in range(8):
                        P.op('pe', lambda e, g=g: e.transpose(out=PSt[g // 4][:, (g % 4) * 128:(g % 4 + 1) * 128], in_=wsf[:, g, :], identity=ident_f),
                             reads=[R_wsf, R_consts], writes=[R_pst[g // 4]])
                    for hh in range(2):
                        P.op('act', lambda e, hh=hh: e.copy(out=wsT[:, hh * 4:(hh + 1) * 4, :].rearrange("p g q -> p (g q)"), in_=PSt[hh][:]), reads=[R_pst[hh]], writes=[R_ws])
                    P.flush()
                with ExitStack() as ph:
                    hm = [SB(f"hm1_{i}", [128, 8, 512], BF16, ph) for i in range(2)]; R_hm = [[Res() for _ in range(8)] for _ in range(2)]
                    w_in = SB("w_in1", [128, 8, D], BF16, ph); R_win = Res()
                    rows1 = SB("rows1", [128, 3 * D], F32, ph); R_rows1 = Res()
                    ntmp = mk_norm_tmp(ph, "b1")
                    vz = SB("vz", [128, D], F32, ph); R_vz = Res()
                    bst = SB("bst", [128, 2, 6], F32, ph); R_bst = Res()
                    mv = SB("mv", [128, 2], F32, ph); R_mv = Res()
                    PSs = ph.enter_context(nc.psum_tensor(f"L{P.phase}_b1pss", [128, 512], F32)); R_pss = Res()
                    PSa = [ph.enter_context(nc.psum_tensor(f"L{P.phase}_b1psa{i}", [128, 512], F32)) for i in range(2)]; R_psa = [Res() for _ in range(2)]
                    PSv = [ph.enter_context(nc.psum_tensor(f"L{P.phase}_b1psv{i}", [128, 512], F32)) for i in range(4)]; R_psv = [Res() for _ in range(4)]
                    P.dma('sp', rows1[:], rows1_d[0:1, 0:3 * D].partition_broadcast(128), cs_ld, writes=[R_rows1])
                    for pas in range(2):
                        P.dma('pool', w_in[:], od_w_in[:, pas * D:(pas + 1) * D].rearrange("(k p) n -> p k n", p=128), wA, writes=[R_win])
                        na = 0
                        for tt in range(4):
                            hb = tt % 2; hmt = hm[hb]
                            tok = slice(tt * 512, (tt + 1) * 512)
                            norm_mod(ph, lambda k, tok=tok: hT[:, k, tok], 512, lambda k: der[:, 1, 0, k, 0:1], lambda k: mod[:, 1, k, 0:1],
                                     lambda k, hmt=hmt: hmt[:, k, :], [R_h[k][tt] for k in range(8)], R_hm[hb], PSs, R_pss, ntmp)
                            if pas == 0 and tt == 0: dump('hm1', hmt[:], R_hm[hb])
                            if pas == 0:
                                for c in range(8):
                                    pi = na % 2; na += 1
                                    for k in range(8):
                                        P.op('pe', lambda e, k=k, c=c, pi=pi, hmt=hmt: e.matmul(PSa[pi][:], w_in[:, k, c * 128:(c + 1) * 128], hmt[:, k, :], start=(k == 0), stop=(k == 7)),
                                             reads=[R_win, R_hm[hb][k]], writes=[R_psa[pi]])
                                    P.op('act', lambda e, c=c, pi=pi, tok=tok: e.activation(out=uT[:, c, tok], in_=PSa[pi][:], func=AF.Gelu, bias=V('od_bin_u', c, 1), scale=1.0),
                                         reads=[R_psa[pi], R_vecs], writes=[R_uT[c][tt]])
                            else:
                                for sub in range(4):
                                    i = tt * 4 + sub
                                    lc = slice(sub * 128, (sub + 1) * 128)
                                    for hh in range(2):
                                        pv = (i * 2 + hh) % 4
                                        for k in range(8):
                                            P.op('pe', lambda e, k=k, hh=hh, pv=pv, lc=lc, hmt=hmt: e.matmul(PSv[pv][:], hmt[:, k, lc], w_in[:, k, hh * 512:(hh + 1) * 512], start=(k == 0), stop=(k == 7)),
                                                 reads=[R_win, R_hm[hb][k]], writes=[R_psv[pv]])
                                        P.op('dve', lambda e, hh=hh, pv=pv: e.tensor_tensor(vz[:, hh * 512:(hh + 1) * 512], PSv[pv][:], rows1[:, hh * 512:(hh + 1) * 512], op=ALU.add),
                                             reads=[R_psv[pv], R_rows1], writes=[R_vz])
                                    P.op('act', lambda e: e.activation(out=vz[:], in_=vz[:], func=AF.Gelu), reads=[R_vz], writes=[R_vz])
                                    for hh in range(2):
                                        P.op('dve', lambda e, hh=hh: e.bn_stats(out=bst[:, hh, :], in_=vz[:, hh * 512:(hh + 1) * 512]), reads=[R_vz], writes=[R_bst])
                                    P.op('dve', lambda e: e.bn_aggr(out=mv[:], in_=bst[:]), reads=[R_bst], writes=[R_mv])
                                    P.op('act', lambda e: e.activation(out=mv[:, 1:2], in_=mv[:, 1:2], func=AF.Sqrt, bias=ntmp['eps'][:, 0:1], scale=1.0), reads=[R_mv], writes=[R_mv])
                                    P.op('dve', lambda e: e.reciprocal(mv[:, 1:2], mv[:, 1:2]), reads=[R_mv], writes=[R_mv])
                                    P.op('dve', lambda e: e.tensor_scalar(vz[:], vz[:], mv[:, 0:1], mv[:, 1:2], op0=ALU.subtract, op1=ALU.mult),
                                         reads=[R_vz, R_mv], writes=[R_vz])
                                    P.op('pool', lambda e: e.tensor_tensor(vz[:], vz[:], rows1[:, 1024:2048], op=ALU.mult), reads=[R_vz, R_rows1], writes=[R_vz])
                                    P.op('pool', lambda e, i=i: e.tensor_tensor(vt[:, i, :], vz[:], rows1[:, 2048:3072], op=ALU.add), reads=[R_vz, R_rows1], writes=[R_vt[i]])
                    dump('uT', uT[:], sum(R_uT, [])); dump('vt', vt[:], R_vt)
                    P.flush()
                with ExitStack() as ph:
                    w_out = SB("w_out1", [128, 8, D], BF16, ph); R_wo = Res()
                    bsr = SB("bsr", [128, D], F32, ph); R_bsr = Res()
                    tmp = [SB(f"b3t{i}", [128, 512], F32, ph) for i in range(2)]; R_tmp = [Res() for _ in range(2)]
                    svb = [SB(f"svb{i}", [128, 128], F32, ph) for i in range(2)]; R_svb = [Res() for _ in range(2)]
                    PSg_ = [ph.enter_context(nc.psum_tensor(f"L{P.phase}_b2ps{i}", [128, 512], F32)) for i in range(2)]; R_psg_ = [Res() for _ in range(2)]
                    PSo3 = [ph.enter_context(nc.psum_tensor(f"L{P.phase}_b3ps{i}", [128, 512], F32)) for i in range(2)]; R_pso3 = [Res() for _ in range(2)]
                    P.dma('pool', w_out[:], od_w_out.rearrange("(k p) n -> p k n", p=128), wB, writes=[R_wo])
                    P.dma('sp', bsr[:], rows1_d[0:1, 3 * D:4 * D].partition_broadcast(128), cs_ld, writes=[R_bsr])
                    n2 = 0
                    for i in range(16):
                        tokc = slice(i * 128, (i + 1) * 128); tt = i // 4
                        for g in range(8):
                            pi = n2 % 2; n2 += 1
                            P.op('pe', lambda e, g=g, i=i, pi=pi: e.matmul(PSg_[pi][:, 0:128], vt[:, i, g * 128:(g + 1) * 128], wsT[:, g, :], start=True, stop=True),
                                 reads=[R_vt[i], R_ws], writes=[R_psg_[pi]])
                            P.op('dve', lambda e, g=g, pi=pi: e.tensor_tensor(svb[pi][:], PSg_[pi][:, 0:128], bsr[:, g * 128:(g + 1) * 128], op=ALU.add),
                                 reads=[R_psg_[pi], R_bsr], writes=[R_svb[pi]])
                            P.op('pool', lambda e, g=g, pi=pi, tokc=tokc: e.tensor_tensor(uT[:, g, tokc], uT[:, g, tokc], svb[pi][:], op=ALU.mult),
                                 reads=[R_svb[pi], R_uT[g][tt]], writes=[R_uT[g][tt]])
                    dump('prod', uT[:], sum(R_uT, []))
                    n3 = 0
                    for tt in range(4):
                        tok = slice(tt * 512, (tt + 1) * 512)
                        for m in range(8):
                            oi = n3 % 2; n3 += 1
                            for c in range(8):
                                P.op('pe', lambda e, c=c, m=m, oi=oi, tok=tok: e.matmul(PSo3[oi][:], w_out[:, c, m * 128:(m + 1) * 128], uT[:, c, tok], start=(c == 0), stop=(c == 7)),
                                     reads=[R_wo, R_uT[c][tt]], writes=[R_pso3[oi]])
                            P.op('act', lambda e, m=m, oi=oi: e.activation(out=tmp[oi][:], in_=PSo3[oi][:], func=AF.Identity, scale=mod[:, 1, 16 + m, 0:1], bias=der[:, 1, 2, m, 0:1]),
                                 reads=[R_pso3[oi], R_mod, R_der], writes=[R_tmp[oi]])
                            P.op('dve', lambda e, m=m, oi=oi, tok=tok: e.tensor_tensor(hT[:, m, tok], hT[:, m, tok], tmp[oi][:], op=ALU.add),
                                 reads=[R_tmp[oi], R_h[m][tt]], writes=[R_h[m][tt]])
                    dump('h_mix1', hT[:], sum(R_h, []))
                    P.flush()

        stages = dbg.get('_stages', None)
        stop_after = DEBUG.get('stop_after', 99)
        if stop_after >= 1 and DEBUG.get('sub', 9) >= 1: mixer0()
        if stop_after >= 2: moe_phase(0)
        if stop_after >= 3: mixer1()
        if stop_after >= 4: moe_phase(1)
        for k in range(8):
            P.dma('sp', outT[k * 128:(k + 1) * 128, :], hT[:, k, :], out_st, reads=R_h[k])
        P.flush()
        P.final_wait()
    return nc


def _rope_tables():
    rows = S // 64
    r = np.repeat(np.arange(rows), 64).astype(np.float32)
    col = np.tile(np.arange(64), rows).astype(np.float32)
    inv_freq = (np.float32(10000.0) ** (-np.arange(16, dtype=np.float32) / np.float32(16))).astype(np.float32)
    ang = np.concatenate([r[:, None] * inv_freq, col[:, None] * inv_freq], axis=-1).astype(np.float32)
    return np.cos(ang).astype(np.float32), np.sin(ang).astype(np.float32)


def _consts():
    c = np.zeros((128, NCONST), np.float32)
    c[:, 0:128] = np.eye(128, dtype=np.float32)
    j = np.arange(128)[:, None]; i = np.arange(128)[None, :]
    c[:, 128:256] = (j >= i).astype(np.float32)
    c[:, 256:384] = (j <= i).astype(np.float32)
    return c


def _cossin():
    c = np.zeros((128, NCS), np.float32)
    cos, sin = _rope_tables()
    c[:, 0:512] = cos.reshape(16, 128, 32).transpose(1, 0, 2).reshape(128, 512)
    c[:, 512:1024] = sin.reshape(16, 128, 32).transpose(1, 0, 2).reshape(128, 512)
    return c


def _cols(v):
    v = np.asarray(v, np.float32).reshape(-1, 128)
    return v.T


def _build_vecs(inp, b):
    f = lambda a: np.asarray(a, np.float32)
    parts = []
    sc = np.stack([_cols(f(inp['c'])[b]), _cols(f(inp['c_ctx']))], axis=2).reshape(128, 16)
    parts.append(sc)
    parts.append(_cols(f(inp['ada_b'])))
    parts.append(_cols(f(inp['norm_mix_g']))); parts.append(_cols(f(inp['norm_ffn_g'])))
    parts.append(_cols(f(inp['ev_b_in'])[0, :1024]))
    cw = f(inp['ev_conv_w'])[0]
    parts.append(cw.reshape(31, 4, 128).transpose(2, 1, 0).reshape(128, 124))
    parts.append(_cols(f(inp['ev_conv_b'])[0])); parts.append(_cols(f(inp['ev_conv_ln_g'])[0])); parts.append(_cols(f(inp['ev_conv_ln_b'])[0]))
    parts.append(_cols(f(inp['ev_b_out'])[0]))
    parts.append(_cols(f(inp['od_b_in'])[0, :1024]))
    parts.append(_cols(f(inp['od_b_out'])[0]))
    v = np.concatenate(parts, axis=1)
    assert v.shape == (128, NV), v.shape
    return np.ascontiguousarray(v)


_NC_CACHE = {}

def kernel(**inp):
    f = lambda a: np.ascontiguousarray(np.asarray(a, np.float32))
    key = (tuple(sorted(DEBUG.get('dbg', {}).items())), DEBUG.get('stop_after', 99), DEBUG.get('sub', 9), DEBUG.get('n_exp', 32), DEBUG.get('a1', 99))
    if key not in _NC_CACHE:
        _NC_CACHE[key] = build_program(DEBUG.get('dbg'))
    nc = _NC_CACHE[key]
    n = DEBUG.get('n_cores', 8)
    consts = _consts()
    x = f(inp['x']); ctx = f(inp['ctx'])
    rows0 = np.concatenate([f(inp['ev_b_in'])[0, 1024:], f(inp['ev_q_norm_g'])[0], f(inp['ev_k_norm_g'])[0], f(inp['ev_sink'])[0]])[None, :]
    rows1 = np.concatenate([f(inp['od_b_in'])[0, 1024:], f(inp['od_v_ln_g'])[0], f(inp['od_v_ln_b'])[0], f(inp['od_b_s'])[0].reshape(-1)])[None, :]
    rowsm = f(inp['moe_router_b']).reshape(1, 64)
    b1 = f(inp['moe_b1'])
    b1cols = np.ascontiguousarray(np.stack([_cols(b1[l]) for l in range(2)], axis=0))
    shared = dict(consts=consts, cossin=_cossin(), b1cols=b1cols, rows0=np.ascontiguousarray(rows0), rows1=np.ascontiguousarray(rows1), rowsm=rowsm,
                  ada_w=f(inp['ada_w']), ev_w_in=f(inp['ev_w_in'])[0], ev_w_out=f(inp['ev_w_out'])[0],
                  od_w_in=f(inp['od_w_in'])[0], od_w_s=f(inp['od_w_s'])[0], od_w_out=f(inp['od_w_out'])[0],
                  r_w=f(inp['moe_router_w']), w1=f(inp['moe_w1']), w2=f(inp['moe_w2']), b2=f(inp['moe_b2']))
    in_maps = []
    for b in range(n):
        m = dict(shared)
        m['xT'] = np.ascontiguousarray(x[b].T); m['ctxT'] = np.ascontiguousarray(ctx[b].T)
        m['vecs'] = _build_vecs(inp, b)
        in_maps.append(m)
    res = run_bass_kernel_spmd(nc, in_maps, core_ids=list(range(n)))
    DEBUG['last'] = res.results
    out = np.stack([np.ascontiguousarray(res.results[b]['outT'].T) for b in range(n)], axis=0)
    return out.astype(np.float32)
```

```python
import numpy as np
import ml_dtypes
import concourse.bass as bass
import concourse.mybir as mybir
from concourse.bass_utils import run_bass_kernel_spmd
from contextlib import ExitStack

F32, BF16 = mybir.dt.float32, mybir.dt.bfloat16
AF = mybir.ActivationFunctionType
ALU = mybir.AluOpType
AX = mybir.AxisListType

D = 1024; S = 2048; L = 256; NE = 32; EPS = 1e-6
DEBUG = {}

VOFF = {}
def _mk_voff():
    o = 0
    for name, n in [('sc_in', 16), ('ada_b', 96), ('nmg', 16), ('nfg', 16), ('ev_bin_a', 8),
                    ('conv_w', 124), ('conv_b', 4), ('cln_g', 4), ('cln_b', 4), ('ev_bout', 8),
                    ('od_bin_u', 8), ('od_bout', 8)]:
        VOFF[name] = o; o += n
    return o
NV = _mk_voff()
NCONST = 128 + 256
NCS = 1024
NROW0 = 768 + 64 + 64 + 8
NROW1 = 4096


class Res:
    __slots__ = ('name', 'w', 'r')
    def __init__(self, name=''):
        self.name = name; self.w = {}; self.r = {}

class DSem:
    def __init__(self, h):
        self.h = h; self.count = 0

class Ins:
    __slots__ = ('eng', 'fn', 'deps', 'seq', 'need', 'dsem', 'dord', 'phase')

class Prog:
    CE = ('act', 'dve', 'pool', 'pe')
    def __init__(self, nc, es):
        self.nc = nc
        self.esem = {e: es.enter_context(nc.semaphore('s_' + e)) for e in self.CE}
        self.seq = {e: 0 for e in self.CE}
        self.waited = {x: {} for x in self.CE + ('sp',)}
        self.dsems = []
        self.es = es
        self.cur = {x: [] for x in self.CE + ('sp',)}
        self.phase = 0
        self.bar = None
        self.n_ins = 0
        self.dpool = {}; self.dpi = {}

    def dsem(self, name):
        s = DSem(self.es.enter_context(self.nc.semaphore(name)))
        self.dsems.append(s)
        return s

    def op(self, eng, fn, reads=(), writes=(), dsem=None):
        I = Ins(); I.eng = eng; I.fn = fn; I.seq = 0; I.need = False; I.dsem = dsem; I.phase = self.phase
        key = dsem if dsem is not None else eng
        if dsem is not None:
            dsem.count += 1; I.dord = dsem.count
        raw = set()
        deps = set()
        for r in reads:
            for d in r.w.values():
                deps.add(d); raw.add(d)
        for w in writes:
            for d in w.w.values(): deps.add(d)
            for d in w.r.values(): deps.add(d)
        out = []
        for d in deps:
            if d.phase != self.phase: continue
            if d.dsem is None and d.eng == eng and dsem is None:
                if eng == 'pe': continue
            out.append(d)
            if d.dsem is None: d.need = True
        I.deps = out
        for r in reads: r.r[key] = I
        for w in writes: w.w[key] = I
        self.cur[eng].append(I)
        self.n_ins += 1
        return I

    def dma(self, eng, out, in_, dsem=None, reads=(), writes=()):
        auto = dsem is None or getattr(dsem, 'auto', True)
        if auto:
            pool = self.dpool.setdefault(eng, [])
            if not pool:
                for i in range(12 if eng == 'sp' else 8):
                    d = self.dsem(f"dp_{eng}{i}"); d.last = None; pool.append(d)
                self.dpi[eng] = 0
            dsem = pool[self.dpi[eng] % len(pool)]; self.dpi[eng] += 1
        I = self.op(eng, lambda e, o=out, i=in_: e.dma_start(out=o, in_=i), reads, writes, dsem=dsem)
        if auto:
            if dsem.last is not None and dsem.last.phase == self.phase and dsem.last not in I.deps:
                I.deps.append(dsem.last)
            dsem.last = I
        return I

    def flush(self):
        nc = self.nc
        for e in self.CE:
            for I in reversed(self.cur[e]):
                if I.dsem is None:
                    I.need = True; break
        for e in self.CE:
            for I in self.cur[e]:
                if I.need and I.dsem is None:
                    self.seq[e] += 1; I.seq = self.seq[e]
        bar_prev = self.bar
        with nc.Block() as block:
            def emit(X, engobj):
                wt = self.waited[X]
                def wait(key, sem, val):
                    if wt.get(key, 0) < val:
                        engobj.wait_ge(sem, val); wt[key] = val
                if bar_prev is not None:
                    for e, v in bar_prev['seq'].items():
                        if v > 0: wait(e, self.esem[e], v)
                    for s, c in bar_prev['dma']:
                        if c > 0: wait(s, s.h, 16 * c)
                for I in self.cur[X]:
                    for d in I.deps:
                        if d.dsem is not None: wait(d.dsem, d.dsem.h, 16 * d.dord)
                        else: wait(d.eng, self.esem[d.eng], d.seq)
                    bi = I.fn(engobj)
                    if I.dsem is not None: bi.then_inc(I.dsem.h, 16)
                    elif I.need: bi.then_inc(self.esem[X], 1)
            @block.sync
            def _(e): emit('sp', e)
            @block.scalar
            def _(e): emit('act', e)
            @block.vector
            def _(e): emit('dve', e)
            @block.gpsimd
            def _(e): emit('pool', e)
            @block.tensor
            def _(e): emit('pe', e)
        self.bar = {'seq': dict(self.seq), 'dma': [(s, s.count) for s in self.dsems]}
        self.cur = {x: [] for x in self.CE + ('sp',)}
        self.phase += 1

    def final_wait(self):
        nc = self.nc
        bar = self.bar
        with nc.Block() as block:
            @block.sync
            def _(e):
                for en, v in bar['seq'].items():
                    if v > 0: e.wait_ge(self.esem[en], v)
                for s, c in bar['dma']:
                    if c > 0: e.wait_ge(s.h, 16 * c)


class Ring:
    def __init__(self, items):
        self.items = items; self.i = 0
    def next(self):
        it = self.items[self.i % len(self.items)]; self.i += 1
        return it


def build_program(dbg=None):
    dbg = dbg or {}
    nc = bass.Bass("TRN2", target_bir_lowering=False)
    dt_in = lambda name, shape, dt=F32: nc.dram_tensor(name, list(shape), dt, kind="ExternalInput").ap()
    xT = dt_in("xT", [D, S]); ctxT = dt_in("ctxT", [D, L])
    vecs_d = dt_in("vecs", [128, NV]); consts_d = dt_in("consts", [128, NCONST]); cs_d = dt_in("cossin", [128, NCS])
    b1c_d = dt_in("b1cols", [2, 128, 512])
    rows0_d = dt_in("rows0", [1, NROW0]); rows1_d = dt_in("rows1", [1, NROW1]); rowsm_d = dt_in("rowsm", [1, 64])
    ada_w = dt_in("ada_w", [2, D, 6 * D])
    ev_w_in = dt_in("ev_w_in", [D, 1792]); ev_w_out = dt_in("ev_w_out", [D, D])
    od_w_in = dt_in("od_w_in", [D, 2 * D]); od_w_s = dt_in("od_w_s", [8, 128, 128]); od_w_out = dt_in("od_w_out", [D, D])
    r_w = dt_in("r_w", [2, D, NE]); w1 = dt_in("w1", [2, NE, D, 2 * D]); w2 = dt_in("w2", [2, NE, D, D])
    b2 = dt_in("b2", [2, NE, D])
    outT = nc.dram_tensor("outT", [D, S], F32, kind="ExternalOutput").ap()
    gt_scr = nc.dram_tensor("gt_scr", [2, NE, S], F32, kind="Internal").ap()
    mk_scr = nc.dram_tensor("mk_scr", [2, NE, S], BF16, kind="Internal").ap()
    dbg_out = {}
    for k, shp in dbg.items():
        dbg_out[k] = nc.dram_tensor("dbg_" + k, list(shp), F32, kind="ExternalOutput").ap()

    es = ExitStack()
    with es:
        P = Prog(nc, es)
        _sbn = [0]
        def SB(name, shape, dt=F32, st=es):
            _sbn[0] += 1
            return st.enter_context(nc.sbuf_tensor(f"sb{_sbn[0]}_{name}", list(shape), dt))
        hT = SB("hT", [128, 8, S]); R_h = [[Res() for _ in range(4)] for _ in range(8)]
        vecs = SB("vecs", [128, NV]); R_vecs = Res()
        consts = SB("consts", [128, 128]); R_consts = Res()
        mod = SB("mod", [128, 2, 48, 2]); R_mod = Res()
        der = SB("der", [128, 2, 5, 8, 2]); R_der = Res()
        ones_bf = SB("ones_bf", [128, 128], BF16); ones_f = SB("ones_f", [128, 128]); ident_bf = SB("ident_bf", [128, 128], BF16)
        maskb = SB("maskb", [128, 256], BF16)
        R_k = Res()
        out_st = P.dsem("out_st"); out_st.auto = False; dbg_st = P.dsem("dbg_st"); dbg_st.auto = False
        cs_ld = None; wA = None; wB = None
        ident_f = consts[:, 0:128]
        def V(name, i=0, n=1): return vecs[:, VOFF[name] + i: VOFF[name] + i + n]

        def dump(name, ap, res_list):
            if name in dbg_out:
                P.dma('sp', dbg_out[name], ap, dbg_st, reads=res_list)

        scg = SB("scg", [128, 16], F32); R_sc = Res()
        def ada_layer(l, ph, nstage, width, lazy=False):
            PS = [ph.enter_context(nc.psum_tensor(f"L{P.phase}_adaps{l}_{i}", [128, 512], F32)) for i in range(2)]
            R_ps = [Res() for _ in range(2)]
            PST = ph.enter_context(nc.psum_tensor(f"L{P.phase}_adapst{l}", [128, 512], F32)); R_pst0 = Res()
            stage = [SB(f"adast{l}_{i}", [128, 8, width], F32, ph) for i in range(nstage)]
            R_st = [Res() for _ in range(nstage)]
            modrow = SB(f"modrow{l}", [2, 6 * D], F32, ph); R_mr = Res()
            items = []
            def chunk(cb):
                si = cb % nstage; pi = cb % 2
                P.dma('sp', stage[si][:], ada_w[l].rearrange("(k p) n -> p k n", p=128)[:, :, cb * width:(cb + 1) * width],
                      None, writes=[R_st[si]])
                for k in range(8):
                    P.op('pe', lambda e, k=k: e.matmul(
                        PS[pi][0:2, 0:width], scg[:, 2 * k:2 * k + 2], stage[si][:, k, :], start=(k == 0), stop=(k == 7)),
                        reads=[R_st[si], R_sc], writes=[R_ps[pi]])
                P.op('act', lambda e: e.copy(out=modrow[:, cb * width:(cb + 1) * width], in_=PS[pi][0:2, 0:width]),
                     reads=[R_ps[pi]], writes=[R_mr])
            for cb in range(6 * D // width):
                items.append(lambda cb=cb: chunk(cb))
            items.append(lambda: ada_finish(l, PST, R_pst0, modrow, R_mr))
            if lazy:
                return items
            for it in items: it()

        def ada_finish(l, PST, R_pst0, modrow, R_mr):
            for j in range(48):
                P.op('pe', lambda e, j=j: e.transpose(out=PST[:, 2 * j:2 * j + 2], in_=modrow[:, j * 128:(j + 1) * 128], identity=ident_f[0:2, 0:2]),
                     reads=[R_mr, R_consts], writes=[R_pst0])
            P.op('dve', lambda e: e.tensor_tensor(
                mod[:, l, :, :], PST[:, 0:96].rearrange("p (j s) -> p j s", s=2),
                V('ada_b', l * 48, 48).unsqueeze(2).to_broadcast([128, 48, 2]), op=ALU.add),
                reads=[R_pst0, R_vecs], writes=[R_mod])
            P.op('dve', lambda e: e.scalar_tensor_tensor(
                out=der[:, l, 0], in0=mod[:, l, 8:16, :], scalar=1.0,
                in1=V('nmg', l * 8, 8).unsqueeze(2).to_broadcast([128, 8, 2]), op0=ALU.add, op1=ALU.mult),
                reads=[R_mod, R_vecs], writes=[R_der])
            P.op('dve', lambda e: e.scalar_tensor_tensor(
                out=der[:, l, 1], in0=mod[:, l, 32:40, :], scalar=1.0,
                in1=V('nfg', l * 8, 8).unsqueeze(2).to_broadcast([128, 8, 2]), op0=ALU.add, op1=ALU.mult),
                reads=[R_mod, R_vecs], writes=[R_der])
            bo = 'ev_bout' if l == 0 else 'od_bout'
            P.op('dve', lambda e: e.tensor_tensor(
                der[:, l, 2], mod[:, l, 16:24, :], V(bo, 0, 8).unsqueeze(2).to_broadcast([128, 8, 2]), op=ALU.mult),
                reads=[R_mod, R_vecs], writes=[R_der])

        with ExitStack() as ph:
            P.dma('sp', vecs[:], vecs_d[:, :], cs_ld, writes=[R_vecs])
            P.dma('sp', consts[:], consts_d[:, 0:128], cs_ld, writes=[R_consts])
            mtmp = SB("mtmp", [128, 256], F32, ph); R_mtmp = Res()
            P.dma('sp', mtmp[:], consts_d[:, 128:384], cs_ld, writes=[R_mtmp])
            for k in range(8):
                P.dma('sp', hT[:, k, :], xT[k * 128:(k + 1) * 128, :], cs_ld, writes=R_h[k])
            P.op('dve', lambda e: e.memset(ones_f[:], 1.0), writes=[R_k])
            P.op('dve', lambda e: e.memset(ones_bf[:], 1.0), writes=[R_k])
            P.op('dve', lambda e: e.tensor_copy(ident_bf[:], ident_f), reads=[R_consts], writes=[R_k])
            P.op('dve', lambda e: e.tensor_copy(maskb[:], mtmp[:]), reads=[R_mtmp], writes=[R_k])
            P.op('act', lambda e: e.activation(out=scg[:], in_=V('sc_in', 0, 16), func=AF.Silu), reads=[R_vecs], writes=[R_sc])
            ada_layer(0, ph, 3, 512)
            dump('mod', mod[:].rearrange("p l j s -> p (l j s)"), [R_mod])
            P.flush()

        def norm_mod(ph_tiles, src_fn, n_tok, A_fn, sh_fn, dst_fn, R_src, R_dst, ps_ss, R_ps_ss, tmp, also32=None):
            sq, R_sq = tmp['sq'], tmp['R_sq']
            for k in range(8):
                s_i = tmp['sqi'][0] % len(sq); tmp['sqi'][0] += 1
                P.op('act', lambda e, k=k, s_i=s_i: e.activation(out=sq[s_i][:, 0:n_tok], in_=src_fn(k), func=AF.Square),
                     reads=[R_src[k]], writes=[R_sq[s_i]])
                P.op('pe', lambda e, k=k, s_i=s_i: e.matmul(ps_ss[:, 0:n_tok], ones_bf[:], sq[s_i][:, 0:n_tok], start=(k == 0), stop=(k == 7)),
                     reads=[R_sq[s_i], R_k], writes=[R_ps_ss])
            rs, R_rs = tmp['rs'], tmp['R_rs']
            P.op('act', lambda e: e.activation(out=rs[:, 0:n_tok], in_=ps_ss[:, 0:n_tok], func=AF.Sqrt, scale=1.0 / D, bias=tmp['eps'][:, 0:1]),
                 reads=[R_ps_ss], writes=[R_rs])
            P.op('dve', lambda e: e.reciprocal(rs[:, 0:n_tok], rs[:, 0:n_tok]), reads=[R_rs], writes=[R_rs])
            for k in range(8):
                t_i = tmp['ti'][0] % len(tmp['t']); tmp['ti'][0] += 1
                tt_, R_tt = tmp['t'][t_i], tmp['R_t'][t_i]
                P.op('dve', lambda e, k=k, tt_=tt_: e.tensor_tensor(tt_[:, 0:n_tok], src_fn(k), rs[:, 0:n_tok], op=ALU.mult),
                     reads=[R_src[k], R_rs], writes=[R_tt])
                if also32 is None:
                    P.op('act', lambda e, k=k, tt_=tt_: e.activation(out=dst_fn(k), in_=tt_[:, 0:n_tok], func=AF.Identity, scale=A_fn(k), bias=sh_fn(k)),
                         reads=[R_tt, R_der, R_mod], writes=[R_dst[k]])
                else:
                    d32, R_d32 = also32
                    P.op('act', lambda e, k=k, tt_=tt_: e.activation(out=d32(k), in_=tt_[:, 0:n_tok], func=AF.Identity, scale=A_fn(k), bias=sh_fn(k)),
                         reads=[R_tt, R_der, R_mod], writes=[R_d32[k]])
                    P.op('pool', lambda e, k=k: e.tensor_copy(dst_fn(k), d32(k)), reads=[R_d32[k]], writes=[R_dst[k]])

        def mk_norm_tmp(ph, pfx):
            t = {'sq': [SB(f"{pfx}sq{i}", [128, 512], BF16, ph) for i in range(3)], 'R_sq': [Res() for _ in range(3)], 'sqi': [0],
                 'rs': SB(f"{pfx}rs", [128, 512], F32, ph), 'R_rs': Res(),
                 't': [SB(f"{pfx}t{i}", [128, 512], F32, ph) for i in range(2)], 'R_t': [Res() for _ in range(2)], 'ti': [0],
                 'eps': SB(f"{pfx}eps", [128, 1], F32, ph)}
            P.op('dve', lambda e: e.memset(t['eps'][:], EPS), writes=[t['R_rs']])
            return t

        def moe_phase(l):
            gtf = lambda m: mod[:, l, 40 + m, 0:1]
            with ExitStack() as pm:
                hf = SB("hf", [128, 8, S], BF16, pm); R_hf = [[Res() for _ in range(4)] for _ in range(8)]
                with ExitStack() as ph:
                    hf32 = SB("hf32", [128, 8, 512], F32, ph); R_hf32 = [Res() for _ in range(8)]
                    GT = SB("GT", [NE, S], F32, ph); R_GT = [Res() for _ in range(4)]
                    GTs = SB("GTs", [NE, S], F32, ph); R_GTs = Res()
                    rw = SB("rw", [128, 8, NE], F32, ph); R_rw = Res()
                    B2 = SB("B2", [NE, D], F32, ph); R_B2 = Res()
                    rb = SB("rb", [128, NE], F32, ph); R_rb = Res()
                    lg = SB("lg", [128, 16, NE], F32, ph); R_lg = [Res() for _ in range(16)]
                    Gt = SB("Gt", [128, 16, NE], F32, ph)
                    sm = SB("sm", [128, 16, 16], F32, ph)
                    ntmp = mk_norm_tmp(ph, f"m{l}")
                    PSx = ph.enter_context(nc.psum_tensor(f"L{P.phase}_psx", [128, 512], F32)); R_psx = Res()
                    PSy = [ph.enter_context(nc.psum_tensor(f"L{P.phase}_psy{i}", [128, 512], F32)) for i in range(2)]; R_psy = [Res() for _ in range(2)]
                    PSo = [ph.enter_context(nc.psum_tensor(f"L{P.phase}_m1pso{i}", [128, 512], F32)) for i in range(2)]; R_pso = [Res() for _ in range(2)]
                    P.dma('sp', rw[:], r_w[l].rearrange("(k p) n -> p k n", p=128), cs_ld, writes=[R_rw])
                    P.dma('sp', B2[:], b2[l], cs_ld, writes=[R_B2])
                    P.dma('sp', rb[:], rowsm_d[0:1, l * 32:(l + 1) * 32].partition_broadcast(128), cs_ld, writes=[R_rb])
                    ada_items = ada_layer(1, ph, 2, 256, lazy=True) if l == 0 else []
                    for tt in range(4):
                        tok = slice(tt * 512, (tt + 1) * 512)
                        norm_mod(ph, lambda k, tok=tok: hT[:, k, tok], 512,
                                 lambda k: der[:, l, 1, k, 0:1], lambda k: mod[:, l, 24 + k, 0:1],
                                 lambda k, tok=tok: hf[:, k, tok], [R_h[k][tt] for k in range(8)], [R_hf[k][tt] for k in range(8)],
                                 PSx, R_psx, ntmp, also32=(lambda k: hf32[:, k, :], R_hf32))
                        for sub in range(4):
                            i = tt * 4 + sub; yi = i % 2
                            for k in range(8):
                                P.op('pe', lambda e, k=k, sub=sub, yi=yi: e.matmul(PSy[yi][:, 0:NE], hf32[:, k, sub * 128:(sub + 1) * 128], rw[:, k, :],
                                                                                  start=(k == 0), stop=(k == 7)),
                                     reads=[R_hf32[k], R_rw], writes=[R_psy[yi]])
                            P.op('dve', lambda e, i=i, yi=yi: e.tensor_tensor(lg[:, i, :], PSy[yi][:, 0:NE], rb[:], op=ALU.add),
                                 reads=[R_psy[yi], R_rb], writes=[R_lg[i]])
                            P.op('dve', lambda e, i=i: e.max(out=sm[:, i, 0:8], in_=lg[:, i, :]), reads=[R_lg[i]], writes=[R_lg[i]])
                            P.op('dve', lambda e, i=i: e.tensor_scalar(sm[:, i, 8:9], sm[:, i, 0:1], -1.0, None, op0=ALU.mult),
                                 reads=[R_lg[i]], writes=[R_lg[i]])
                            P.op('act', lambda e, i=i: e.activation(out=Gt[:, i, :], in_=lg[:, i, :], func=AF.Exp, bias=sm[:, i, 8:9], scale=1.0),
                                 reads=[R_lg[i]], writes=[R_lg[i]])
                            P.op('dve', lambda e, i=i: e.scalar_tensor_tensor(out=Gt[:, i, :], in0=lg[:, i, :], scalar=sm[:, i, 3:4], in1=Gt[:, i, :],
                                                                                op0=ALU.is_ge, op1=ALU.mult),
                                 reads=[R_lg[i]], writes=[R_lg[i]])
                            P.op('dve', lambda e, i=i: e.reduce_sum(out=sm[:, i, 9:10], in_=Gt[:, i, :], axis=AX.X), reads=[R_lg[i]], writes=[R_lg[i]])
                            P.op('dve', lambda e, i=i: e.reciprocal(sm[:, i, 10:11], sm[:, i, 9:10]), reads=[R_lg[i]], writes=[R_lg[i]])
                            P.op('dve', lambda e, i=i: e.tensor_scalar(Gt[:, i, :], Gt[:, i, :], sm[:, i, 10:11], None, op0=ALU.mult),
                                 reads=[R_lg[i]], writes=[R_lg[i]])
                            P.op('pe', lambda e, i=i, yi=yi: e.transpose(out=PSy[yi][0:NE, 128:256], in_=Gt[:, i, :], identity=ident_f),
                                 reads=[R_lg[i], R_consts], writes=[R_psy[yi]])
                            P.op('act', lambda e, i=i, yi=yi: e.copy(out=GT[:, i * 128:(i + 1) * 128], in_=PSy[yi][0:NE, 128:256]),
                                 reads=[R_psy[yi]], writes=[R_GT[tt]])
                            if ada_items: ada_items.pop(0)()
                    dump(f'hf{l}', hf[:], sum(R_hf, []))
                    dump(f'G{l}', Gt[:].rearrange("p i e -> p (i e)"), R_lg)
                    P.op('dve', lambda e: e.tensor_scalar(GTs[:], GT[:], 1.0 / 1.702, None, op0=ALU.mult), reads=R_GT, writes=[R_GTs])
                    P.dma('sp', gt_scr[l], GTs[:], reads=[R_GTs])
                    GTm = SB("GTm", [NE, S], BF16, ph); R_GTm = Res()
                    P.op('dve', lambda e: e.tensor_scalar(GTm[:], GT[:], 0.0, None, op0=ALU.is_gt), reads=R_GT, writes=[R_GTm])
                    P.dma('sp', mk_scr[l], GTm[:], reads=[R_GTm])
                    while ada_items: ada_items.pop(0)()
                    for tt in range(4):
                        tok = slice(tt * 512, (tt + 1) * 512)
                        for m in range(8):
                            oi = (tt * 8 + m) % 2
                            P.op('pe', lambda e, m=m, oi=oi, tok=tok: e.matmul(PSo[oi][:], B2[:, m * 128:(m + 1) * 128], GT[:, tok], start=True, stop=True),
                                 reads=[R_B2, R_GT[tt]], writes=[R_pso[oi]])
                            P.op('dve', lambda e, m=m, oi=oi, tok=tok: e.scalar_tensor_tensor(out=hT[:, m, tok], in0=PSo[oi][:], scalar=gtf(m), in1=hT[:, m, tok],
                                                                                               op0=ALU.mult, op1=ALU.add),
                                 reads=[R_pso[oi], R_mod, R_h[m][tt]], writes=[R_h[m][tt]])
                    P.flush()
                with ExitStack() as ph:
                    act = SB("actT", [128, 8, S], BF16, ph); R_act = [[Res() for _ in range(4)] for _ in range(8)]
                    W1s = [SB(f"W1s{i}", [128, 8, 2, 512], BF16, ph) for i in range(2)]; R_W1 = [[Res(), Res()] for _ in range(2)]
                    W2q = [SB(f"W2q{i}", [128, 8, 256], BF16, ph) for i in range(3)]; R_W2 = [Res() for _ in range(3)]
                    nq = [0]
                    w1sem = [None] * 2; w2sem = [None] * 2
                    hfm = [SB(f"hfm{i}", [128, 8, 512], BF16, ph) for i in range(2)]; R_hfm = [[Res() for _ in range(8)] for _ in range(2)]
                    maskt = SB("maskt", [128, 512], BF16, ph); R_maskt = Res()
                    sg = [SB(f"sg_{i}", [128, 512], F32, ph) for i in range(2)]; R_sg = [Res() for _ in range(2)]
                    u0 = [SB(f"u0_{i}", [128, 512], F32, ph) for i in range(2)]; R_u0 = [Res() for _ in range(2)]
                    b1e = [SB(f"b1e{i}", [128, 16], F32, ph) for i in range(2)]; R_b1e = [Res() for _ in range(2)]
                    PSg = [ph.enter_context(nc.psum_tensor(f"L{P.phase}_psg{i}", [128, 512], F32)) for i in range(2)]; R_psg = [Res() for _ in range(2)]
                    PSu = [ph.enter_context(nc.psum_tensor(f"L{P.phase}_psu{i}", [128, 512], F32)) for i in range(2)]; R_psu = [Res() for _ in range(2)]
                    PSo = [ph.enter_context(nc.psum_tensor(f"L{P.phase}_pso{i}", [128, 512], F32)) for i in range(4)]; R_pso = [Res() for _ in range(4)]

                    def load_w1(e, half):
                        for two in range(2):
                            src = w1[l, e].rearrange("(k p) (two h j) -> p k two h j", p=128, two=2, h=2)[:, :, two, half, :]
                            P.dma('pool', W1s[half][:, :, two, :], src, w1sem[half], writes=[R_W1[half][two]])
                    def load_w2q(e, q):
                        slot = (e * 4 + q) % 3
                        src = w2[l, e].rearrange("(k p) (q j) -> p k q j", p=128, q=4)[:, :, q, :]
                        P.dma('pool', W2q[slot][:], src, None, writes=[R_W2[slot]])

                    def load_b1(e):
                        eb_ = e % 2
                        P.dma('sp', b1e[eb_][:], b1c_d[l][:, e * 16:(e + 1) * 16], writes=[R_b1e[eb_]])
                        P.op('dve', lambda e_: e_.tensor_scalar(b1e[eb_][:, 8:16], b1e[eb_][:, 8:16], 1.0, None, op0=ALU.add), reads=[R_b1e[eb_]], writes=[R_b1e[eb_]])
                    load_b1(0)
                    load_w1(0, 0); load_w1(0, 1)
                    cnt = {'g': 0, 'o': 0, 'b': 0}
                    n_exp = DEBUG.get('n_exp', NE)
                    Gb = [SB(f"Gb{i}", [128, 512], F32, ph) for i in range(2)]; R_Gb = [Res() for _ in range(2)]
                    pend = [None]
                    def flush_tail():
                        if pend[0] is not None:
                            pend[0](); pend[0] = None
                    groups = [(e_, tt) for e_ in range(n_exp) for tt in range(4)]
                    def prep(n):
                        e_, tt = groups[n]; r = n % 2
                        tok = slice(tt * 512, (tt + 1) * 512)
                        P.dma('sp', maskt[:], mk_scr[l, e_:e_ + 1, tok].partition_broadcast(128), writes=[R_maskt])
                        return [lambda k=k: P.op('dve', lambda e: e.tensor_tensor(hfm[r][:, k, :], hf[:, k, tok], maskt[:], op=ALU.mult),
                                                 reads=[R_hf[k][tt], R_maskt], writes=[R_hfm[r][k]]) for k in range(8)]
                    for it in prep(0): it()
                    for n_, (e_, tt) in enumerate(groups):
                        if tt == 0:
                            load_w2q(e_, 0); load_w2q(e_, 1); load_w2q(e_, 2)
                            if e_ + 1 < n_exp: load_b1(e_ + 1)
                        pitems = prep(n_ + 1) if n_ + 1 < len(groups) else []
                        hr = n_ % 2
                        if True:
                            if True:
                                tok = slice(tt * 512, (tt + 1) * 512)
                                gi = cnt['b'] % 2; cnt['b'] += 1
                                P.dma('sp', Gb[gi][:], gt_scr[l, e_:e_ + 1, tok].partition_broadcast(128), writes=[R_Gb[gi]])
                                for ui in range(8):
                                    half = ui // 4; cc = ui % 4
                                    c = ui
                                    pi = cnt['g'] % 2; cnt['g'] += 1
                                    for it in pitems[ui:ui + 1]: it()
                                    for k in range(8):
                                        P.op('pe', lambda e, k=k, cc=cc, pi=pi, half=half, hr=hr: e.matmul(
                                            PSg[pi][:], W1s[half][:, k, 0, cc * 128:(cc + 1) * 128], hfm[hr][:, k, :], start=(k == 0), stop=(k == 7)),
                                            reads=[R_W1[half][0], R_hfm[hr][k]], writes=[R_psg[pi]])
                                    for k in range(8):
                                        P.op('pe', lambda e, k=k, cc=cc, pi=pi, half=half, hr=hr: e.matmul(
                                            PSu[pi][:], W1s[half][:, k, 1, cc * 128:(cc + 1) * 128], hfm[hr][:, k, :], start=(k == 0), stop=(k == 7)),
                                            reads=[R_W1[half][1], R_hfm[hr][k]], writes=[R_psu[pi]])
                                    eb = e_ % 2
                                    bg = b1e[eb][:, c:c + 1]
                                    bu = b1e[eb][:, 8 + c:9 + c]
                                    P.op('dve', lambda e, pi=pi, bg=bg: e.tensor_scalar(sg[pi][:], PSg[pi][:], bg, 7.0, op0=ALU.add, op1=ALU.min),
                                         reads=[R_psg[pi], R_b1e[eb]], writes=[R_sg[pi]])
                                    P.op('act', lambda e, pi=pi: e.activation(out=sg[pi][:], in_=sg[pi][:], func=AF.Silu, scale=1.702),
                                         reads=[R_sg[pi]], writes=[R_sg[pi]])
                                    P.op('act', lambda e, pi=pi, bu=bu: e.activation(out=u0[pi][:], in_=PSu[pi][:], func=AF.Identity, bias=bu, scale=1.0),
                                         reads=[R_psu[pi], R_b1e[eb]], writes=[R_u0[pi]])
                                    P.op('dve', lambda e, pi=pi: e.tensor_scalar(u0[pi][:], u0[pi][:], -6.0, 8.0, op0=ALU.max, op1=ALU.min),
                                         reads=[R_u0[pi]], writes=[R_u0[pi]])
                                    flush_tail()
                                    def tail(pi=pi, gi=gi, c=c, tok=tok, tt=tt):
                                        P.op('dve', lambda e: e.tensor_tensor(u0[pi][:], u0[pi][:], sg[pi][:], op=ALU.mult),
                                             reads=[R_sg[pi], R_u0[pi]], writes=[R_u0[pi]])
                                        P.op('dve', lambda e: e.tensor_tensor(act[:, c, tok], u0[pi][:], Gb[gi][:], op=ALU.mult),
                                             reads=[R_u0[pi], R_Gb[gi]], writes=[R_act[c][tt]])
                                    pend[0] = tail
                                    if tt == 3 and cc == 3 and e_ + 1 < n_exp:
                                        load_w1(e_ + 1, half)
                        if tt != 3: continue
                        flush_tail()
                        for q in range(4):
                            slot = (e_ * 4 + q) % 3
                            for tt2 in range(4):
                                tok = slice(tt2 * 512, (tt2 + 1) * 512)
                                for mm_ in range(2):
                                    m = q * 2 + mm_
                                    oi = cnt['o'] % 4; cnt['o'] += 1
                                    for k in range(8):
                                        P.op('pe', lambda e, k=k, mm_=mm_, oi=oi, slot=slot, tok=tok: e.matmul(
                                            PSo[oi][:], W2q[slot][:, k, mm_ * 128:(mm_ + 1) * 128], act[:, k, tok], start=(k == 0), stop=(k == 7)),
                                            reads=[R_W2[slot], R_act[k][tt2]], writes=[R_pso[oi]])
                                    P.op('dve', lambda e, m=m, oi=oi, tok=tok: e.scalar_tensor_tensor(out=hT[:, m, tok], in0=PSo[oi][:], scalar=gtf(m), in1=hT[:, m, tok],
                                                                                                       op0=ALU.mult, op1=ALU.add),
                                         reads=[R_pso[oi], R_mod, R_h[m][tt2]], writes=[R_h[m][tt2]])
                            if q == 0: load_w2q(e_, 3)
                    dump(f'h_moe{l}', hT[:], sum(R_h, []))
                    P.flush()

        def mixer0():
            with ExitStack() as pa:
                u_bf = SB("u_bf", [128, 4, S + 30], BF16, pa); R_u = [[Res() for _ in range(4)] for _ in range(4)]; R_upad = Res()
                qT = SB("qT", [128, 16, 512], BF16, pa); R_qT = [Res() for _ in range(16)]
                kT = SB("kT", [128, S + L], BF16, pa); R_kT = [Res() for _ in range(18)]
                vtok = SB("vtok", [128, 18, 128], BF16, pa); R_v = [Res() for _ in range(18)]
                rows0 = SB("rows0", [128, NROW0], F32, pa); R_rows0 = Res()
                es_t = SB("es_t", [128, 4], F32, pa); R_es = Res()
                with ExitStack() as ph:
                    hm = [SB(f"hm{i}", [128, 8, 512], BF16, ph) for i in range(2)]; R_hm = [[Res() for _ in range(8)] for _ in range(2)]
                    cT = SB("cT", [128, 8, L], F32, ph); R_cT = [Res() for _ in range(8)]
                    w_in = SB("w_in", [128, 8, 1792], BF16, ph); R_win = Res()
                    cs_t = SB("cs_t", [128, NCS], F32, ph); R_cs = Res()
                    G10 = SB("G10", [128, 10, 64], F32, ph); R_G10 = Res()
                    cos_t = cs_t[:, 0:512].rearrange("p (i f) -> p i f", f=32)
                    sin_t = cs_t[:, 512:1024].rearrange("p (i f) -> p i f", f=32)
                    ntmp = mk_norm_tmp(ph, "a1")
                    qkv = SB("qkv", [128, 768], F32, ph); R_qkv = Res()
                    sqq = SB("sqq", [128, 640], F32, ph); R_sqq = Res()
                    st10 = SB("st10", [128, 16], F32, ph); R_st10 = Res()
                    qn = SB("qn", [128, 10, 64], F32, ph); R_qn = Res()
                    ra = SB("ra", [128, 10, 32], F32, ph); R_ra = Res()
                    rbb = SB("rbb", [128, 10, 32], F32, ph); R_rbb = Res()
                    qr = SB("qr", [128, 10, 64], F32, ph); R_qr = Res()
                    sgl = [SB(f"sgl{i}", [128, 512], F32, ph) for i in range(2)]; R_sgl = [Res() for _ in range(2)]
                    PSs = ph.enter_context(nc.psum_tensor(f"L{P.phase}_a1pss", [128, 512], F32)); R_pss = Res()
                    PSa = [ph.enter_context(nc.psum_tensor(f"L{P.phase}_a1psa{i}", [128, 512], F32)) for i in range(4)]; R_psa = [Res() for _ in range(4)]
                    PSq = ph.enter_context(nc.psum_tensor(f"L{P.phase}_a1psq", [128, 512], F32)); R_psq = Res()
                    PSk = ph.enter_context(nc.psum_tensor(f"L{P.phase}_a1psk", [128, 512], F32)); R_psk = Res()
                    PSt = ph.enter_context(nc.psum_tensor(f"L{P.phase}_a1pst", [128, 512], F32)); R_pst = Res()
                    for k in range(8):
                        P.dma('sp', cT[:, k, :], ctxT[k * 128:(k + 1) * 128, :], cs_ld, writes=[R_cT[k]])
                    P.dma('sp', rows0[:], rows0_d[0:1, :].partition_broadcast(128), cs_ld, writes=[R_rows0])
                    P.dma('sp', cs_t[:], cs_d[:, :], cs_ld, writes=[R_cs])
                    P.dma('pool', w_in[:], ev_w_in.rearrange("(k p) n -> p k n", p=128), wA, writes=[R_win])
                    P.op('dve', lambda e: e.memset(u_bf[:, :, 0:15], 0.0), writes=[R_upad])
                    P.op('dve', lambda e: e.memset(u_bf[:, :, S + 15:S + 30], 0.0), writes=[R_upad])
                    P.op('dve', lambda e: e.tensor_copy(G10[:, 0:8, :], rows0[:, 768:832].unsqueeze(1).to_broadcast([128, 8, 64])), reads=[R_rows0], writes=[R_G10])
                    P.op('dve', lambda e: e.tensor_copy(G10[:, 8:10, :], rows0[:, 832:896].unsqueeze(1).to_broadcast([128, 2, 64])), reads=[R_rows0], writes=[R_G10])
                    P.op('act', lambda e: e.activation(out=es_t[0:64, :], in_=rows0[0:64, 896:900], func=AF.Exp), reads=[R_rows0], writes=[R_es])
                    P.op('act', lambda e: e.activation(out=es_t[64:128, :], in_=rows0[64:128, 900:904], func=AF.Exp), reads=[R_rows0], writes=[R_es])
                    na = 0
                    A1C = DEBUG.get('a1', 99)
                    for tt in range(5 if A1C >= 9 else (1 if A1C >= 1 else 0)):
                        hb = tt % 2
                        hmt = hm[hb]
                        if tt < 4:
                            tok = slice(tt * 512, (tt + 1) * 512)
                            norm_mod(ph, lambda k, tok=tok: hT[:, k, tok], 512, lambda k: der[:, 0, 0, k, 0:1], lambda k: mod[:, 0, k, 0:1],
                                     lambda k, hmt=hmt: hmt[:, k, :], [R_h[k][tt] for k in range(8)], R_hm[hb], PSs, R_pss, ntmp)
                        else:
                            norm_mod(ph, lambda k: cT[:, k, :], L, lambda k: der[:, 0, 0, k, 1:2], lambda k: mod[:, 0, k, 1:2],
                                     lambda k, hmt=hmt: hmt[:, k, 0:L], R_cT, R_hm[hb], PSs, R_pss, ntmp)
                        if tt == 0: dump('hm0', hmt[:], R_hm[hb])
                        if A1C == 1: break
                        if tt < 4:
                            for c in range(4):
                                pu = na % 4; pg = (na + 1) % 4; si = (na // 2) % 2; na += 2
                                for k in range(8):
                                    P.op('pe', lambda e, k=k, c=c, pu=pu, hmt=hmt: e.matmul(PSa[pu][:], w_in[:, k, c * 128:(c + 1) * 128], hmt[:, k, :], start=(k == 0), stop=(k == 7)),
                                         reads=[R_win, R_hm[hb][k]], writes=[R_psa[pu]])
                                for k in range(8):
                                    P.op('pe', lambda e, k=k, c=c, pg=pg, hmt=hmt: e.matmul(PSa[pg][:], w_in[:, k, 512 + c * 128:512 + (c + 1) * 128], hmt[:, k, :], start=(k == 0), stop=(k == 7)),
                                         reads=[R_win, R_hm[hb][k]], writes=[R_psa[pg]])
                                P.op('act', lambda e, c=c, pg=pg, si=si: e.activation(out=sgl[si][:], in_=PSa[pg][:], func=AF.Sigmoid, bias=V('ev_bin_a', 4 + c, 1), scale=1.0),
                                     reads=[R_psa[pg], R_vecs], writes=[R_sgl[si]])
                                P.op('dve', lambda e, c=c, pu=pu, si=si, tt=tt: e.scalar_tensor_tensor(
                                    out=u_bf[:, c, 15 + tt * 512:15 + (tt + 1) * 512], in0=PSa[pu][:], scalar=V('ev_bin_a', c, 1), in1=sgl[si][:], op0=ALU.add, op1=ALU.mult),
                                    reads=[R_psa[pu], R_sgl[si], R_vecs], writes=[R_u[c][tt]])
                        nsub = 4 if tt < 4 else 2
                        if A1C == 2: break
                        if A1C < 9: nsub = 1
                        for sub in range(nsub):
                            i = tt * 4 + sub
                            tokc = slice(i * 128, (i + 1) * 128)
                            lc = slice(sub * 128, (sub + 1) * 128)
                            main = tt < 4
                            if main:
                                for k in range(8):
                                    P.op('pe', lambda e, k=k, lc=lc, hmt=hmt: e.matmul(PSq[:], hmt[:, k, lc], w_in[:, k, 1024:1536], start=(k == 0), stop=(k == 7)),
                                         reads=[R_win, R_hm[hb][k]], writes=[R_psq])
                            for k in range(8):
                                P.op('pe', lambda e, k=k, lc=lc, hmt=hmt: e.matmul(PSk[:, 0:256], hmt[:, k, lc], w_in[:, k, 1536:1792], start=(k == 0), stop=(k == 7)),
                                     reads=[R_win, R_hm[hb][k]], writes=[R_psk])
                            if main:
                                P.op('dve', lambda e: e.tensor_tensor(qkv[:, 0:512], PSq[:], rows0[:, 0:512], op=ALU.add),
                                     reads=[R_psq, R_rows0], writes=[R_qkv])
                            P.op('dve', lambda e: e.tensor_tensor(qkv[:, 512:768], PSk[:, 0:256], rows0[:, 512:768], op=ALU.add),
                                 reads=[R_psk, R_rows0], writes=[R_qkv])
                            P.op('act', lambda e, i=i: e.copy(out=vtok[:, i, :], in_=qkv[:, 640:768]), reads=[R_qkv], writes=[R_v[i]])
                            h0 = 0 if main else 8
                            nh = 10 - h0
                            c0 = h0 * 64
                            P.op('pool', lambda e, c0=c0: e.tensor_tensor(sqq[:, c0:640], qkv[:, c0:640], qkv[:, c0:640], op=ALU.mult),
                                 reads=[R_qkv], writes=[R_sqq])
                            P.op('dve', lambda e, h0=h0, c0=c0: e.tensor_reduce(out=st10[:, h0:10], in_=sqq[:, c0:640].rearrange("p (h d) -> p h d", d=64),
                                                                                 axis=AX.X, op=ALU.add),
                                 reads=[R_sqq], writes=[R_st10])
                            P.op('act', lambda e, h0=h0: e.activation(out=st10[:, h0:10], in_=st10[:, h0:10], func=AF.Sqrt, scale=1.0 / 64, bias=ntmp['eps'][:, 0:1]),
                                 reads=[R_st10], writes=[R_st10])
                            P.op('dve', lambda e, h0=h0: e.reciprocal(st10[:, h0:10], st10[:, h0:10]), reads=[R_st10], writes=[R_st10])
                            P.op('dve', lambda e, h0=h0, nh=nh, c0=c0: e.tensor_tensor(
                                qn[:, h0:10, :], qkv[:, c0:640].rearrange("p (h d) -> p h d", d=64),
                                st10[:, h0:10].unsqueeze(2).to_broadcast([128, nh, 64]), op=ALU.mult),
                                reads=[R_qkv, R_st10], writes=[R_qn])
                            if A1C == 3: break
                            if main:
                                P.op('pool', lambda e: e.tensor_tensor(qn[:], qn[:], G10[:], op=ALU.mult), reads=[R_qn, R_G10], writes=[R_qn])
                                cb_ = lambda i=i: cos_t[:, i, :].unsqueeze(1).to_broadcast([128, 10, 32])
                                sb_ = lambda i=i: sin_t[:, i, :].unsqueeze(1).to_broadcast([128, 10, 32])
                                P.op('dve', lambda e, cb_=cb_: e.tensor_tensor(ra[:], qn[:, :, 0:32], cb_(), op=ALU.mult), reads=[R_qn, R_cs], writes=[R_ra])
                                P.op('pool', lambda e, sb_=sb_: e.tensor_tensor(rbb[:], qn[:, :, 32:64], sb_(), op=ALU.mult), reads=[R_qn, R_cs], writes=[R_rbb])
                                qperm = lambda lo, hi: qr[:, 0:8, lo:hi].rearrange("p (g kv) d -> p kv g d", kv=2)
                                v4 = lambda t: t[:, 0:8, :].rearrange("p (kv g) d -> p kv g d", kv=2)
                                P.op('dve', lambda e, qperm=qperm, v4=v4: e.tensor_tensor(qperm(0, 32), v4(ra), v4(rbb), op=ALU.subtract), reads=[R_ra, R_rbb], writes=[R_qr])
                                P.op('dve', lambda e: e.tensor_tensor(qr[:, 8:10, 0:32], ra[:, 8:10, :], rbb[:, 8:10, :], op=ALU.subtract), reads=[R_ra, R_rbb], writes=[R_qr])
                                P.op('dve', lambda e, cb_=cb_: e.tensor_tensor(ra[:], qn[:, :, 32:64], cb_(), op=ALU.mult), reads=[R_qn, R_cs], writes=[R_ra])
                                P.op('pool', lambda e, sb_=sb_: e.tensor_tensor(rbb[:], qn[:, :, 0:32], sb_(), op=ALU.mult), reads=[R_qn, R_cs], writes=[R_rbb])
                                P.op('dve', lambda e, qperm=qperm, v4=v4: e.tensor_tensor(qperm(32, 64), v4(ra), v4(rbb), op=ALU.add), reads=[R_ra, R_rbb], writes=[R_qr])
                                P.op('dve', lambda e: e.tensor_tensor(qr[:, 8:10, 32:64], ra[:, 8:10, :], rbb[:, 8:10, :], op=ALU.add), reads=[R_ra, R_rbb], writes=[R_qr])
                                if A1C == 4: break
                                for g in range(4):
                                    P.op('pe', lambda e, g=g: e.transpose(
                                        out=PSt[:, g * 128:(g + 1) * 128],
                                        in_=qr[:, 2 * g:2 * g + 2, :], identity=ident_f),
                                        reads=[R_qr, R_consts], writes=[R_pst])
                                P.op('pe', lambda e: e.transpose(out=PSk[:, 384:512], in_=qr[:, 8:10, :], identity=ident_f),
                                     reads=[R_qr, R_consts], writes=[R_psk])
                                P.op('act', lambda e, i=i: e.copy(out=qT[:, i, :], in_=PSt[:, 0:512]),
                                     reads=[R_pst], writes=[R_qT[i]])
                                P.op('dve', lambda e, tokc=tokc: e.tensor_copy(kT[:, tokc], PSk[:, 384:512]), reads=[R_psk], writes=[R_kT[i]])
                            else:
                                P.op('pool', lambda e: e.tensor_tensor(qr[:, 8:10, :], qn[:, 8:10, :], G10[:, 8:10, :], op=ALU.mult),
                                     reads=[R_qn, R_G10], writes=[R_qr])
                                P.op('pe', lambda e: e.transpose(out=PSk[:, 384:512], in_=qr[:, 8:10, :], identity=ident_f),
                                     reads=[R_qr, R_consts], writes=[R_psk])
                                P.op('dve', lambda e, tokc=tokc: e.tensor_copy(kT[:, tokc], PSk[:, 384:512]), reads=[R_psk], writes=[R_kT[i]])
                    dump('u', u_bf[:], sum(R_u, []))
                    dump('qT', qT[:].rearrange('p i f -> p (i f)'), R_qT); dump('kT', kT[:], R_kT); dump('v', vtok[:], R_v)
                    P.flush()
                if DEBUG.get('sub', 9) <= 1: return
                with ExitStack() as pb:
                    chT = SB("chT", [128, 4, S], BF16, pb); R_ch = [[Res() for _ in range(4)] for _ in range(4)]
                    atT = SB("atT", [128, 4, S], BF16, pb); R_at = [Res() for _ in range(16)]
                    with ExitStack() as ph:
                        acc = SB("acc", [128, 4, 512], F32, ph); R_acc = [Res() for _ in range(4)]
                        sq4 = SB("sq4", [128, 4, 512], F32, ph); R_sq4 = [Res() for _ in range(4)]
                        mean = SB("mean", [128, 512], F32, ph); R_mean = Res()
                        var = SB("var", [128, 512], F32, ph); R_var = Res()
                        m2 = SB("m2", [128, 512], F32, ph); R_m2 = Res()
                        tl = [SB(f"tl{i}", [128, 512], F32, ph) for i in range(2)]; R_tl = [Res() for _ in range(2)]
                        epsb = SB("epsb", [128, 1], F32, ph); R_epsb = Res()
                        Eb = [SB(f"Eb{i}", [128, 512], BF16, ph) for i in range(6)]; R_Eb = [Res() for _ in range(6)]
                        den = SB("den", [128, 512], F32, ph); R_den = Res()
                        PSm = ph.enter_context(nc.psum_tensor(f"L{P.phase}_a2psm", [128, 512], F32)); R_psm = Res()
                        PSv = ph.enter_context(nc.psum_tensor(f"L{P.phase}_a2psv", [128, 512], F32)); R_psv = Res()
                        PSs2 = [ph.enter_context(nc.psum_tensor(f"L{P.phase}_a2pss{i}", [128, 512], F32)) for i in range(2)]; R_pss2 = [Res() for _ in range(2)]
                        PSo2 = [ph.enter_context(nc.psum_tensor(f"L{P.phase}_a2pso{i}", [128, 512], F32)) for i in range(2)]; R_pso2 = [Res() for _ in range(2)]
                        PSd2 = [ph.enter_context(nc.psum_tensor(f"L{P.phase}_a2psd{i}", [128, 512], F32)) for i in range(2)]; R_psd2 = [Res() for _ in range(2)]
                        P.op('dve', lambda e: e.memset(epsb[:], EPS), writes=[R_epsb])
                        jobs = []
                        for i in range(16):
                            blocks = [(jb, m_) for jb, m_ in ((i - 1, 0), (i, None), (i + 1, 1)) if 0 <= jb < 16] + [(16, None), (17, None)]
                            for kv in range(2):
                                for bi_, (jb, m_) in enumerate(blocks):
                                    jobs.append(dict(i=i, kv=kv, jb=jb, m=m_, first=(bi_ == 0), last=(bi_ == len(blocks) - 1), n=len(jobs)))
                        def job_scores(J):
                            i, kv, jb = J['i'], J['kv'], J['jb']; si = J['n'] % 2
                            pr = slice(kv * 64, (kv + 1) * 64)
                            P.op('pe', lambda e: e.matmul(PSs2[si][:], kT[pr, jb * 128:(jb + 1) * 128], qT[pr, i, :], start=True, stop=True),
                                 reads=[R_kT[jb], R_qT[i]], writes=[R_pss2[si]])
                        def job_rest(J):
                            i, kv, jb, m_ = J['i'], J['kv'], J['jb'], J['m']; si = J['n'] % 2; ei = J['n'] % 6; pi = i % 2
                            pr = slice(kv * 64, (kv + 1) * 64)
                            first, last = J['first'], J['last']
                            P.op('act', lambda e: e.activation(out=Eb[ei][:], in_=PSs2[si][:], func=AF.Exp, scale=0.125),
                                 reads=[R_pss2[si]], writes=[R_Eb[ei]])
                            if m_ is not None:
                                P.op('pool', lambda e: e.tensor_tensor(
                                    Eb[ei][:].rearrange("p (g t) -> p g t", g=4), Eb[ei][:].rearrange("p (g t) -> p g t", g=4),
                                    maskb[:, m_ * 128:(m_ + 1) * 128].unsqueeze(1).to_broadcast([128, 4, 128]), op=ALU.mult),
                                    reads=[R_Eb[ei], R_k], writes=[R_Eb[ei]])
                            P.op('pe', lambda e: e.matmul(PSo2[pi][pr, :], vtok[:, jb, kv * 64:(kv + 1) * 64], Eb[ei][:], start=first, stop=last),
                                 reads=[R_v[jb], R_Eb[ei]], writes=[R_pso2[pi]])
                            P.op('pe', lambda e: e.matmul(PSd2[pi][pr, :], ones_bf[:, 0:64], Eb[ei][:], start=first, stop=last),
                                 reads=[R_k, R_Eb[ei]], writes=[R_psd2[pi]])
                            if last and kv == 1:
                                tokq = slice(i * 128, (i + 1) * 128)
                                P.op('dve', lambda e: e.tensor_tensor(den[:].rearrange("p (g t) -> p g t", g=4), PSd2[pi][:].rearrange("p (g t) -> p g t", g=4),
                                                                       es_t[:].unsqueeze(2).to_broadcast([128, 4, 128]), op=ALU.add),
                                     reads=[R_psd2[pi], R_es], writes=[R_den])
                                P.op('dve', lambda e: e.reciprocal(den[:], den[:]), reads=[R_den], writes=[R_den])
                                P.op('dve', lambda e: e.tensor_tensor(atT[:, :, tokq], PSo2[pi][:].rearrange("p (g t) -> p g t", g=4),
                                                                      den[:].rearrange("p (g t) -> p g t", g=4), op=ALU.mult),
                                     reads=[R_pso2[pi], R_den], writes=[R_at[i]])
                                return True
                            return False

                        ptmp = SB("ptmp", [128, 512], F32, ph); R_ptmp = Res()
                        def conv_taps(tt):
                            ops = []
                            for tap in range(31):
                                for c in range(4):
                                    src = u_bf[:, c, tt * 512 + tap: tt * 512 + tap + 512]
                                    wcol = V('conv_w', c * 31 + tap, 1)
                                    rd = [R_u[c][t2] for t2 in range(max(0, tt - 1), min(4, tt + 2))] + [R_upad, R_vecs]
                                    eng = 'dve'
                                    if tap == 0:
                                        ops.append(lambda eng=eng, c=c, src=src, wcol=wcol, rd=rd: P.op(eng, lambda e: e.tensor_scalar(
                                            acc[:, c, :], src, wcol, V('conv_b', c, 1), op0=ALU.mult, op1=ALU.add),
                                            reads=rd, writes=[R_acc[c]]))
                                    elif True:
                                        ops.append(lambda c=c, src=src, wcol=wcol, rd=rd: P.op('dve', lambda e: e.scalar_tensor_tensor(
                                            out=acc[:, c, :], in0=src, scalar=wcol, in1=acc[:, c, :], op0=ALU.mult, op1=ALU.add),
                                            reads=rd + [R_acc[c]], writes=[R_acc[c]]))
                                    else:
                                        def two(c=c, src=src, wcol=wcol, rd=rd):
                                            P.op('pool', lambda e: e.tensor_scalar(ptmp[:], src, wcol, None, op0=ALU.mult), reads=rd, writes=[R_ptmp])
                                            P.op('pool', lambda e: e.tensor_tensor(acc[:, c, :], acc[:, c, :], ptmp[:], op=ALU.add), reads=[R_ptmp, R_acc[c]], writes=[R_acc[c]])
                                        ops.append(two)
                            return ops

                        def conv_tile(tt):
                            for c in range(4):
                                P.op('act', lambda e, c=c: e.activation(out=sq4[:, c, :], in_=acc[:, c, :], func=AF.Square),
                                     reads=[R_acc[c]], writes=[R_sq4[c]])
                            for c in range(4):
                                P.op('pe', lambda e, c=c: e.matmul(PSm[:], ones_f[:], acc[:, c, :], start=(c == 0), stop=(c == 3)),
                                     reads=[R_acc[c], R_k], writes=[R_psm])
                            for c in range(4):
                                P.op('pe', lambda e, c=c: e.matmul(PSv[:], ones_f[:], sq4[:, c, :], start=(c == 0), stop=(c == 3)),
                                     reads=[R_sq4[c], R_k], writes=[R_psv])
                            P.op('act', lambda e: e.activation(out=mean[:], in_=PSm[:], func=AF.Identity, scale=1.0 / 512), reads=[R_psm], writes=[R_mean])
                            P.op('pool', lambda e: e.tensor_tensor(m2[:], mean[:], mean[:], op=ALU.mult), reads=[R_mean], writes=[R_m2])
                            P.op('dve', lambda e: e.scalar_tensor_tensor(out=var[:], in0=PSv[:], scalar=1.0 / 512, in1=m2[:], op0=ALU.mult, op1=ALU.subtract),
                                 reads=[R_psv, R_m2], writes=[R_var])
                            P.op('act', lambda e: e.activation(out=var[:], in_=var[:], func=AF.Sqrt, bias=epsb[:, 0:1], scale=1.0), reads=[R_var, R_epsb], writes=[R_var])
                            P.op('dve', lambda e: e.reciprocal(var[:], var[:]), reads=[R_var], writes=[R_var])
                            for c in range(4):
                                ti = c % 2
                                P.op('dve', lambda e, c=c, ti=ti: e.tensor_tensor(tl[ti][:], acc[:, c, :], mean[:], op=ALU.subtract),
                                     reads=[R_acc[c], R_mean], writes=[R_tl[ti]])
                                P.op('pool', lambda e, ti=ti: e.tensor_tensor(tl[ti][:], tl[ti][:], var[:], op=ALU.mult), reads=[R_tl[ti], R_var], writes=[R_tl[ti]])
                                P.op('act', lambda e, c=c, ti=ti, tt=tt: e.activation(out=chT[:, c, tt * 512:(tt + 1) * 512], in_=tl[ti][:], func=AF.Silu,
                                                                                      scale=V('cln_g', c, 1), bias=V('cln_b', c, 1)),
                                     reads=[R_tl[ti], R_vecs], writes=[R_ch[c][tt]])
                        conv_q = []
                        for tt in range(4):
                            ops = conv_taps(tt)
                            per = (len(ops) + 3) // 4
                            for qi in range(4):
                                conv_q.append((tt, qi, ops[qi * per:(qi + 1) * per]))
                        def run_conv(item):
                            tt, qi, ops = item
                            for o in ops: o()
                            if qi == 3: conv_tile(tt)
                        job_scores(jobs[0])
                        done_tiles = 0
                        for n in range(len(jobs)):
                            if n + 1 < len(jobs): job_scores(jobs[n + 1])
                            if job_rest(jobs[n]):
                                run_conv(conv_q[done_tiles]); done_tiles += 1
                        dump('chT', chT[:], sum(R_ch, [])); dump('atT', atT[:], R_at)
                        P.flush()
                    if DEBUG.get('sub', 9) <= 2: return
                    with ExitStack() as ph:
                        w_out = SB("w_out", [128, 8, D], BF16, ph); R_wo = [Res() for _ in range(3)]
                        tmp = [SB(f"a3t{i}", [128, 512], F32, ph) for i in range(2)]; R_tmp = [Res() for _ in range(2)]
                        PSo3 = [ph.enter_context(nc.psum_tensor(f"L{P.phase}_a3ps{i}", [128, 512], F32)) for i in range(2)]; R_pso3 = [Res() for _ in range(2)]
                        P.dma('pool', w_out[:, 0:4, :], ev_w_out[0:512, :].rearrange("(k p) n -> p k n", p=128), wB, writes=[R_wo[0]])
                        P.dma('pool', w_out[0:64, 4:8, :], ev_w_out[512:768, :].rearrange("(g d) n -> d g n", d=64), wB, writes=[R_wo[1]])
                        P.dma('pool', w_out[64:128, 4:8, :], ev_w_out[768:1024, :].rearrange("(g d) n -> d g n", d=64), wB, writes=[R_wo[2]])
                        n3 = 0
                        for tt in range(4):
                            tok = slice(tt * 512, (tt + 1) * 512)
                            for m in range(8):
                                oi = n3 % 2; n3 += 1
                                for c in range(4):
                                    P.op('pe', lambda e, c=c, m=m, oi=oi, tok=tok: e.matmul(PSo3[oi][:], w_out[:, c, m * 128:(m + 1) * 128], chT[:, c, tok], start=(c == 0), stop=False),
                                         reads=R_wo + [R_ch[c][tt]], writes=[R_pso3[oi]])
                                for g in range(4):
                                    P.op('pe', lambda e, g=g, m=m, oi=oi, tok=tok: e.matmul(PSo3[oi][:], w_out[:, 4 + g, m * 128:(m + 1) * 128], atT[:, g, tok], start=False, stop=(g == 3)),
                                         reads=R_wo + [R_at[tt * 4 + q_] for q_ in range(4)], writes=[R_pso3[oi]])
                                P.op('act', lambda e, m=m, oi=oi: e.activation(out=tmp[oi][:], in_=PSo3[oi][:], func=AF.Identity, scale=mod[:, 0, 16 + m, 0:1], bias=der[:, 0, 2, m, 0:1]),
                                     reads=[R_pso3[oi], R_mod, R_der], writes=[R_tmp[oi]])
                                P.op('dve', lambda e, m=m, oi=oi, tok=tok: e.tensor_tensor(hT[:, m, tok], hT[:, m, tok], tmp[oi][:], op=ALU.add),
                                     reads=[R_tmp[oi], R_h[m][tt]], writes=[R_h[m][tt]])
                        dump('h_mix0', hT[:], sum(R_h, []))
                        P.flush()

        def mixer1():
            with ExitStack() as pa:
                uT = SB("uT", [128, 8, S], BF16, pa); R_uT = [[Res() for _ in range(4)] for _ in range(8)]
                vt = SB("vt", [128, 16, D], BF16, pa); R_vt = [Res() for _ in range(16)]
                wsT = SB("wsT", [128, 8, 128], BF16, pa); R_ws = Res()
                with ExitStack() as ph:
                    wsf = SB("wsf", [128, 8, 128], F32, ph); R_wsf = Res()
                    PSt = [ph.enter_context(nc.psum_tensor(f"L{P.phase}_b0pst{i}", [128, 512], F32)) for i in range(2)]; R_pst = [Res(), Res()]
                    P.dma('sp', wsf[:], od_w_s.rearrange("g p q -> p g q"), cs_ld, writes=[R_wsf])
                    for g in range(8):
                        P.op('pe', lambda e, g=g: e.transpose(out=PSt[g // 4][:, (g % 4) * 128:(g % 4 + 1) * 128], in_=wsf[:, g, :], identity=ident_f),
                             reads=[R_wsf, R_consts], writes=[R_pst[g // 4]])
                    for hh in range(2):
                        P.op('act', lambda e, hh=hh: e.copy(out=wsT[:, hh * 4:(hh + 1) * 4, :].rearrange("p g q -> p (g q)"), in_=PSt[hh][:]), reads=[R_pst[hh]], writes=[R_ws])
                    P.flush()
                with ExitStack() as ph:
                    hm = [SB(f"hm1_{i}", [128, 8, 512], BF16, ph) for i in range(2)]; R_hm = [[Res() for _ in range(8)] for _ in range(2)]
                    w_in = SB("w_in1", [128, 8, D], BF16, ph); R_win = Res()
                    rows1 = SB("rows1", [128, 3 * D], F32, ph); R_rows1 = Res()
                    ntmp = mk_norm_tmp(ph, "b1")
                    vz = SB("vz", [128, D], F32, ph); R_vz = Res()
                    bst = SB("bst", [128, 2, 6], F32, ph); R_bst = Res()
                    mv = SB("mv", [128, 2], F32, ph); R_mv = Res()
                    PSs = ph.enter_context(nc.psum_tensor(f"L{P.phase}_b1pss", [128, 512], F32)); R_pss = Res()
                    PSa = [ph.enter_context(nc.psum_tensor(f"L{P.phase}_b1psa{i}", [128, 512], F32)) for i in range(2)]; R_psa = [Res() for _ in range(2)]
                    PSv = [ph.enter_context(nc.psum_tensor(f"L{P.phase}_b1psv{i}", [128, 512], F32)) for i in range(4)]; R_psv = [Res() for _ in range(4)]
                    P.dma('sp', rows1[:], rows1_d[0:1, 0:3 * D].partition_broadcast(128), cs_ld, writes=[R_rows1])
                    for pas in range(2):
                        P.dma('pool', w_in[:], od_w_in[:, pas * D:(pas + 1) * D].rearrange("(k p) n -> p k n", p=128), wA, writes=[R_win])
                        na = 0
                        for tt in range(4):
                            hb = tt % 2; hmt = hm[hb]
                            tok = slice(tt * 512, (tt + 1) * 512)
                            norm_mod(ph, lambda k, tok=tok: hT[:, k, tok], 512, lambda k: der[:, 1, 0, k, 0:1], lambda k: mod[:, 1, k, 0:1],
                                     lambda k, hmt=hmt: hmt[:, k, :], [R_h[k][tt] for k in range(8)], R_hm[hb], PSs, R_pss, ntmp)
                            if pas == 0 and tt == 0: dump('hm1', hmt[:], R_hm[hb])
                            if pas == 0:
                                for c in range(8):
                                    pi = na % 2; na += 1
                                    for k in range(8):
                                        P.op('pe', lambda e, k=k, c=c, pi=pi, hmt=hmt: e.matmul(PSa[pi][:], w_in[:, k, c * 128:(c + 1) * 128], hmt[:, k, :], start=(k == 0), stop=(k == 7)),
                                             reads=[R_win, R_hm[hb][k]], writes=[R_psa[pi]])
                                    P.op('act', lambda e, c=c, pi=pi, tok=tok: e.activation(out=uT[:, c, tok], in_=PSa[pi][:], func=AF.Gelu, bias=V('od_bin_u', c, 1), scale=1.0),
                                         reads=[R_psa[pi], R_vecs], writes=[R_uT[c][tt]])
                            else:
                                for sub in range(4):
                                    i = tt * 4 + sub
                                    lc = slice(sub * 128, (sub + 1) * 128)
                                    for hh in range(2):
                                        pv = (i * 2 + hh) % 4
                                        for k in range(8):
                                            P.op('pe', lambda e, k=k, hh=hh, pv=pv, lc=lc, hmt=hmt: e.matmul(PSv[pv][:], hmt[:, k, lc], w_in[:, k, hh * 512:(hh + 1) * 512], start=(k == 0), stop=(k == 7)),
                                                 reads=[R_win, R_hm[hb][k]], writes=[R_psv[pv]])
                                        P.op('dve', lambda e, hh=hh, pv=pv: e.tensor_tensor(vz[:, hh * 512:(hh + 1) * 512], PSv[pv][:], rows1[:, hh * 512:(hh + 1) * 512], op=ALU.add),
                                             reads=[R_psv[pv], R_rows1], writes=[R_vz])
                                    P.op('act', lambda e: e.activation(out=vz[:], in_=vz[:], func=AF.Gelu), reads=[R_vz], writes=[R_vz])
                                    for hh in range(2):
                                        P.op('dve', lambda e, hh=hh: e.bn_stats(out=bst[:, hh, :], in_=vz[:, hh * 512:(hh + 1) * 512]), reads=[R_vz], writes=[R_bst])
                                    P.op('dve', lambda e: e.bn_aggr(out=mv[:], in_=bst[:]), reads=[R_bst], writes=[R_mv])
                                    P.op('act', lambda e: e.activation(out=mv[:, 1:2], in_=mv[:, 1:2], func=AF.Sqrt, bias=ntmp['eps'][:, 0:1], scale=1.0), reads=[R_mv], writes=[R_mv])
                                    P.op('dve', lambda e: e.reciprocal(mv[:, 1:2], mv[:, 1:2]), reads=[R_mv], writes=[R_mv])
                                    P.op('dve', lambda e: e.tensor_scalar(vz[:], vz[:], mv[:, 0:1], mv[:, 1:2], op0=ALU.subtract, op1=ALU.mult),
                                         reads=[R_vz, R_mv], writes=[R_vz])
                                    P.op('pool', lambda e: e.tensor_tensor(vz[:], vz[:], rows1[:, 1024:2048], op=ALU.mult), reads=[R_vz, R_rows1], writes=[R_vz])
                                    P.op('pool', lambda e, i=i: e.tensor_tensor(vt[:, i, :], vz[:], rows1[:, 2048:3072], op=ALU.add), reads=[R_vz, R_rows1], writes=[R_vt[i]])
                    dump('uT', uT[:], sum(R_uT, [])); dump('vt', vt[:], R_vt)
                    P.flush()
                with ExitStack() as ph:
                    w_out = SB("w_out1", [128, 8, D], BF16, ph); R_wo = Res()
                    bsr = SB("bsr", [128, D], F32, ph); R_bsr = Res()
                    tmp = [SB(f"b3t{i}", [128, 512], F32, ph) for i in range(2)]; R_tmp = [Res() for _ in range(2)]
                    svb = [SB(f"svb{i}", [128, 128], F32, ph) for i in range(2)]; R_svb = [Res() for _ in range(2)]
                    PSg_ = [ph.enter_context(nc.psum_tensor(f"L{P.phase}_b2ps{i}", [128, 512], F32)) for i in range(2)]; R_psg_ = [Res() for _ in range(2)]
                    PSo3 = [ph.enter_context(nc.psum_tensor(f"L{P.phase}_b3ps{i}", [128, 512], F32)) for i in range(2)]; R_pso3 = [Res() for _ in range(2)]
                    P.dma('pool', w_out[:], od_w_out.rearrange("(k p) n -> p k n", p=128), wB, writes=[R_wo])
                    P.dma('sp', bsr[:], rows1_d[0:1, 3 * D:4 * D].partition_broadcast(128), cs_ld, writes=[R_bsr])
                    n2 = 0
                    for i in range(16):
                        tokc = slice(i * 128, (i + 1) * 128); tt = i // 4
                        for g in range(8):
                            pi = n2 % 2; n2 += 1
                            P.op('pe', lambda e, g=g, i=i, pi=pi: e.matmul(PSg_[pi][:, 0:128], vt[:, i, g * 128:(g + 1) * 128], wsT[:, g, :], start=True, stop=True),
                                 reads=[R_vt[i], R_ws], writes=[R_psg_[pi]])
                            P.op('dve', lambda e, g=g, pi=pi: e.tensor_tensor(svb[pi][:], PSg_[pi][:, 0:128], bsr[:, g * 128:(g + 1) * 128], op=ALU.add),
                                 reads=[R_psg_[pi], R_bsr], writes=[R_svb[pi]])
                            P.op('pool', lambda e, g=g, pi=pi, tokc=tokc: e.tensor_tensor(uT[:, g, tokc], uT[:, g, tokc], svb[pi][:], op=ALU.mult),
                                 reads=[R_svb[pi], R_uT[g][tt]], writes=[R_uT[g][tt]])
                    dump('prod', uT[:], sum(R_uT, []))
                    n3 = 0
                    for tt in range(4):
                        tok = slice(tt * 512, (tt + 1) * 512)
                        for m in range(8):
                            oi = n3 % 2; n3 += 1
                            for c in range(8):
                                P.op('pe', lambda e, c=c, m=m, oi=oi, tok=tok: e.matmul(PSo3[oi][:], w_out[:, c, m * 128:(m + 1) * 128], uT[:, c, tok], start=(c == 0), stop=(c == 7)),
                                     reads=[R_wo, R_uT[c][tt]], writes=[R_pso3[oi]])
                            P.op('act', lambda e, m=m, oi=oi: e.activation(out=tmp[oi][:], in_=PSo3[oi][:], func=AF.Identity, scale=mod[:, 1, 16 + m, 0:1], bias=der[:, 1, 2, m, 0:1]),
                                 reads=[R_pso3[oi], R_mod, R_der], writes=[R_tmp[oi]])
                            P.op('dve', lambda e, m=m, oi=oi, tok=tok: e.tensor_tensor(hT[:, m, tok], hT[:, m, tok], tmp[oi][:], op=ALU.add),
                                 reads=[R_tmp[oi], R_h[m][tt]], writes=[R_h[m][tt]])
                    dump('h_mix1', hT[:], sum(R_h, []))
                    P.flush()

        stages = dbg.get('_stages', None)
        stop_after = DEBUG.get('stop_after', 99)
        if stop_after >= 1 and DEBUG.get('sub', 9) >= 1: mixer0()
        if stop_after >= 2: moe_phase(0)
        if stop_after >= 3: mixer1()
        if stop_after >= 4: moe_phase(1)
        for k in range(8):
            P.dma('sp', outT[k * 128:(k + 1) * 128, :], hT[:, k, :], out_st, reads=R_h[k])
        P.flush()
        P.final_wait()
    return nc


def _rope_tables():
    rows = S // 64
    r = np.repeat(np.arange(rows), 64).astype(np.float32)
    col = np.tile(np.arange(64), rows).astype(np.float32)
    inv_freq = (np.float32(10000.0) ** (-np.arange(16, dtype=np.float32) / np.float32(16))).astype(np.float32)
    ang = np.concatenate([r[:, None] * inv_freq, col[:, None] * inv_freq], axis=-1).astype(np.float32)
    return np.cos(ang).astype(np.float32), np.sin(ang).astype(np.float32)


def _consts():
    c = np.zeros((128, NCONST), np.float32)
    c[:, 0:128] = np.eye(128, dtype=np.float32)
    j = np.arange(128)[:, None]; i = np.arange(128)[None, :]
    c[:, 128:256] = (j >= i).astype(np.float32)
    c[:, 256:384] = (j <= i).astype(np.float32)
    return c


def _cossin():
    c = np.zeros((128, NCS), np.float32)
    cos, sin = _rope_tables()
    c[:, 0:512] = cos.reshape(16, 128, 32).transpose(1, 0, 2).reshape(128, 512)
    c[:, 512:1024] = sin.reshape(16, 128, 32).transpose(1, 0, 2).reshape(128, 512)
    return c


def _cols(v):
    v = np.asarray(v, np.float32).reshape(-1, 128)
    return v.T


def _build_vecs(inp, b):
    f = lambda a: np.asarray(a, np.float32)
    parts = []
    sc = np.stack([_cols(f(inp['c'])[b]), _cols(f(inp['c_ctx']))], axis=2).reshape(128, 16)
    parts.append(sc)
    parts.append(_cols(f(inp['ada_b'])))
    parts.append(_cols(f(inp['norm_mix_g']))); parts.append(_cols(f(inp['norm_ffn_g'])))
    parts.append(_cols(f(inp['ev_b_in'])[0, :1024]))
    cw = f(inp['ev_conv_w'])[0]
    parts.append(cw.reshape(31, 4, 128).transpose(2, 1, 0).reshape(128, 124))
    parts.append(_cols(f(inp['ev_conv_b'])[0])); parts.append(_cols(f(inp['ev_conv_ln_g'])[0])); parts.append(_cols(f(inp['ev_conv_ln_b'])[0]))
    parts.append(_cols(f(inp['ev_b_out'])[0]))
    parts.append(_cols(f(inp['od_b_in'])[0, :1024]))
    parts.append(_cols(f(inp['od_b_out'])[0]))
    v = np.concatenate(parts, axis=1)
    assert v.shape == (128, NV), v.shape
    return np.ascontiguousarray(v)


_NC_CACHE = {}

def kernel(**inp):
    f = lambda a: np.ascontiguousarray(np.asarray(a, np.float32))
    key = (tuple(sorted(DEBUG.get('dbg', {}).items())), DEBUG.get('stop_after', 99), DEBUG.get('sub', 9), DEBUG.get('n_exp', 32), DEBUG.get('a1', 99))
    if key not in _NC_CACHE:
        _NC_CACHE[key] = build_program(DEBUG.get('dbg'))
    nc = _NC_CACHE[key]
    n = DEBUG.get('n_cores', 8)
    consts = _consts()
    x = f(inp['x']); ctx = f(inp['ctx'])
    rows0 = np.concatenate([f(inp['ev_b_in'])[0, 1024:], f(inp['ev_q_norm_g'])[0], f(inp['ev_k_norm_g'])[0], f(inp['ev_sink'])[0]])[None, :]
    rows1 = np.concatenate([f(inp['od_b_in'])[0, 1024:], f(inp['od_v_ln_g'])[0], f(inp['od_v_ln_b'])[0], f(inp['od_b_s'])[0].reshape(-1)])[None, :]
    rowsm = f(inp['moe_router_b']).reshape(1, 64)
    b1 = f(inp['moe_b1'])
    b1cols = np.ascontiguousarray(np.stack([_cols(b1[l]) for l in range(2)], axis=0))
    shared = dict(consts=consts, cossin=_cossin(), b1cols=b1cols, rows0=np.ascontiguousarray(rows0), rows1=np.ascontiguousarray(rows1), rowsm=rowsm,
                  ada_w=f(inp['ada_w']), ev_w_in=f(inp['ev_w_in'])[0], ev_w_out=f(inp['ev_w_out'])[0],
                  od_w_in=f(inp['od_w_in'])[0], od_w_s=f(inp['od_w_s'])[0], od_w_out=f(inp['od_w_out'])[0],
                  r_w=f(inp['moe_router_w']), w1=f(inp['moe_w1']), w2=f(inp['moe_w2']), b2=f(inp['moe_b2']))
    in_maps = []
    for b in range(n):
        m = dict(shared)
        m['xT'] = np.ascontiguousarray(x[b].T); m['ctxT'] = np.ascontiguousarray(ctx[b].T)
        m['vecs'] = _build_vecs(inp, b)
        in_maps.append(m)
    res = run_bass_kernel_spmd(nc, in_maps, core_ids=list(range(n)))
    DEBUG['last'] = res.results
    out = np.stack([np.ascontiguousarray(res.results[b]['outT'].T) for b in range(n)], axis=0)
    return out.astype(np.float32)
```

```python
import numpy as np
import ml_dtypes
import concourse.bass as bass
import concourse.mybir as mybir
from concourse.bass_utils import run_bass_kernel_spmd
from contextlib import ExitStack

F32, BF16 = mybir.dt.float32, mybir.dt.bfloat16
AF = mybir.ActivationFunctionType
ALU = mybir.AluOpType
AX = mybir.AxisListType

D = 1024; S = 2048; L = 256; NE = 32; EPS = 1e-6
DEBUG = {}

VOFF = {}
def _mk_voff():
    o = 0
    for name, n in [('sc_in', 16), ('ada_b', 96), ('nmg', 16), ('nfg', 16), ('ev_bin_a', 8),
                    ('conv_w', 124), ('conv_b', 4), ('cln_g', 4), ('cln_b', 4), ('ev_bout', 8),
                    ('od_bin_u', 8), ('od_bout', 8)]:
        VOFF[name] = o; o += n
    return o
NV = _mk_voff()
NCONST = 128 + 256
NCS = 1024
NROW0 = 768 + 64 + 64 + 8
NROW1 = 4096


class Res:
    __slots__ = ('name', 'w', 'r')
    def __init__(self, name=''):
        self.name = name; self.w = {}; self.r = {}

class DSem:
    def __init__(self, h):
        self.h = h; self.count = 0

class Ins:
    __slots__ = ('eng', 'fn', 'deps', 'seq', 'need', 'dsem', 'dord', 'phase')

class Prog:
    CE = ('act', 'dve', 'pool', 'pe')
    def __init__(self, nc, es):
        self.nc = nc
        self.esem = {e: es.enter_context(nc.semaphore('s_' + e)) for e in self.CE}
        self.seq = {e: 0 for e in self.CE}
        self.waited = {x: {} for x in self.CE + ('sp',)}
        self.dsems = []
        self.es = es
        self.cur = {x: [] for x in self.CE + ('sp',)}
        self.phase = 0
        self.bar = None
        self.n_ins = 0
        self.dpool = {}; self.dpi = {}

    def dsem(self, name):
        s = DSem(self.es.enter_context(self.nc.semaphore(name)))
        self.dsems.append(s)
        return s

    def op(self, eng, fn, reads=(), writes=(), dsem=None):
        I = Ins(); I.eng = eng; I.fn = fn; I.seq = 0; I.need = False; I.dsem = dsem; I.phase = self.phase
        key = dsem if dsem is not None else eng
        if dsem is not None:
            dsem.count += 1; I.dord = dsem.count
        raw = set()
        deps = set()
        for r in reads:
            for d in r.w.values():
                deps.add(d); raw.add(d)
        for w in writes:
            for d in w.w.values(): deps.add(d)
            for d in w.r.values(): deps.add(d)
        out = []
        for d in deps:
            if d.phase != self.phase: continue
            if d.dsem is None and d.eng == eng and dsem is None:
                if eng == 'pe': continue
            out.append(d)
            if d.dsem is None: d.need = True
        I.deps = out
        for r in reads: r.r[key] = I
        for w in writes: w.w[key] = I
        self.cur[eng].append(I)
        self.n_ins += 1
        return I

    def dma(self, eng, out, in_, dsem=None, reads=(), writes=()):
        auto = dsem is None or getattr(dsem, 'auto', True)
        if auto:
            pool = self.dpool.setdefault(eng, [])
            if not pool:
                for i in range(12 if eng == 'sp' else 8):
                    d = self.dsem(f"dp_{eng}{i}"); d.last = None; pool.append(d)
                self.dpi[eng] = 0
            dsem = pool[self.dpi[eng] % len(pool)]; self.dpi[eng] += 1
        I = self.op(eng, lambda e, o=out, i=in_: e.dma_start(out=o, in_=i), reads, writes, dsem=dsem)
        if auto:
            if dsem.last is not None and dsem.last.phase == self.phase and dsem.last not in I.deps:
                I.deps.append(dsem.last)
            dsem.last = I
        return I

    def flush(self):
        nc = self.nc
        for e in self.CE:
            for I in reversed(self.cur[e]):
                if I.dsem is None:
                    I.need = True; break
        for e in self.CE:
            for I in self.cur[e]:
                if I.need and I.dsem is None:
                    self.seq[e] += 1; I.seq = self.seq[e]
        bar_prev = self.bar
        with nc.Block() as block:
            def emit(X, engobj):
                wt = self.waited[X]
                def wait(key, sem, val):
                    if wt.get(key, 0) < val:
                        engobj.wait_ge(sem, val); wt[key] = val
                if bar_prev is not None:
                    for e, v in bar_prev['seq'].items():
                        if v > 0: wait(e, self.esem[e], v)
                    for s, c in bar_prev['dma']:
                        if c > 0: wait(s, s.h, 16 * c)
                for I in self.cur[X]:
                    for d in I.deps:
                        if d.dsem is not None: wait(d.dsem, d.dsem.h, 16 * d.dord)
                        else: wait(d.eng, self.esem[d.eng], d.seq)
                    bi = I.fn(engobj)
                    if I.dsem is not None: bi.then_inc(I.dsem.h, 16)
                    elif I.need: bi.then_inc(self.esem[X], 1)
            @block.sync
            def _(e): emit('sp', e)
            @block.scalar
            def _(e): emit('act', e)
            @block.vector
            def _(e): emit('dve', e)
            @block.gpsimd
            def _(e): emit('pool', e)
            @block.tensor
            def _(e): emit('pe', e)
        self.bar = {'seq': dict(self.seq), 'dma': [(s, s.count) for s in self.dsems]}
        self.cur = {x: [] for x in self.CE + ('sp',)}
        self.phase += 1

    def final_wait(self):
        nc = self.nc
        bar = self.bar
        with nc.Block() as block:
            @block.sync
            def _(e):
                for en, v in bar['seq'].items():
                    if v > 0: e.wait_ge(self.esem[en], v)
                for s, c in bar['dma']:
                    if c > 0: e.wait_ge(s.h, 16 * c)


class Ring:
    def __init__(self, items):
        self.items = items; self.i = 0
    def next(self):
        it = self.items[self.i % len(self.items)]; self.i += 1
        return it


def build_program(dbg=None):
    dbg = dbg or {}
    nc = bass.Bass("TRN2", target_bir_lowering=False)
    dt_in = lambda name, shape, dt=F32: nc.dram_tensor(name, list(shape), dt, kind="ExternalInput").ap()
    xT = dt_in("xT", [D, S]); ctxT = dt_in("ctxT", [D, L])
    vecs_d = dt_in("vecs", [128, NV]); consts_d = dt_in("consts", [128, NCONST]); cs_d = dt_in("cossin", [128, NCS])
    b1c_d = dt_in("b1cols", [2, 128, 512])
    rows0_d = dt_in("rows0", [1, NROW0]); rows1_d = dt_in("rows1", [1, NROW1]); rowsm_d = dt_in("rowsm", [1, 64])
    ada_w = dt_in("ada_w", [2, D, 6 * D])
    ev_w_in = dt_in("ev_w_in", [D, 1792]); ev_w_out = dt_in("ev_w_out", [D, D])
    od_w_in = dt_in("od_w_in", [D, 2 * D]); od_w_s = dt_in("od_w_s", [8, 128, 128]); od_w_out = dt_in("od_w_out", [D, D])
    r_w = dt_in("r_w", [2, D, NE]); w1 = dt_in("w1", [2, NE, D, 2 * D]); w2 = dt_in("w2", [2, NE, D, D])
    b2 = dt_in("b2", [2, NE, D])
    outT = nc.dram_tensor("outT", [D, S], F32, kind="ExternalOutput").ap()
    gt_scr = nc.dram_tensor("gt_scr", [2, NE, S], F32, kind="Internal").ap()
    mk_scr = nc.dram_tensor("mk_scr", [2, NE, S], BF16, kind="Internal").ap()
    dbg_out = {}
    for k, shp in dbg.items():
        dbg_out[k] = nc.dram_tensor("dbg_" + k, list(shp), F32, kind="ExternalOutput").ap()

    es = ExitStack()
    with es:
        P = Prog(nc, es)
        _sbn = [0]
        def SB(name, shape, dt=F32, st=es):
            _sbn[0] += 1
            return st.enter_context(nc.sbuf_tensor(f"sb{_sbn[0]}_{name}", list(shape), dt))
        hT = SB("hT", [128, 8, S]); R_h = [[Res() for _ in range(4)] for _ in range(8)]
        vecs = SB("vecs", [128, NV]); R_vecs = Res()
        consts = SB("consts", [128, 128]); R_consts = Res()
        mod = SB("mod", [128, 2, 48, 2]); R_mod = Res()
        der = SB("der", [128, 2, 5, 8, 2]); R_der = Res()
        ones_bf = SB("ones_bf", [128, 128], BF16); ones_f = SB("ones_f", [128, 128]); ident_bf = SB("ident_bf", [128, 128], BF16)
        maskb = SB("maskb", [128, 256], BF16)
        R_k = Res()
        out_st = P.dsem("out_st"); out_st.auto = False; dbg_st = P.dsem("dbg_st"); dbg_st.auto = False
        cs_ld = None; wA = None; wB = None
        ident_f = consts[:, 0:128]
        def V(name, i=0, n=1): return vecs[:, VOFF[name] + i: VOFF[name] + i + n]

        def dump(name, ap, res_list):
            if name in dbg_out:
                P.dma('sp', dbg_out[name], ap, dbg_st, reads=res_list)

        scg = SB("scg", [128, 16], F32); R_sc = Res()
        def ada_layer(l, ph, nstage, width, lazy=False):
            PS = [ph.enter_context(nc.psum_tensor(f"L{P.phase}_adaps{l}_{i}", [128, 512], F32)) for i in range(2)]
            R_ps = [Res() for _ in range(2)]
            PST = ph.enter_context(nc.psum_tensor(f"L{P.phase}_adapst{l}", [128, 512], F32)); R_pst0 = Res()
            stage = [SB(f"adast{l}_{i}", [128, 8, width], F32, ph) for i in range(nstage)]
            R_st = [Res() for _ in range(nstage)]
            modrow = SB(f"modrow{l}", [2, 6 * D], F32, ph); R_mr = Res()
            items = []
            def chunk(cb):
                si = cb % nstage; pi = cb % 2
                P.dma('sp', stage[si][:], ada_w[l].rearrange("(k p) n -> p k n", p=128)[:, :, cb * width:(cb + 1) * width],
                      None, writes=[R_st[si]])
                for k in range(8):
                    P.op('pe', lambda e, k=k: e.matmul(
                        PS[pi][0:2, 0:width], scg[:, 2 * k:2 * k + 2], stage[si][:, k, :], start=(k == 0), stop=(k == 7)),
                        reads=[R_st[si], R_sc], writes=[R_ps[pi]])
                P.op('act', lambda e: e.copy(out=modrow[:, cb * width:(cb + 1) * width], in_=PS[pi][0:2, 0:width]),
                     reads=[R_ps[pi]], writes=[R_mr])
            for cb in range(6 * D // width):
                items.append(lambda cb=cb: chunk(cb))
            items.append(lambda: ada_finish(l, PST, R_pst0, modrow, R_mr))
            if lazy:
                return items
            for it in items: it()

        def ada_finish(l, PST, R_pst0, modrow, R_mr):
            for j in range(48):
                P.op('pe', lambda e, j=j: e.transpose(out=PST[:, 2 * j:2 * j + 2], in_=modrow[:, j * 128:(j + 1) * 128], identity=ident_f[0:2, 0:2]),
                     reads=[R_mr, R_consts], writes=[R_pst0])
            P.op('dve', lambda e: e.tensor_tensor(
                mod[:, l, :, :], PST[:, 0:96].rearrange("p (j s) -> p j s", s=2),
                V('ada_b', l * 48, 48).unsqueeze(2).to_broadcast([128, 48, 2]), op=ALU.add),
                reads=[R_pst0, R_vecs], writes=[R_mod])
            P.op('dve', lambda e: e.scalar_tensor_tensor(
                out=der[:, l, 0], in0=mod[:, l, 8:16, :], scalar=1.0,
                in1=V('nmg', l * 8, 8).unsqueeze(2).to_broadcast([128, 8, 2]), op0=ALU.add, op1=ALU.mult),
                reads=[R_mod, R_vecs], writes=[R_der])
            P.op('dve', lambda e: e.scalar_tensor_tensor(
                out=der[:, l, 1], in0=mod[:, l, 32:40, :], scalar=1.0,
                in1=V('nfg', l * 8, 8).unsqueeze(2).to_broadcast([128, 8, 2]), op0=ALU.add, op1=ALU.mult),
                reads=[R_mod, R_vecs], writes=[R_der])
            bo = 'ev_bout' if l == 0 else 'od_bout'
            P.op('dve', lambda e: e.tensor_tensor(
                der[:, l, 2], mod[:, l, 16:24, :], V(bo, 0, 8).unsqueeze(2).to_broadcast([128, 8, 2]), op=ALU.mult),
                reads=[R_mod, R_vecs], writes=[R_der])

        with ExitStack() as ph:
            P.dma('sp', vecs[:], vecs_d[:, :], cs_ld, writes=[R_vecs])
            P.dma('sp', consts[:], consts_d[:, 0:128], cs_ld, writes=[R_consts])
            mtmp = SB("mtmp", [128, 256], F32, ph); R_mtmp = Res()
            P.dma('sp', mtmp[:], consts_d[:, 128:384], cs_ld, writes=[R_mtmp])
            for k in range(8):
                P.dma('sp', hT[:, k, :], xT[k * 128:(k + 1) * 128, :], cs_ld, writes=R_h[k])
            P.op('dve', lambda e: e.memset(ones_f[:], 1.0), writes=[R_k])
            P.op('dve', lambda e: e.memset(ones_bf[:], 1.0), writes=[R_k])
            P.op('dve', lambda e: e.tensor_copy(ident_bf[:], ident_f), reads=[R_consts], writes=[R_k])
            P.op('dve', lambda e: e.tensor_copy(maskb[:], mtmp[:]), reads=[R_mtmp], writes=[R_k])
            P.op('act', lambda e: e.activation(out=scg[:], in_=V('sc_in', 0, 16), func=AF.Silu), reads=[R_vecs], writes=[R_sc])
            ada_layer(0, ph, 3, 512)
            dump('mod', mod[:].rearrange("p l j s -> p (l j s)"), [R_mod])
            P.flush()

        def norm_mod(ph_tiles, src_fn, n_tok, A_fn, sh_fn, dst_fn, R_src, R_dst, ps_ss, R_ps_ss, tmp, also32=None):
            sq, R_sq = tmp['sq'], tmp['R_sq']
            for k in range(8):
                s_i = tmp['sqi'][0] % len(sq); tmp['sqi'][0] += 1
                P.op('act', lambda e, k=k, s_i=s_i: e.activation(out=sq[s_i][:, 0:n_tok], in_=src_fn(k), func=AF.Square),
                     reads=[R_src[k]], writes=[R_sq[s_i]])
                P.op('pe', lambda e, k=k, s_i=s_i: e.matmul(ps_ss[:, 0:n_tok], ones_bf[:], sq[s_i][:, 0:n_tok], start=(k == 0), stop=(k == 7)),
                     reads=[R_sq[s_i], R_k], writes=[R_ps_ss])
            rs, R_rs = tmp['rs'], tmp['R_rs']
            P.op('act', lambda e: e.activation(out=rs[:, 0:n_tok], in_=ps_ss[:, 0:n_tok], func=AF.Sqrt, scale=1.0 / D, bias=tmp['eps'][:, 0:1]),
                 reads=[R_ps_ss], writes=[R_rs])
            P.op('dve', lambda e: e.reciprocal(rs[:, 0:n_tok], rs[:, 0:n_tok]), reads=[R_rs], writes=[R_rs])
            for k in range(8):
                t_i = tmp['ti'][0] % len(tmp['t']); tmp['ti'][0] += 1
                tt_, R_tt = tmp['t'][t_i], tmp['R_t'][t_i]
                P.op('dve', lambda e, k=k, tt_=tt_: e.tensor_tensor(tt_[:, 0:n_tok], src_fn(k), rs[:, 0:n_tok], op=ALU.mult),
                     reads=[R_src[k], R_rs], writes=[R_tt])
                if also32 is None:
                    P.op('act', lambda e, k=k, tt_=tt_: e.activation(out=dst_fn(k), in_=tt_[:, 0:n_tok], func=AF.Identity, scale=A_fn(k), bias=sh_fn(k)),
                         reads=[R_tt, R_der, R_mod], writes=[R_dst[k]])
                else:
                    d32, R_d32 = also32
                    P.op('act', lambda e, k=k, tt_=tt_: e.activation(out=d32(k), in_=tt_[:, 0:n_tok], func=AF.Identity, scale=A_fn(k), bias=sh_fn(k)),
                         reads=[R_tt, R_der, R_mod], writes=[R_d32[k]])
                    P.op('pool', lambda e, k=k: e.tensor_copy(dst_fn(k), d32(k)), reads=[R_d32[k]], writes=[R_dst[k]])

        def mk_norm_tmp(ph, pfx):
            t = {'sq': [SB(f"{pfx}sq{i}", [128, 512], BF16, ph) for i in range(3)], 'R_sq': [Res() for _ in range(3)], 'sqi': [0],
                 'rs': SB(f"{pfx}rs", [128, 512], F32, ph), 'R_rs': Res(),
                 't': [SB(f"{pfx}t{i}", [128, 512], F32, ph) for i in range(2)], 'R_t': [Res() for _ in range(2)], 'ti': [0],
                 'eps': SB(f"{pfx}eps", [128, 1], F32, ph)}
            P.op('dve', lambda e: e.memset(t['eps'][:], EPS), writes=[t['R_rs']])
            return t

        def moe_phase(l):
            gtf = lambda m: mod[:, l, 40 + m, 0:1]
            with ExitStack() as pm:
                hf = SB("hf", [128, 8, S], BF16, pm); R_hf = [[Res() for _ in range(4)] for _ in range(8)]
                with ExitStack() as ph:
                    hf32 = SB("hf32", [128, 8, 512], F32, ph); R_hf32 = [Res() for _ in range(8)]
                    GT = SB("GT", [NE, S], F32, ph); R_GT = [Res() for _ in range(4)]
                    GTs = SB("GTs", [NE, S], F32, ph); R_GTs = Res()
                    rw = SB("rw", [128, 8, NE], F32, ph); R_rw = Res()
                    B2 = SB("B2", [NE, D], F32, ph); R_B2 = Res()
                    rb = SB("rb", [128, NE], F32, ph); R_rb = Res()
                    lg = SB("lg", [128, 16, NE], F32, ph); R_lg = [Res() for _ in range(16)]
                    Gt = SB("Gt", [128, 16, NE], F32, ph)
                    sm = SB("sm", [128, 16, 16], F32, ph)
                    ntmp = mk_norm_tmp(ph, f"m{l}")
                    PSx = ph.enter_context(nc.psum_tensor(f"L{P.phase}_psx", [128, 512], F32)); R_psx = Res()
                    PSy = [ph.enter_context(nc.psum_tensor(f"L{P.phase}_psy{i}", [128, 512], F32)) for i in range(2)]; R_psy = [Res() for _ in range(2)]
                    PSo = [ph.enter_context(nc.psum_tensor(f"L{P.phase}_m1pso{i}", [128, 512], F32)) for i in range(2)]; R_pso = [Res() for _ in range(2)]
                    P.dma('sp', rw[:], r_w[l].rearrange("(k p) n -> p k n", p=128), cs_ld, writes=[R_rw])
                    P.dma('sp', B2[:], b2[l], cs_ld, writes=[R_B2])
                    P.dma('sp', rb[:], rowsm_d[0:1, l * 32:(l + 1) * 32].partition_broadcast(128), cs_ld, writes=[R_rb])
                    ada_items = ada_layer(1, ph, 2, 256, lazy=True) if l == 0 else []
                    for tt in range(4):
                        tok = slice(tt * 512, (tt + 1) * 512)
                        norm_mod(ph, lambda k, tok=tok: hT[:, k, tok], 512,
                                 lambda k: der[:, l, 1, k, 0:1], lambda k: mod[:, l, 24 + k, 0:1],
                                 lambda k, tok=tok: hf[:, k, tok], [R_h[k][tt] for k in range(8)], [R_hf[k][tt] for k in range(8)],
                                 PSx, R_psx, ntmp, also32=(lambda k: hf32[:, k, :], R_hf32))
                        for sub in range(4):
                            i = tt * 4 + sub; yi = i % 2
                            for k in range(8):
                                P.op('pe', lambda e, k=k, sub=sub, yi=yi: e.matmul(PSy[yi][:, 0:NE], hf32[:, k, sub * 128:(sub + 1) * 128], rw[:, k, :],
                                                                                  start=(k == 0), stop=(k == 7)),
                                     reads=[R_hf32[k], R_rw], writes=[R_psy[yi]])
                            P.op('dve', lambda e, i=i, yi=yi: e.tensor_tensor(lg[:, i, :], PSy[yi][:, 0:NE], rb[:], op=ALU.add),
                                 reads=[R_psy[yi], R_rb], writes=[R_lg[i]])
                            P.op('dve', lambda e, i=i: e.max(out=sm[:, i, 0:8], in_=lg[:, i, :]), reads=[R_lg[i]], writes=[R_lg[i]])
                            P.op('dve', lambda e, i=i: e.tensor_scalar(sm[:, i, 8:9], sm[:, i, 0:1], -1.0, None, op0=ALU.mult),
                                 reads=[R_lg[i]], writes=[R_lg[i]])
                            P.op('act', lambda e, i=i: e.activation(out=Gt[:, i, :], in_=lg[:, i, :], func=AF.Exp, bias=sm[:, i, 8:9], scale=1.0),
                                 reads=[R_lg[i]], writes=[R_lg[i]])
                            P.op('dve', lambda e, i=i: e.scalar_tensor_tensor(out=Gt[:, i, :], in0=lg[:, i, :], scalar=sm[:, i, 3:4], in1=Gt[:, i, :],
                                                                                op0=ALU.is_ge, op1=ALU.mult),
                                 reads=[R_lg[i]], writes=[R_lg[i]])
                            P.op('dve', lambda e, i=i: e.reduce_sum(out=sm[:, i, 9:10], in_=Gt[:, i, :], axis=AX.X), reads=[R_lg[i]], writes=[R_lg[i]])
                            P.op('dve', lambda e, i=i: e.reciprocal(sm[:, i, 10:11], sm[:, i, 9:10]), reads=[R_lg[i]], writes=[R_lg[i]])
                            P.op('dve', lambda e, i=i: e.tensor_scalar(Gt[:, i, :], Gt[:, i, :], sm[:, i, 10:11], None, op0=ALU.mult),
                                 reads=[R_lg[i]], writes=[R_lg[i]])
                            P.op('pe', lambda e, i=i, yi=yi: e.transpose(out=PSy[yi][0:NE, 128:256], in_=Gt[:, i, :], identity=ident_f),
                                 reads=[R_lg[i], R_consts], writes=[R_psy[yi]])
                            P.op('act', lambda e, i=i, yi=yi: e.copy(out=GT[:, i * 128:(i + 1) * 128], in_=PSy[yi][0:NE, 128:256]),
                                 reads=[R_psy[yi]], writes=[R_GT[tt]])
                            if ada_items: ada_items.pop(0)()
                    dump(f'hf{l}', hf[:], sum(R_hf, []))
                    dump(f'G{l}', Gt[:].rearrange("p i e -> p (i e)"), R_lg)
                    P.op('dve', lambda e: e.tensor_scalar(GTs[:], GT[:], 1.0 / 1.702, None, op0=ALU.mult), reads=R_GT, writes=[R_GTs])
                    P.dma('sp', gt_scr[l], GTs[:], reads=[R_GTs])
                    GTm = SB("GTm", [NE, S], BF16, ph); R_GTm = Res()
                    P.op('dve', lambda e: e.tensor_scalar(GTm[:], GT[:], 0.0, None, op0=ALU.is_gt), reads=R_GT, writes=[R_GTm])
                    P.dma('sp', mk_scr[l], GTm[:], reads=[R_GTm])
                    while ada_items: ada_items.pop(0)()
                    for tt in range(4):
                        tok = slice(tt * 512, (tt + 1) * 512)
                        for m in range(8):
                            oi = (tt * 8 + m) % 2
                            P.op('pe', lambda e, m=m, oi=oi, tok=tok: e.matmul(PSo[oi][:], B2[:, m * 128:(m + 1) * 128], GT[:, tok], start=True, stop=True),
                                 reads=[R_B2, R_GT[tt]], writes=[R_pso[oi]])
                            P.op('dve', lambda e, m=m, oi=oi, tok=tok: e.scalar_tensor_tensor(out=hT[:, m, tok], in0=PSo[oi][:], scalar=gtf(m), in1=hT[:, m, tok],
                                                                                               op0=ALU.mult, op1=ALU.add),
                                 reads=[R_pso[oi], R_mod, R_h[m][tt]], writes=[R_h[m][tt]])
                    P.flush()
                with ExitStack() as ph:
                    act = SB("actT", [128, 8, S], BF16, ph); R_act = [[Res() for _ in range(4)] for _ in range(8)]
                    W1s = [SB(f"W1s{i}", [128, 8, 2, 512], BF16, ph) for i in range(2)]; R_W1 = [[Res(), Res()] for _ in range(2)]
                    W2q = [SB(f"W2q{i}", [128, 8, 256], BF16, ph) for i in range(3)]; R_W2 = [Res() for _ in range(3)]
                    nq = [0]
                    w1sem = [None] * 2; w2sem = [None] * 2
                    hfm = [SB(f"hfm{i}", [128, 8, 512], BF16, ph) for i in range(2)]; R_hfm = [[Res() for _ in range(8)] for _ in range(2)]
                    maskt = SB("maskt", [128, 512], BF16, ph); R_maskt = Res()
                    sg = [SB(f"sg_{i}", [128, 512], F32, ph) for i in range(2)]; R_sg = [Res() for _ in range(2)]
                    u0 = [SB(f"u0_{i}", [128, 512], F32, ph) for i in range(2)]; R_u0 = [Res() for _ in range(2)]
                    b1e = [SB(f"b1e{i}", [128, 16], F32, ph) for i in range(2)]; R_b1e = [Res() for _ in range(2)]
                    PSg = [ph.enter_context(nc.psum_tensor(f"L{P.phase}_psg{i}", [128, 512], F32)) for i in range(2)]; R_psg = [Res() for _ in range(2)]
                    PSu = [ph.enter_context(nc.psum_tensor(f"L{P.phase}_psu{i}", [128, 512], F32)) for i in range(2)]; R_psu = [Res() for _ in range(2)]
                    PSo = [ph.enter_context(nc.psum_tensor(f"L{P.phase}_pso{i}", [128, 512], F32)) for i in range(4)]; R_pso = [Res() for _ in range(4)]

                    def load_w1(e, half):
                        for two in range(2):
                            src = w1[l, e].rearrange("(k p) (two h j) -> p k two h j", p=128, two=2, h=2)[:, :, two, half, :]
                            P.dma('pool', W1s[half][:, :, two, :], src, w1sem[half], writes=[R_W1[half][two]])
                    def load_w2q(e, q):
                        slot = (e * 4 + q) % 3
                        src = w2[l, e].rearrange("(k p) (q j) -> p k q j", p=128, q=4)[:, :, q, :]
                        P.dma('pool', W2q[slot][:], src, None, writes=[R_W2[slot]])

                    def load_b1(e):
                        eb_ = e % 2
                        P.dma('sp', b1e[eb_][:], b1c_d[l][:, e * 16:(e + 1) * 16], writes=[R_b1e[eb_]])
                        P.op('dve', lambda e_: e_.tensor_scalar(b1e[eb_][:, 8:16], b1e[eb_][:, 8:16], 1.0, None, op0=ALU.add), reads=[R_b1e[eb_]], writes=[R_b1e[eb_]])
                    load_b1(0)
                    load_w1(0, 0); load_w1(0, 1)
                    cnt = {'g': 0, 'o': 0, 'b': 0}
                    n_exp = DEBUG.get('n_exp', NE)
                    Gb = [SB(f"Gb{i}", [128, 512], F32, ph) for i in range(2)]; R_Gb = [Res() for _ in range(2)]
                    pend = [None]
                    def flush_tail():
                        if pend[0] is not None:
                            pend[0](); pend[0] = None
                    groups = [(e_, tt) for e_ in range(n_exp) for tt in range(4)]
                    def prep(n):
                        e_, tt = groups[n]; r = n % 2
                        tok = slice(tt * 512, (tt + 1) * 512)
                        P.dma('sp', maskt[:], mk_scr[l, e_:e_ + 1, tok].partition_broadcast(128), writes=[R_maskt])
                        return [lambda k=k: P.op('dve', lambda e: e.tensor_tensor(hfm[r][:, k, :], hf[:, k, tok], maskt[:], op=ALU.mult),
                                                 reads=[R_hf[k][tt], R_maskt], writes=[R_hfm[r][k]]) for k in range(8)]
                    for it in prep(0): it()
                    for n_, (e_, tt) in enumerate(groups):
                        if tt == 0:
                            load_w2q(e_, 0); load_w2q(e_, 1); load_w2q(e_, 2)
                            if e_ + 1 < n_exp: load_b1(e_ + 1)
                        pitems = prep(n_ + 1) if n_ + 1 < len(groups) else []
                        hr = n_ % 2
                        if True:
                            if True:
                                tok = slice(tt * 512, (tt + 1) * 512)
                                gi = cnt['b'] % 2; cnt['b'] += 1
                                P.dma('sp', Gb[gi][:], gt_scr[l, e_:e_ + 1, tok].partition_broadcast(128), writes=[R_Gb[gi]])
                                for ui in range(8):
                                    half = ui // 4; cc = ui % 4
                                    c = ui
                                    pi = cnt['g'] % 2; cnt['g'] += 1
                                    for it in pitems[ui:ui + 1]: it()
                                    for k in range(8):
                                        P.op('pe', lambda e, k=k, cc=cc, pi=pi, half=half, hr=hr: e.matmul(
                                            PSg[pi][:], W1s[half][:, k, 0, cc * 128:(cc + 1) * 128], hfm[hr][:, k, :], start=(k == 0), stop=(k == 7)),
                                            reads=[R_W1[half][0], R_hfm[hr][k]], writes=[R_psg[pi]])
                                    for k in range(8):
                                        P.op('pe', lambda e, k=k, cc=cc, pi=pi, half=half, hr=hr: e.matmul(
                                            PSu[pi][:], W1s[half][:, k, 1, cc * 128:(cc + 1) * 128], hfm[hr][:, k, :], start=(k == 0), stop=(k == 7)),
                                            reads=[R_W1[half][1], R_hfm[hr][k]], writes=[R_psu[pi]])
                                    eb = e_ % 2
                                    bg = b1e[eb][:, c:c + 1]
                                    bu = b1e[eb][:, 8 + c:9 + c]
                                    P.op('act', lambda e, pi=pi, bu=bu: e.activation(out=u0[pi][:], in_=PSu[pi][:], func=AF.Identity, bias=bu, scale=1.0),
                                         reads=[R_psu[pi], R_b1e[eb]], writes=[R_u0[pi]])
                                    P.op('dve', lambda e, pi=pi, bg=bg: e.tensor_scalar(sg[pi][:], PSg[pi][:], bg, 7.0, op0=ALU.add, op1=ALU.min),
                                         reads=[R_psg[pi], R_b1e[eb]], writes=[R_sg[pi]])
                                    P.op('act', lambda e, pi=pi: e.activation(out=sg[pi][:], in_=sg[pi][:], func=AF.Silu, scale=1.702),
                                         reads=[R_sg[pi]], writes=[R_sg[pi]])
                                    P.op('dve', lambda e, pi=pi: e.tensor_scalar(u0[pi][:], u0[pi][:], -6.0, 8.0, op0=ALU.max, op1=ALU.min),
                                         reads=[R_u0[pi]], writes=[R_u0[pi]])
                                    flush_tail()
                                    def tail(pi=pi, gi=gi, c=c, tok=tok, tt=tt):
                                        P.op('dve', lambda e: e.tensor_tensor(u0[pi][:], u0[pi][:], sg[pi][:], op=ALU.mult),
                                             reads=[R_sg[pi], R_u0[pi]], writes=[R_u0[pi]])
                                        P.op('dve', lambda e: e.tensor_tensor(act[:, c, tok], u0[pi][:], Gb[gi][:], op=ALU.mult),
                                             reads=[R_u0[pi], R_Gb[gi]], writes=[R_act[c][tt]])
                                    pend[0] = tail
                                    if tt == 3 and cc == 3 and e_ + 1 < n_exp:
                                        load_w1(e_ + 1, half)
                        if tt != 3: continue
                        flush_tail()
                        for q in range(4):
                            slot = (e_ * 4 + q) % 3
                            for tt2 in range(4):
                                tok = slice(tt2 * 512, (tt2 + 1) * 512)
                                for mm_ in range(2):
                                    m = q * 2 + mm_
                                    oi = cnt['o'] % 4; cnt['o'] += 1
                                    for k in range(8):
                                        P.op('pe', lambda e, k=k, mm_=mm_, oi=oi, slot=slot, tok=tok: e.matmul(
                                            PSo[oi][:], W2q[slot][:, k, mm_ * 128:(mm_ + 1) * 128], act[:, k, tok], start=(k == 0), stop=(k == 7)),
                                            reads=[R_W2[slot], R_act[k][tt2]], writes=[R_pso[oi]])
                                    P.op('dve', lambda e, m=m, oi=oi, tok=tok: e.scalar_tensor_tensor(out=hT[:, m, tok], in0=PSo[oi][:], scalar=gtf(m), in1=hT[:, m, tok],
                                                                                                       op0=ALU.mult, op1=ALU.add),
                                         reads=[R_pso[oi], R_mod, R_h[m][tt2]], writes=[R_h[m][tt2]])
                            if q == 0: load_w2q(e_, 3)
                    dump(f'h_moe{l}', hT[:], sum(R_h, []))
                    P.flush()

        def mixer0():
            with ExitStack() as pa:
                u_bf = SB("u_bf", [128, 4, S + 30], BF16, pa); R_u = [[Res() for _ in range(4)] for _ in range(4)]; R_upad = Res()
                qT = SB("qT", [128, 16, 512], BF16, pa); R_qT = [Res() for _ in range(16)]
                kT = SB("kT", [128, S + L], BF16, pa); R_kT = [Res() for _ in range(18)]
                vtok = SB("vtok", [128, 18, 128], BF16, pa); R_v = [Res() for _ in range(18)]
                rows0 = SB("rows0", [128, NROW0], F32, pa); R_rows0 = Res()
                es_t = SB("es_t", [128, 4], F32, pa); R_es = Res()
                with ExitStack() as ph:
                    hm = [SB(f"hm{i}", [128, 8, 512], BF16, ph) for i in range(2)]; R_hm = [[Res() for _ in range(8)] for _ in range(2)]
                    cT = SB("cT", [128, 8, L], F32, ph); R_cT = [Res() for _ in range(8)]
                    w_in = SB("w_in", [128, 8, 1792], BF16, ph); R_win = Res()
                    cs_t = SB("cs_t", [128, NCS], F32, ph); R_cs = Res()
                    G10 = SB("G10", [128, 10, 64], F32, ph); R_G10 = Res()
                    cos_t = cs_t[:, 0:512].rearrange("p (i f) -> p i f", f=32)
                    sin_t = cs_t[:, 512:1024].rearrange("p (i f) -> p i f", f=32)
                    ntmp = mk_norm_tmp(ph, "a1")
                    qkv = SB("qkv", [128, 768], F32, ph); R_qkv = Res()
                    sqq = SB("sqq", [128, 640], F32, ph); R_sqq = Res()
                    st10 = SB("st10", [128, 16], F32, ph); R_st10 = Res()
                    qn = SB("qn", [128, 10, 64], F32, ph); R_qn = Res()
                    ra = SB("ra", [128, 10, 32], F32, ph); R_ra = Res()
                    rbb = SB("rbb", [128, 10, 32], F32, ph); R_rbb = Res()
                    qr = SB("qr", [128, 10, 64], F32, ph); R_qr = Res()
                    sgl = [SB(f"sgl{i}", [128, 512], F32, ph) for i in range(2)]; R_sgl = [Res() for _ in range(2)]
                    PSs = ph.enter_context(nc.psum_tensor(f"L{P.phase}_a1pss", [128, 512], F32)); R_pss = Res()
                    PSa = [ph.enter_context(nc.psum_tensor(f"L{P.phase}_a1psa{i}", [128, 512], F32)) for i in range(4)]; R_psa = [Res() for _ in range(4)]
                    PSq = ph.enter_context(nc.psum_tensor(f"L{P.phase}_a1psq", [128, 512], F32)); R_psq = Res()
                    PSk = ph.enter_context(nc.psum_tensor(f"L{P.phase}_a1psk", [128, 512], F32)); R_psk = Res()
                    PSt = ph.enter_context(nc.psum_tensor(f"L{P.phase}_a1pst", [128, 512], F32)); R_pst = Res()
                    for k in range(8):
                        P.dma('sp', cT[:, k, :], ctxT[k * 128:(k + 1) * 128, :], cs_ld, writes=[R_cT[k]])
                    P.dma('sp', rows0[:], rows0_d[0:1, :].partition_broadcast(128), cs_ld, writes=[R_rows0])
                    P.dma('sp', cs_t[:], cs_d[:, :], cs_ld, writes=[R_cs])
                    P.dma('pool', w_in[:], ev_w_in.rearrange("(k p) n -> p k n", p=128), wA, writes=[R_win])
                    P.op('dve', lambda e: e.memset(u_bf[:, :, 0:15], 0.0), writes=[R_upad])
                    P.op('dve', lambda e: e.memset(u_bf[:, :, S + 15:S + 30], 0.0), writes=[R_upad])
                    P.op('dve', lambda e: e.tensor_copy(G10[:, 0:8, :], rows0[:, 768:832].unsqueeze(1).to_broadcast([128, 8, 64])), reads=[R_rows0], writes=[R_G10])
                    P.op('dve', lambda e: e.tensor_copy(G10[:, 8:10, :], rows0[:, 832:896].unsqueeze(1).to_broadcast([128, 2, 64])), reads=[R_rows0], writes=[R_G10])
                    P.op('act', lambda e: e.activation(out=es_t[0:64, :], in_=rows0[0:64, 896:900], func=AF.Exp), reads=[R_rows0], writes=[R_es])
                    P.op('act', lambda e: e.activation(out=es_t[64:128, :], in_=rows0[64:128, 900:904], func=AF.Exp), reads=[R_rows0], writes=[R_es])
                    na = 0
                    A1C = DEBUG.get('a1', 99)
                    for tt in range(5 if A1C >= 9 else (1 if A1C >= 1 else 0)):
                        hb = tt % 2
                        hmt = hm[hb]
                        if tt < 4:
                            tok = slice(tt * 512, (tt + 1) * 512)
                            norm_mod(ph, lambda k, tok=tok: hT[:, k, tok], 512, lambda k: der[:, 0, 0, k, 0:1], lambda k: mod[:, 0, k, 0:1],
                                     lambda k, hmt=hmt: hmt[:, k, :], [R_h[k][tt] for k in range(8)], R_hm[hb], PSs, R_pss, ntmp)
                        else:
                            norm_mod(ph, lambda k: cT[:, k, :], L, lambda k: der[:, 0, 0, k, 1:2], lambda k: mod[:, 0, k, 1:2],
                                     lambda k, hmt=hmt: hmt[:, k, 0:L], R_cT, R_hm[hb], PSs, R_pss, ntmp)
                        if tt == 0: dump('hm0', hmt[:], R_hm[hb])
                        if A1C == 1: break
                        if tt < 4:
                            for c in range(4):
                                pu = na % 4; pg = (na + 1) % 4; si = (na // 2) % 2; na += 2
                                for k in range(8):
                                    P.op('pe', lambda e, k=k, c=c, pu=pu, hmt=hmt: e.matmul(PSa[pu][:], w_in[:, k, c * 128:(c + 1) * 128], hmt[:, k, :], start=(k == 0), stop=(k == 7)),
                                         reads=[R_win, R_hm[hb][k]], writes=[R_psa[pu]])
                                for k in range(8):
                                    P.op('pe', lambda e, k=k, c=c, pg=pg, hmt=hmt: e.matmul(PSa[pg][:], w_in[:, k, 512 + c * 128:512 + (c + 1) * 128], hmt[:, k, :], start=(k == 0), stop=(k == 7)),
                                         reads=[R_win, R_hm[hb][k]], writes=[R_psa[pg]])
                                P.op('act', lambda e, c=c, pg=pg, si=si: e.activation(out=sgl[si][:], in_=PSa[pg][:], func=AF.Sigmoid, bias=V('ev_bin_a', 4 + c, 1), scale=1.0),
                                     reads=[R_psa[pg], R_vecs], writes=[R_sgl[si]])
                                P.op('dve', lambda e, c=c, pu=pu, si=si, tt=tt: e.scalar_tensor_tensor(
                                    out=u_bf[:, c, 15 + tt * 512:15 + (tt + 1) * 512], in0=PSa[pu][:], scalar=V('ev_bin_a', c, 1), in1=sgl[si][:], op0=ALU.add, op1=ALU.mult),
                                    reads=[R_psa[pu], R_sgl[si], R_vecs], writes=[R_u[c][tt]])
                        nsub = 4 if tt < 4 else 2
                        if A1C == 2: break
                        if A1C < 9: nsub = 1
                        for sub in range(nsub):
                            i = tt * 4 + sub
                            tokc = slice(i * 128, (i + 1) * 128)
                            lc = slice(sub * 128, (sub + 1) * 128)
                            main = tt < 4
                            if main:
                                for k in range(8):
                                    P.op('pe', lambda e, k=k, lc=lc, hmt=hmt: e.matmul(PSq[:], hmt[:, k, lc], w_in[:, k, 1024:1536], start=(k == 0), stop=(k == 7)),
                                         reads=[R_win, R_hm[hb][k]], writes=[R_psq])
                            for k in range(8):
                                P.op('pe', lambda e, k=k, lc=lc, hmt=hmt: e.matmul(PSk[:, 0:256], hmt[:, k, lc], w_in[:, k, 1536:1792], start=(k == 0), stop=(k == 7)),
                                     reads=[R_win, R_hm[hb][k]], writes=[R_psk])
                            if main:
                                P.op('dve', lambda e: e.tensor_tensor(qkv[:, 0:512], PSq[:], rows0[:, 0:512], op=ALU.add),
                                     reads=[R_psq, R_rows0], writes=[R_qkv])
                            P.op('dve', lambda e: e.tensor_tensor(qkv[:, 512:768], PSk[:, 0:256], rows0[:, 512:768], op=ALU.add),
                                 reads=[R_psk, R_rows0], writes=[R_qkv])
                            P.op('act', lambda e, i=i: e.copy(out=vtok[:, i, :], in_=qkv[:, 640:768]), reads=[R_qkv], writes=[R_v[i]])
                            h0 = 0 if main else 8
                            nh = 10 - h0
                            c0 = h0 * 64
                            P.op('pool', lambda e, c0=c0: e.tensor_tensor(sqq[:, c0:640], qkv[:, c0:640], qkv[:, c0:640], op=ALU.mult),
                                 reads=[R_qkv], writes=[R_sqq])
                            P.op('dve', lambda e, h0=h0, c0=c0: e.tensor_reduce(out=st10[:, h0:10], in_=sqq[:, c0:640].rearrange("p (h d) -> p h d", d=64),
                                                                                 axis=AX.X, op=ALU.add),
                                 reads=[R_sqq], writes=[R_st10])
                            P.op('act', lambda e, h0=h0: e.activation(out=st10[:, h0:10], in_=st10[:, h0:10], func=AF.Sqrt, scale=1.0 / 64, bias=ntmp['eps'][:, 0:1]),
                                 reads=[R_st10], writes=[R_st10])
                            P.op('dve', lambda e, h0=h0: e.reciprocal(st10[:, h0:10], st10[:, h0:10]), reads=[R_st10], writes=[R_st10])
                            P.op('dve', lambda e, h0=h0, nh=nh, c0=c0: e.tensor_tensor(
                                qn[:, h0:10, :], qkv[:, c0:640].rearrange("p (h d) -> p h d", d=64),
                                st10[:, h0:10].unsqueeze(2).to_broadcast([128, nh, 64]), op=ALU.mult),
                                reads=[R_qkv, R_st10], writes=[R_qn])
                            if A1C == 3: break
                            if main:
                                P.op('pool', lambda e: e.tensor_tensor(qn[:], qn[:], G10[:], op=ALU.mult), reads=[R_qn, R_G10], writes=[R_qn])
                                cb_ = lambda i=i: cos_t[:, i, :].unsqueeze(1).to_broadcast([128, 10, 32])
                                sb_ = lambda i=i: sin_t[:, i, :].unsqueeze(1).to_broadcast([128, 10, 32])
                                P.op('dve', lambda e, cb_=cb_: e.tensor_tensor(ra[:], qn[:, :, 0:32], cb_(), op=ALU.mult), reads=[R_qn, R_cs], writes=[R_ra])
                                P.op('pool', lambda e, sb_=sb_: e.tensor_tensor(rbb[:], qn[:, :, 32:64], sb_(), op=ALU.mult), reads=[R_qn, R_cs], writes=[R_rbb])
                                qperm = lambda lo, hi: qr[:, 0:8, lo:hi].rearrange("p (g kv) d -> p kv g d", kv=2)
                                v4 = lambda t: t[:, 0:8, :].rearrange("p (kv g) d -> p kv g d", kv=2)
                                P.op('dve', lambda e, qperm=qperm, v4=v4: e.tensor_tensor(qperm(0, 32), v4(ra), v4(rbb), op=ALU.subtract), reads=[R_ra, R_rbb], writes=[R_qr])
                                P.op('dve', lambda e: e.tensor_tensor(qr[:, 8:10, 0:32], ra[:, 8:10, :], rbb[:, 8:10, :], op=ALU.subtract), reads=[R_ra, R_rbb], writes=[R_qr])
                                P.op('dve', lambda e, cb_=cb_: e.tensor_tensor(ra[:], qn[:, :, 32:64], cb_(), op=ALU.mult), reads=[R_qn, R_cs], writes=[R_ra])
                                P.op('pool', lambda e, sb_=sb_: e.tensor_tensor(rbb[:], qn[:, :, 0:32], sb_(), op=ALU.mult), reads=[R_qn, R_cs], writes=[R_rbb])
                                P.op('dve', lambda e, qperm=qperm, v4=v4: e.tensor_tensor(qperm(32, 64), v4(ra), v4(rbb), op=ALU.add), reads=[R_ra, R_rbb], writes=[R_qr])
                                P.op('dve', lambda e: e.tensor_tensor(qr[:, 8:10, 32:64], ra[:, 8:10, :], rbb[:, 8:10, :], op=ALU.add), reads=[R_ra, R_rbb], writes=[R_qr])
                                if A1C == 4: break
                                for g in range(4):
                                    P.op('pe', lambda e, g=g: e.transpose(
                                        out=PSt[:, g * 128:(g + 1) * 128],
                                        in_=qr[:, 2 * g:2 * g + 2, :], identity=ident_f),
                                        reads=[R_qr, R_consts], writes=[R_pst])
                                P.op('pe', lambda e: e.transpose(out=PSk[:, 384:512], in_=qr[:, 8:10, :], identity=ident_f),
                                     reads=[R_qr, R_consts], writes=[R_psk])
                                P.op('act', lambda e, i=i: e.copy(out=qT[:, i, :], in_=PSt[:, 0:512]),
                                     reads=[R_pst], writes=[R_qT[i]])
                                P.op('dve', lambda e, tokc=tokc: e.tensor_copy(kT[:, tokc], PSk[:, 384:512]), reads=[R_psk], writes=[R_kT[i]])
                            else:
                                P.op('pool', lambda e: e.tensor_tensor(qr[:, 8:10, :], qn[:, 8:10, :], G10[:, 8:10, :], op=ALU.mult),
                                     reads=[R_qn, R_G10], writes=[R_qr])
                                P.op('pe', lambda e: e.transpose(out=PSk[:, 384:512], in_=qr[:, 8:10, :], identity=ident_f),
                                     reads=[R_qr, R_consts], writes=[R_psk])
                                P.op('dve', lambda e, tokc=tokc: e.tensor_copy(kT[:, tokc], PSk[:, 384:512]), reads=[R_psk], writes=[R_kT[i]])
                    dump('u', u_bf[:], sum(R_u, []))
                    dump('qT', qT[:].rearrange('p i f -> p (i f)'), R_qT); dump('kT', kT[:], R_kT); dump('v', vtok[:], R_v)
                    P.flush()
                if DEBUG.get('sub', 9) <= 1: return
                with ExitStack() as pb:
                    chT = SB("chT", [128, 4, S], BF16, pb); R_ch = [[Res() for _ in range(4)] for _ in range(4)]
                    atT = SB("atT", [128, 4, S], BF16, pb); R_at = [Res() for _ in range(16)]
                    with ExitStack() as ph:
                        acc = SB("acc", [128, 4, 512], F32, ph); R_acc = [Res() for _ in range(4)]
                        sq4 = SB("sq4", [128, 4, 512], F32, ph); R_sq4 = [Res() for _ in range(4)]
                        mean = SB("mean", [128, 512], F32, ph); R_mean = Res()
                        var = SB("var", [128, 512], F32, ph); R_var = Res()
                        m2 = SB("m2", [128, 512], F32, ph); R_m2 = Res()
                        tl = [SB(f"tl{i}", [128, 512], F32, ph) for i in range(2)]; R_tl = [Res() for _ in range(2)]
                        epsb = SB("epsb", [128, 1], F32, ph); R_epsb = Res()
                        Eb = [SB(f"Eb{i}", [128, 512], BF16, ph) for i in range(6)]; R_Eb = [Res() for _ in range(6)]
                        den = SB("den", [128, 512], F32, ph); R_den = Res()
                        PSm = ph.enter_context(nc.psum_tensor(f"L{P.phase}_a2psm", [128, 512], F32)); R_psm = Res()
                        PSv = ph.enter_context(nc.psum_tensor(f"L{P.phase}_a2psv", [128, 512], F32)); R_psv = Res()
                        PSs2 = [ph.enter_context(nc.psum_tensor(f"L{P.phase}_a2pss{i}", [128, 512], F32)) for i in range(2)]; R_pss2 = [Res() for _ in range(2)]
                        PSo2 = [ph.enter_context(nc.psum_tensor(f"L{P.phase}_a2pso{i}", [128, 512], F32)) for i in range(2)]; R_pso2 = [Res() for _ in range(2)]
                        PSd2 = [ph.enter_context(nc.psum_tensor(f"L{P.phase}_a2psd{i}", [128, 512], F32)) for i in range(2)]; R_psd2 = [Res() for _ in range(2)]
                        P.op('dve', lambda e: e.memset(epsb[:], EPS), writes=[R_epsb])
                        jobs = []
                        for i in range(16):
                            blocks = [(jb, m_) for jb, m_ in ((i - 1, 0), (i, None), (i + 1, 1)) if 0 <= jb < 16] + [(16, None), (17, None)]
                            for kv in range(2):
                                for bi_, (jb, m_) in enumerate(blocks):
                                    jobs.append(dict(i=i, kv=kv, jb=jb, m=m_, first=(bi_ == 0), last=(bi_ == len(blocks) - 1), n=len(jobs)))
                        def job_scores(J):
                            i, kv, jb = J['i'], J['kv'], J['jb']; si = J['n'] % 2
                            pr = slice(kv * 64, (kv + 1) * 64)
                            P.op('pe', lambda e: e.matmul(PSs2[si][:], kT[pr, jb * 128:(jb + 1) * 128], qT[pr, i, :], start=True, stop=True),
                                 reads=[R_kT[jb], R_qT[i]], writes=[R_pss2[si]])
                        def job_rest(J):
                            i, kv, jb, m_ = J['i'], J['kv'], J['jb'], J['m']; si = J['n'] % 2; ei = J['n'] % 6; pi = i % 2
                            pr = slice(kv * 64, (kv + 1) * 64)
                            first, last = J['first'], J['last']
                            P.op('act', lambda e: e.activation(out=Eb[ei][:], in_=PSs2[si][:], func=AF.Exp, scale=0.125),
                                 reads=[R_pss2[si]], writes=[R_Eb[ei]])
                            if m_ is not None:
                                P.op('pool', lambda e: e.tensor_tensor(
                                    Eb[ei][:].rearrange("p (g t) -> p g t", g=4), Eb[ei][:].rearrange("p (g t) -> p g t", g=4),
                                    maskb[:, m_ * 128:(m_ + 1) * 128].unsqueeze(1).to_broadcast([128, 4, 128]), op=ALU.mult),
                                    reads=[R_Eb[ei], R_k], writes=[R_Eb[ei]])
                            P.op('pe', lambda e: e.matmul(PSo2[pi][pr, :], vtok[:, jb, kv * 64:(kv + 1) * 64], Eb[ei][:], start=first, stop=last),
                                 reads=[R_v[jb], R_Eb[ei]], writes=[R_pso2[pi]])
                            P.op('pe', lambda e: e.matmul(PSd2[pi][pr, :], ones_bf[:, 0:64], Eb[ei][:], start=first, stop=last),
                                 reads=[R_k, R_Eb[ei]], writes=[R_psd2[pi]])
                            if last and kv == 1:
                                tokq = slice(i * 128, (i + 1) * 128)
                                P.op('dve', lambda e: e.tensor_tensor(den[:].rearrange("p (g t) -> p g t", g=4), PSd2[pi][:].rearrange("p (g t) -> p g t", g=4),
                                                                       es_t[:].unsqueeze(2).to_broadcast([128, 4, 128]), op=ALU.add),
                                     reads=[R_psd2[pi], R_es], writes=[R_den])
                                P.op('dve', lambda e: e.reciprocal(den[:], den[:]), reads=[R_den], writes=[R_den])
                                P.op('dve', lambda e: e.tensor_tensor(atT[:, :, tokq], PSo2[pi][:].rearrange("p (g t) -> p g t", g=4),
                                                                      den[:].rearrange("p (g t) -> p g t", g=4), op=ALU.mult),
                                     reads=[R_pso2[pi], R_den], writes=[R_at[i]])
                                return True
                            return False

                        ptmp = SB("ptmp", [128, 512], F32, ph); R_ptmp = Res()
                        def conv_taps(tt):
                            ops = []
                            for tap in range(31):
                                for c in range(4):
                                    src = u_bf[:, c, tt * 512 + tap: tt * 512 + tap + 512]
                                    wcol = V('conv_w', c * 31 + tap, 1)
                                    rd = [R_u[c][t2] for t2 in range(max(0, tt - 1), min(4, tt + 2))] + [R_upad, R_vecs]
                                    eng = 'dve'
                                    if tap == 0:
                                        ops.append(lambda eng=eng, c=c, src=src, wcol=wcol, rd=rd: P.op(eng, lambda e: e.tensor_scalar(
                                            acc[:, c, :], src, wcol, V('conv_b', c, 1), op0=ALU.mult, op1=ALU.add),
                                            reads=rd, writes=[R_acc[c]]))
                                    elif True:
                                        ops.append(lambda c=c, src=src, wcol=wcol, rd=rd: P.op('dve', lambda e: e.scalar_tensor_tensor(
                                            out=acc[:, c, :], in0=src, scalar=wcol, in1=acc[:, c, :], op0=ALU.mult, op1=ALU.add),
                                            reads=rd + [R_acc[c]], writes=[R_acc[c]]))
                                    else:
                                        def two(c=c, src=src, wcol=wcol, rd=rd):
                                            P.op('pool', lambda e: e.tensor_scalar(ptmp[:], src, wcol, None, op0=ALU.mult), reads=rd, writes=[R_ptmp])
                                            P.op('pool', lambda e: e.tensor_tensor(acc[:, c, :], acc[:, c, :], ptmp[:], op=ALU.add), reads=[R_ptmp, R_acc[c]], writes=[R_acc[c]])
                                        ops.append(two)
                            return ops

                        def conv_tile(tt):
                            for c in range(4):
                                P.op('act', lambda e, c=c: e.activation(out=sq4[:, c, :], in_=acc[:, c, :], func=AF.Square),
                                     reads=[R_acc[c]], writes=[R_sq4[c]])
                            for c in range(4):
                                P.op('pe', lambda e, c=c: e.matmul(PSm[:], ones_f[:], acc[:, c, :], start=(c == 0), stop=(c == 3)),
                                     reads=[R_acc[c], R_k], writes=[R_psm])
                            for c in range(4):
                                P.op('pe', lambda e, c=c: e.matmul(PSv[:], ones_f[:], sq4[:, c, :], start=(c == 0), stop=(c == 3)),
                                     reads=[R_sq4[c], R_k], writes=[R_psv])
                            P.op('act', lambda e: e.activation(out=mean[:], in_=PSm[:], func=AF.Identity, scale=1.0 / 512), reads=[R_psm], writes=[R_mean])
                            P.op('pool', lambda e: e.tensor_tensor(m2[:], mean[:], mean[:], op=ALU.mult), reads=[R_mean], writes=[R_m2])
                            P.op('dve', lambda e: e.scalar_tensor_tensor(out=var[:], in0=PSv[:], scalar=1.0 / 512, in1=m2[:], op0=ALU.mult, op1=ALU.subtract),
                                 reads=[R_psv, R_m2], writes=[R_var])
                            P.op('act', lambda e: e.activation(out=var[:], in_=var[:], func=AF.Sqrt, bias=epsb[:, 0:1], scale=1.0), reads=[R_var, R_epsb], writes=[R_var])
                            P.op('dve', lambda e: e.reciprocal(var[:], var[:]), reads=[R_var], writes=[R_var])
                            for c in range(4):
                                ti = c % 2
                                P.op('dve', lambda e, c=c, ti=ti: e.tensor_tensor(tl[ti][:], acc[:, c, :], mean[:], op=ALU.subtract),
                                     reads=[R_acc[c], R_mean], writes=[R_tl[ti]])
                                P.op('pool', lambda e, ti=ti: e.tensor_tensor(tl[ti][:], tl[ti][:], var[:], op=ALU.mult), reads=[R_tl[ti], R_var], writes=[R_tl[ti]])
                                P.op('act', lambda e, c=c, ti=ti, tt=tt: e.activation(out=chT[:, c, tt * 512:(tt + 1) * 512], in_=tl[ti][:], func=AF.Silu,
                                                                                      scale=V('cln_g', c, 1), bias=V('cln_b', c, 1)),
                                     reads=[R_tl[ti], R_vecs], writes=[R_ch[c][tt]])
                        conv_q = []
                        for tt in range(4):
                            ops = conv_taps(tt)
                            per = (len(ops) + 3) // 4
                            for qi in range(4):
                                conv_q.append((tt, qi, ops[qi * per:(qi + 1) * per]))
                        def run_conv(item):
                            tt, qi, ops = item
                            for o in ops: o()
                            if qi == 3: conv_tile(tt)
                        job_scores(jobs[0])
                        done_tiles = 0
                        for n in range(len(jobs)):
                            if n + 1 < len(jobs): job_scores(jobs[n + 1])
                            if job_rest(jobs[n]):
                                run_conv(conv_q[done_tiles]); done_tiles += 1
                        dump('chT', chT[:], sum(R_ch, [])); dump('atT', atT[:], R_at)
                        P.flush()
                    if DEBUG.get('sub', 9) <= 2: return
                    with ExitStack() as ph:
                        w_out = SB("w_out", [128, 8, D], BF16, ph); R_wo = [Res() for _ in range(3)]
                        tmp = [SB(f"a3t{i}", [128, 512], F32, ph) for i in range(2)]; R_tmp = [Res() for _ in range(2)]
                        PSo3 = [ph.enter_context(nc.psum_tensor(f"L{P.phase}_a3ps{i}", [128, 512], F32)) for i in range(2)]; R_pso3 = [Res() for _ in range(2)]
                        P.dma('pool', w_out[:, 0:4, :], ev_w_out[0:512, :].rearrange("(k p) n -> p k n", p=128), wB, writes=[R_wo[0]])
                        P.dma('pool', w_out[0:64, 4:8, :], ev_w_out[512:768, :].rearrange("(g d) n -> d g n", d=64), wB, writes=[R_wo[1]])
                        P.dma('pool', w_out[64:128, 4:8, :], ev_w_out[768:1024, :].rearrange("(g d) n -> d g n", d=64), wB, writes=[R_wo[2]])
                        n3 = 0
                        for tt in range(4):
                            tok = slice(tt * 512, (tt + 1) * 512)
                            for m in range(8):
                                oi = n3 % 2; n3 += 1
                                for c in range(4):
                                    P.op('pe', lambda e, c=c, m=m, oi=oi, tok=tok: e.matmul(PSo3[oi][:], w_out[:, c, m * 128:(m + 1) * 128], chT[:, c, tok], start=(c == 0), stop=False),
                                         reads=R_wo + [R_ch[c][tt]], writes=[R_pso3[oi]])
                                for g in range(4):
                                    P.op('pe', lambda e, g=g, m=m, oi=oi, tok=tok: e.matmul(PSo3[oi][:], w_out[:, 4 + g, m * 128:(m + 1) * 128], atT[:, g, tok], start=False, stop=(g == 3)),
                                         reads=R_wo + [R_at[tt * 4 + q_] for q_ in range(4)], writes=[R_pso3[oi]])
                                P.op('act', lambda e, m=m, oi=oi: e.activation(out=tmp[oi][:], in_=PSo3[oi][:], func=AF.Identity, scale=mod[:, 0, 16 + m, 0:1], bias=der[:, 0, 2, m, 0:1]),
                                     reads=[R_pso3[oi], R_mod, R_der], writes=[R_tmp[oi]])
                                P.op('dve', lambda e, m=m, oi=oi, tok=tok: e.tensor_tensor(hT[:, m, tok], hT[:, m, tok], tmp[oi][:], op=ALU.add),
                                     reads=[R_tmp[oi], R_h[m][tt]], writes=[R_h[m][tt]])
                        dump('h_mix0', hT[:], sum(R_h, []))
                        P.flush()

        def mixer1():
            with ExitStack() as pa:
                uT = SB("uT", [128, 8, S], BF16, pa); R_uT = [[Res() for _ in range(4)] for _ in range(8)]
                vt = SB("vt", [128, 16, D], BF16, pa); R_vt = [Res() for _ in range(16)]
                wsT = SB("wsT", [128, 8, 128], BF16, pa); R_ws = Res()
                with ExitStack() as ph:
                    wsf = SB("wsf", [128, 8, 128], F32, ph); R_wsf = Res()
                    PSt = [ph.enter_context(nc.psum_tensor(f"L{P.phase}_b0pst{i}", [128, 512], F32)) for i in range(2)]; R_pst = [Res(), Res()]
                    P.dma('sp', wsf[:], od_w_s.rearrange("g p q -> p g q"), cs_ld, writes=[R_wsf])
                    for g in range(8):
                        P.op('pe', lambda e, g=g: e.transpose(out=PSt[g // 4][:, (g % 4) * 128:(g % 4 + 1) * 128], in_=wsf[:, g, :], identity=ident_f),
                             reads=[R_wsf, R_consts], writes=[R_pst[g // 4]])
                    for hh in range(2):
                        P.op('act', lambda e, hh=hh: e.copy(out=wsT[:, hh * 4:(hh + 1) * 4, :].rearrange("p g q -> p (g q)"), in_=PSt[hh][:]), reads=[R_pst[hh]], writes=[R_ws])
                    P.flush()
                with ExitStack() as ph:
                    hm = [SB(f"hm1_{i}", [128, 8, 512], BF16, ph) for i in range(2)]; R_hm = [[Res() for _ in range(8)] for _ in range(2)]
                    w_in = SB("w_in1", [128, 8, D], BF16, ph); R_win = Res()
                    rows1 = SB("rows1", [128, 3 * D], F32, ph); R_rows1 = Res()
                    ntmp = mk_norm_tmp(ph, "b1")
                    vz = SB("vz", [128, D], F32, ph); R_vz = Res()
                    bst = SB("bst", [128, 2, 6], F32, ph); R_bst = Res()
                    mv = SB("mv", [128, 2], F32, ph); R_mv = Res()
                    PSs = ph.enter_context(nc.psum_tensor(f"L{P.phase}_b1pss", [128, 512], F32)); R_pss = Res()
                    PSa = [ph.enter_context(nc.psum_tensor(f"L{P.phase}_b1psa{i}", [128, 512], F32)) for i in range(2)]; R_psa = [Res() for _ in range(2)]
                    PSv = [ph.enter_context(nc.psum_tensor(f"L{P.phase}_b1psv{i}", [128, 512], F32)) for i in range(4)]; R_psv = [Res() for _ in range(4)]
                    P.dma('sp', rows1[:], rows1_d[0:1, 0:3 * D].partition_broadcast(128), cs_ld, writes=[R_rows1])
                    for pas in range(2):
                        P.dma('pool', w_in[:], od_w_in[:, pas * D:(pas + 1) * D].rearrange("(k p) n -> p k n", p=128), wA, writes=[R_win])
                        na = 0
                        for tt in range(4):
                            hb = tt % 2; hmt = hm[hb]
                            tok = slice(tt * 512, (tt + 1) * 512)
                            norm_mod(ph, lambda k, tok=tok: hT[:, k, tok], 512, lambda k: der[:, 1, 0, k, 0:1], lambda k: mod[:, 1, k, 0:1],
                                     lambda k, hmt=hmt: hmt[:, k, :], [R_h[k][tt] for k in range(8)], R_hm[hb], PSs, R_pss, ntmp)
                            if pas == 0 and tt == 0: dump('hm1', hmt[:], R_hm[hb])
                            if pas == 0:
                                for c in range(8):
                                    pi = na % 2; na += 1
                                    for k in range(8):
                                        P.op('pe', lambda e, k=k, c=c, pi=pi, hmt=hmt: e.matmul(PSa[pi][:], w_in[:, k, c * 128:(c + 1) * 128], hmt[:, k, :], start=(k == 0), stop=(k == 7)),
                                             reads=[R_win, R_hm[hb][k]], writes=[R_psa[pi]])
                                    P.op('act', lambda e, c=c, pi=pi, tok=tok: e.activation(out=uT[:, c, tok], in_=PSa[pi][:], func=AF.Gelu, bias=V('od_bin_u', c, 1), scale=1.0),
                                         reads=[R_psa[pi], R_vecs], writes=[R_uT[c][tt]])
                            else:
                                for sub in range(4):
                                    i = tt * 4 + sub
                                    lc = slice(sub * 128, (sub + 1) * 128)
                                    for hh in range(2):
                                        pv = (i * 2 + hh) % 4
                                        for k in range(8):
                                            P.op('pe', lambda e, k=k, hh=hh, pv=pv, lc=lc, hmt=hmt: e.matmul(PSv[pv][:], hmt[:, k, lc], w_in[:, k, hh * 512:(hh + 1) * 512], start=(k == 0), stop=(k == 7)),
                                                 reads=[R_win, R_hm[hb][k]], writes=[R_psv[pv]])
                                        P.op('dve', lambda e, hh=hh, pv=pv: e.tensor_tensor(vz[:, hh * 512:(hh + 1) * 512], PSv[pv][:], rows1[:, hh * 512:(hh + 1) * 512], op=ALU.add),
                                             reads=[R_psv[pv], R_rows1], writes=[R_vz])
                                    P.op('act', lambda e: e.activation(out=vz[:], in_=vz[:], func=AF.Gelu), reads=[R_vz], writes=[R_vz])
                                    for hh in range(2):
                                        P.op('dve', lambda e, hh=hh: e.bn_stats(out=bst[:, hh, :], in_=vz[:, hh * 512:(hh + 1) * 512]), reads=[R_vz], writes=[R_bst])
                                    P.op('dve', lambda e: e.bn_aggr(out=mv[:], in_=bst[:]), reads=[R_bst], writes=[R_mv])
                                    P.op('act', lambda e: e.activation(out=mv[:, 1:2], in_=mv[:, 1:2], func=AF.Sqrt, bias=ntmp['eps'][:, 0:1], scale=1.0), reads=[R_mv], writes=[R_mv])
                                    P.op('dve', lambda e: e.reciprocal(mv[:, 1:2], mv[:, 1:2]), reads=[R_mv], writes=[R_mv])
                                    P.op('dve', lambda e: e.tensor_scalar(vz[:], vz[:], mv[:, 0:1], mv[:, 1:2], op0=ALU.subtract, op1=ALU.mult),
                                         reads=[R_vz, R_mv], writes=[R_vz])
                                    P.op('pool', lambda e: e.tensor_tensor(vz[:], vz[:], rows1[:, 1024:2048], op=ALU.mult), reads=[R_vz, R_rows1], writes=[R_vz])
                                    P.op('pool', lambda e, i=i: e.tensor_tensor(vt[:, i, :], vz[:], rows1[:, 2048:3072], op=ALU.add), reads=[R_vz, R_rows1], writes=[R_vt[i]])
                    dump('uT', uT[:], sum(R_uT, [])); dump('vt', vt[:], R_vt)
                    P.flush()
                with ExitStack() as ph:
                    w_out = SB("w_out1", [128, 8, D], BF16, ph); R_wo = Res()
                    bsr = SB("bsr", [128, D], F32, ph); R_bsr = Res()
                    tmp = [SB(f"b3t{i}", [128, 512], F32, ph) for i in range(2)]; R_tmp = [Res() for _ in range(2)]
                    svb = [SB(f"svb{i}", [128, 128], F32, ph) for i in range(2)]; R_svb = [Res() for _ in range(2)]
                    PSg_ = [ph.enter_context(nc.psum_tensor(f"L{P.phase}_b2ps{i}", [128, 512], F32)) for i in range(2)]; R_psg_ = [Res() for _ in range(2)]
                    PSo3 = [ph.enter_context(nc.psum_tensor(f"L{P.phase}_b3ps{i}", [128, 512], F32)) for i in range(2)]; R_pso3 = [Res() for _ in range(2)]
                    P.dma('pool', w_out[:], od_w_out.rearrange("(k p) n -> p k n", p=128), wB, writes=[R_wo])
                    P.dma('sp', bsr[:], rows1_d[0:1, 3 * D:4 * D].partition_broadcast(128), cs_ld, writes=[R_bsr])
                    n2 = 0
                    for i in range(16):
                        tokc = slice(i * 128, (i + 1) * 128); tt = i // 4
                        for g in range(8):
                            pi = n2 % 2; n2 += 1
                            P.op('pe', lambda e, g=g, i=i, pi=pi: e.matmul(PSg_[pi][:, 0:128], vt[:, i, g * 128:(g + 1) * 128], wsT[:, g, :], start=True, stop=True),
                                 reads=[R_vt[i], R_ws], writes=[R_psg_[pi]])
                            P.op('dve', lambda e, g=g, pi=pi: e.tensor_tensor(svb[pi][:], PSg_[pi][:, 0:128], bsr[:, g * 128:(g + 1) * 128], op=ALU.add),
                                 reads=[R_psg_[pi], R_bsr], writes=[R_svb[pi]])
                            P.op('pool', lambda e, g=g, pi=pi, tokc=tokc: e.tensor_tensor(uT[:, g, tokc], uT[:, g, tokc], svb[pi][:], op=ALU.mult),
                                 reads=[R_svb[pi], R_uT[g][tt]], writes=[R_uT[g][tt]])
                    dump('prod', uT[:], sum(R_uT, []))
                    n3 = 0
                    for tt in range(4):
                        tok = slice(tt * 512, (tt + 1) * 512)
                        for m in range(8):
                            oi = n3 % 2; n3 += 1
                            for c in range(8):
                                P.op('pe', lambda e, c=c, m=m, oi=oi, tok=tok: e.matmul(PSo3[oi][:], w_out[:, c, m * 128:(m + 1) * 128], uT[:, c, tok], start=(c == 0), stop=(c == 7)),
                                     reads=[R_wo, R_uT[c][tt]], writes=[R_pso3[oi]])
                            P.op('act', lambda e, m=m, oi=oi: e.activation(out=tmp[oi][:], in_=PSo3[oi][:], func=AF.Identity, scale=mod[:, 1, 16 + m, 0:1], bias=der[:, 1, 2, m, 0:1]),
                                 reads=[R_pso3[oi], R_mod, R_der], writes=[R_tmp[oi]])
                            P.op('dve', lambda e, m=m, oi=oi, tok=tok: e.tensor_tensor(hT[:, m, tok], hT[:, m, tok], tmp[oi][:], op=ALU.add),
                                 reads=[R_tmp[oi], R_h[m][tt]], writes=[R_h[m][tt]])
                    dump('h_mix1', hT[:], sum(R_h, []))
                    P.flush()

        stages = dbg.get('_stages', None)
        stop_after = DEBUG.get('stop_after', 99)
        if stop_after >= 1 and DEBUG.get('sub', 9) >= 1: mixer0()
        if stop_after >= 2: moe_phase(0)
        if stop_after >= 3: mixer1()
        if stop_after >= 4: moe_phase(1)
        for k in range(8):
            P.dma('sp', outT[k * 128:(k + 1) * 128, :], hT[:, k, :], out_st, reads=R_h[k])
        P.flush()
        P.final_wait()
    return nc


def _rope_tables():
    rows = S // 64
    r = np.repeat(np.arange(rows), 64).astype(np.float32)
    col = np.tile(np.arange(64), rows).astype(np.float32)
    inv_freq = (np.float32(10000.0) ** (-np.arange(16, dtype=np.float32) / np.float32(16))).astype(np.float32)
    ang = np.concatenate([r[:, None] * inv_freq, col[:, None] * inv_freq], axis=-1).astype(np.float32)
    return np.cos(ang).astype(np.float32), np.sin(ang).astype(np.float32)


def _consts():
    c = np.zeros((128, NCONST), np.float32)
    c[:, 0:128] = np.eye(128, dtype=np.float32)
    j = np.arange(128)[:, None]; i = np.arange(128)[None, :]
    c[:, 128:256] = (j >= i).astype(np.float32)
    c[:, 256:384] = (j <= i).astype(np.float32)
    return c


def _cossin():
    c = np.zeros((128, NCS), np.float32)
    cos, sin = _rope_tables()
    c[:, 0:512] = cos.reshape(16, 128, 32).transpose(1, 0, 2).reshape(128, 512)
    c[:, 512:1024] = sin.reshape(16, 128, 32).transpose(1, 0, 2).reshape(128, 512)
    return c


def _cols(v):
    v = np.asarray(v, np.float32).reshape(-1, 128)
    return v.T


def _build_vecs(inp, b):
    f = lambda a: np.asarray(a, np.float32)
    parts = []
    sc = np.stack([_cols(f(inp['c'])[b]), _cols(f(inp['c_ctx']))], axis=2).reshape(128, 16)
    parts.append(sc)
    parts.append(_cols(f(inp['ada_b'])))
    parts.append(_cols(f(inp['norm_mix_g']))); parts.append(_cols(f(inp['norm_ffn_g'])))
    parts.append(_cols(f(inp['ev_b_in'])[0, :1024]))
    cw = f(inp['ev_conv_w'])[0]
    parts.append(cw.reshape(31, 4, 128).transpose(2, 1, 0).reshape(128, 124))
    parts.append(_cols(f(inp['ev_conv_b'])[0])); parts.append(_cols(f(inp['ev_conv_ln_g'])[0])); parts.append(_cols(f(inp['ev_conv_ln_b'])[0]))
    parts.append(_cols(f(inp['ev_b_out'])[0]))
    parts.append(_cols(f(inp['od_b_in'])[0, :1024]))
    parts.append(_cols(f(inp['od_b_out'])[0]))
    v = np.concatenate(parts, axis=1)
    assert v.shape == (128, NV), v.shape
    return np.ascontiguousarray(v)


_NC_CACHE = {}

def kernel(**inp):
    f = lambda a: np.ascontiguousarray(np.asarray(a, np.float32))
    key = (tuple(sorted(DEBUG.get('dbg', {}).items())), DEBUG.get('stop_after', 99), DEBUG.get('sub', 9), DEBUG.get('n_exp', 32), DEBUG.get('a1', 99))
    if key not in _NC_CACHE:
        _NC_CACHE[key] = build_program(DEBUG.get('dbg'))
    nc = _NC_CACHE[key]
    n = DEBUG.get('n_cores', 8)
    consts = _consts()
    x = f(inp['x']); ctx = f(inp['ctx'])
    rows0 = np.concatenate([f(inp['ev_b_in'])[0, 1024:], f(inp['ev_q_norm_g'])[0], f(inp['ev_k_norm_g'])[0], f(inp['ev_sink'])[0]])[None, :]
    rows1 = np.concatenate([f(inp['od_b_in'])[0, 1024:], f(inp['od_v_ln_g'])[0], f(inp['od_v_ln_b'])[0], f(inp['od_b_s'])[0].reshape(-1)])[None, :]
    rowsm = f(inp['moe_router_b']).reshape(1, 64)
    b1 = f(inp['moe_b1'])
    b1cols = np.ascontiguousarray(np.stack([_cols(b1[l]) for l in range(2)], axis=0))
    shared = dict(consts=consts, cossin=_cossin(), b1cols=b1cols, rows0=np.ascontiguousarray(rows0), rows1=np.ascontiguousarray(rows1), rowsm=rowsm,
                  ada_w=f(inp['ada_w']), ev_w_in=f(inp['ev_w_in'])[0], ev_w_out=f(inp['ev_w_out'])[0],
                  od_w_in=f(inp['od_w_in'])[0], od_w_s=f(inp['od_w_s'])[0], od_w_out=f(inp['od_w_out'])[0],
                  r_w=f(inp['moe_router_w']), w1=f(inp['moe_w1']), w2=f(inp['moe_w2']), b2=f(inp['moe_b2']))
    in_maps = []
    for b in range(n):
        m = dict(shared)
        m['xT'] = np.ascontiguousarray(x[b].T); m['ctxT'] = np.ascontiguousarray(ctx[b].T)
        m['vecs'] = _build_vecs(inp, b)
        in_maps.append(m)
    res = run_bass_kernel_spmd(nc, in_maps, core_ids=list(range(n)))
    DEBUG['last'] = res.results
    out = np.stack([np.ascontiguousarray(res.results[b]['outT'].T) for b in range(n)], axis=0)
    return out.astype(np.float32)
```

```python
import numpy as np
import ml_dtypes
import concourse.bass as bass
import concourse.mybir as mybir
from concourse.bass_utils import run_bass_kernel_spmd
from contextlib import ExitStack

F32, BF16 = mybir.dt.float32, mybir.dt.bfloat16
AF = mybir.ActivationFunctionType
ALU = mybir.AluOpType
AX = mybir.AxisListType

D = 1024; S = 2048; L = 256; NE = 32; EPS = 1e-6
DEBUG = {}

VOFF = {}
def _mk_voff():
    o = 0
    for name, n in [('sc_in', 16), ('ada_b', 96), ('nmg', 16), ('nfg', 16), ('ev_bin_a', 8),
                    ('conv_w', 124), ('conv_b', 4), ('cln_g', 4), ('cln_b', 4), ('ev_bout', 8),
                    ('od_bin_u', 8), ('od_bout', 8)]:
        VOFF[name] = o; o += n
    return o
NV = _mk_voff()
NCONST = 128 + 256
NCS = 1024
NROW0 = 768 + 64 + 64 + 8
NROW1 = 4096


class Res:
    __slots__ = ('name', 'w', 'r')
    def __init__(self, name=''):
        self.name = name; self.w = {}; self.r = {}

class DSem:
    def __init__(self, h):
        self.h = h; self.count = 0

class Ins:
    __slots__ = ('eng', 'fn', 'deps', 'seq', 'need', 'dsem', 'dord', 'phase')

class Prog:
    CE = ('act', 'dve', 'pool', 'pe')
    def __init__(self, nc, es):
        self.nc = nc
        self.esem = {e: es.enter_context(nc.semaphore('s_' + e)) for e in self.CE}
        self.seq = {e: 0 for e in self.CE}
        self.waited = {x: {} for x in self.CE + ('sp',)}
        self.dsems = []
        self.es = es
        self.cur = {x: [] for x in self.CE + ('sp',)}
        self.phase = 0
        self.bar = None
        self.n_ins = 0
        self.dpool = {}; self.dpi = {}

    def dsem(self, name):
        s = DSem(self.es.enter_context(self.nc.semaphore(name)))
        self.dsems.append(s)
        return s

    def op(self, eng, fn, reads=(), writes=(), dsem=None):
        I = Ins(); I.eng = eng; I.fn = fn; I.seq = 0; I.need = False; I.dsem = dsem; I.phase = self.phase
        key = dsem if dsem is not None else eng
        if dsem is not None:
            dsem.count += 1; I.dord = dsem.count
        raw = set()
        deps = set()
        for r in reads:
            for d in r.w.values():
                deps.add(d); raw.add(d)
        for w in writes:
            for d in w.w.values(): deps.add(d)
            for d in w.r.values(): deps.add(d)
        out = []
        for d in deps:
            if d.phase != self.phase: continue
            if d.dsem is None and d.eng == eng and dsem is None:
                if eng == 'pe': continue
            out.append(d)
            if d.dsem is None: d.need = True
        I.deps = out
        for r in reads: r.r[key] = I
        for w in writes: w.w[key] = I
        self.cur[eng].append(I)
        self.n_ins += 1
        return I

    def record(self, f):
        buf = []
        self.op = lambda eng, fn, reads=(), writes=(), dsem=None: buf.append((eng, fn, tuple(reads), tuple(writes)))
        try:
            f()
        finally:
            del self.op
        return buf

    def dma(self, eng, out, in_, dsem=None, reads=(), writes=()):
        auto = dsem is None or getattr(dsem, 'auto', True)
        if auto:
            pool = self.dpool.setdefault(eng, [])
            if not pool:
                for i in range(12 if eng == 'sp' else 8):
                    d = self.dsem(f"dp_{eng}{i}"); d.last = None; pool.append(d)
                self.dpi[eng] = 0
            dsem = pool[self.dpi[eng] % len(pool)]; self.dpi[eng] += 1
        I = self.op(eng, lambda e, o=out, i=in_: e.dma_start(out=o, in_=i), reads, writes, dsem=dsem)
        if auto:
            if dsem.last is not None and dsem.last.phase == self.phase and dsem.last not in I.deps:
                I.deps.append(dsem.last)
            dsem.last = I
        return I

    def flush(self):
        nc = self.nc
        for e in self.CE:
            for I in reversed(self.cur[e]):
                if I.dsem is None:
                    I.need = True; break
        for e in self.CE:
            for I in self.cur[e]:
                if I.need and I.dsem is None:
                    self.seq[e] += 1; I.seq = self.seq[e]
        bar_prev = self.bar
        with nc.Block() as block:
            def emit(X, engobj):
                wt = self.waited[X]
                def wait(key, sem, val):
                    if wt.get(key, 0) < val:
                        engobj.wait_ge(sem, val); wt[key] = val
                if bar_prev is not None:
                    for e, v in bar_prev['seq'].items():
                        if v > 0: wait(e, self.esem[e], v)
                    for s, c in bar_prev['dma']:
                        if c > 0: wait(s, s.h, 16 * c)
                for I in self.cur[X]:
                    for d in I.deps:
                        if d.dsem is not None: wait(d.dsem, d.dsem.h, 16 * d.dord)
                        else: wait(d.eng, self.esem[d.eng], d.seq)
                    bi = I.fn(engobj)
                    if I.dsem is not None: bi.then_inc(I.dsem.h, 16)
                    elif I.need: bi.then_inc(self.esem[X], 1)
            @block.sync
            def _(e): emit('sp', e)
            @block.scalar
            def _(e): emit('act', e)
            @block.vector
            def _(e): emit('dve', e)
            @block.gpsimd
            def _(e): emit('pool', e)
            @block.tensor
            def _(e): emit('pe', e)
        self.bar = {'seq': dict(self.seq), 'dma': [(s, s.count) for s in self.dsems]}
        self.cur = {x: [] for x in self.CE + ('sp',)}
        self.phase += 1

    def final_wait(self):
        nc = self.nc
        bar = self.bar
        with nc.Block() as block:
            @block.sync
            def _(e):
                for en, v in bar['seq'].items():
                    if v > 0: e.wait_ge(self.esem[en], v)
                for s, c in bar['dma']:
                    if c > 0: e.wait_ge(s.h, 16 * c)


class Ring:
    def __init__(self, items):
        self.items = items; self.i = 0
    def next(self):
        it = self.items[self.i % len(self.items)]; self.i += 1
        return it


def build_program(dbg=None):
    dbg = dbg or {}
    nc = bass.Bass("TRN2", target_bir_lowering=False)
    dt_in = lambda name, shape, dt=F32: nc.dram_tensor(name, list(shape), dt, kind="ExternalInput").ap()
    xT = dt_in("xT", [D, S]); ctxT = dt_in("ctxT", [D, L])
    vecs_d = dt_in("vecs", [128, NV]); consts_d = dt_in("consts", [128, NCONST]); cs_d = dt_in("cossin", [128, NCS])
    b1c_d = dt_in("b1cols", [2, 128, 512])
    rows0_d = dt_in("rows0", [1, NROW0]); rows1_d = dt_in("rows1", [1, NROW1]); rowsm_d = dt_in("rowsm", [1, 64])
    ada_w = dt_in("ada_w", [2, D, 6 * D])
    ev_w_in = dt_in("ev_w_in", [D, 1792]); ev_w_out = dt_in("ev_w_out", [D, D])
    od_w_in = dt_in("od_w_in", [D, 2 * D]); od_w_s = dt_in("od_w_s", [8, 128, 128]); od_w_out = dt_in("od_w_out", [D, D])
    r_w = dt_in("r_w", [2, D, NE]); w1 = dt_in("w1", [2, NE, D, 2 * D]); w2 = dt_in("w2", [2, NE, D, D])
    b2 = dt_in("b2", [2, NE, D])
    outT = nc.dram_tensor("outT", [D, S], F32, kind="ExternalOutput").ap()
    gt_scr = nc.dram_tensor("gt_scr", [2, NE, S], F32, kind="Internal").ap()
    mk_scr = nc.dram_tensor("mk_scr", [2, NE, S], BF16, kind="Internal").ap()
    dbg_out = {}
    for k, shp in dbg.items():
        dbg_out[k] = nc.dram_tensor("dbg_" + k, list(shp), F32, kind="ExternalOutput").ap()

    es = ExitStack()
    with es:
        P = Prog(nc, es)
        _sbn = [0]
        def SB(name, shape, dt=F32, st=es):
            _sbn[0] += 1
            return st.enter_context(nc.sbuf_tensor(f"sb{_sbn[0]}_{name}", list(shape), dt))
        hT = SB("hT", [128, 8, S]); R_h = [[Res() for _ in range(4)] for _ in range(8)]
        vecs = SB("vecs", [128, NV]); R_vecs = Res()
        consts = SB("consts", [128, 128]); R_consts = Res()
        mod = SB("mod", [128, 2, 48, 2]); R_mod = Res()
        der = SB("der", [128, 2, 5, 8, 2]); R_der = Res()
        ones_bf = SB("ones_bf", [128, 128], BF16); ones_f = SB("ones_f", [128, 128]); ident_bf = SB("ident_bf", [128, 128], BF16)
        maskb = SB("maskb", [128, 256], BF16)
        R_k = Res()
        out_st = P.dsem("out_st"); out_st.auto = False; dbg_st = P.dsem("dbg_st"); dbg_st.auto = False
        cs_ld = None; wA = None; wB = None
        ident_f = consts[:, 0:128]
        def V(name, i=0, n=1): return vecs[:, VOFF[name] + i: VOFF[name] + i + n]

        def dump(name, ap, res_list):
            if name in dbg_out:
                P.dma('sp', dbg_out[name], ap, dbg_st, reads=res_list)

        scg = SB("scg", [128, 16], F32); R_sc = Res()
        def ada_layer(l, ph, nstage, width, lazy=False):
            PS = [ph.enter_context(nc.psum_tensor(f"L{P.phase}_adaps{l}_{i}", [128, 512], F32)) for i in range(2)]
            R_ps = [Res() for _ in range(2)]
            PST = ph.enter_context(nc.psum_tensor(f"L{P.phase}_adapst{l}", [128, 512], F32)); R_pst0 = Res()
            stage = [SB(f"adast{l}_{i}", [128, 8, width], F32, ph) for i in range(nstage)]
            R_st = [Res() for _ in range(nstage)]
            modrow = SB(f"modrow{l}", [2, 6 * D], F32, ph); R_mr = Res()
            items = []
            def chunk(cb):
                si = cb % nstage; pi = cb % 2
                P.dma('sp', stage[si][:], ada_w[l].rearrange("(k p) n -> p k n", p=128)[:, :, cb * width:(cb + 1) * width],
                      None, writes=[R_st[si]])
                for k in range(8):
                    P.op('pe', lambda e, k=k: e.matmul(
                        PS[pi][0:2, 0:width], scg[:, 2 * k:2 * k + 2], stage[si][:, k, :], start=(k == 0), stop=(k == 7)),
                        reads=[R_st[si], R_sc], writes=[R_ps[pi]])
                P.op('act', lambda e: e.copy(out=modrow[:, cb * width:(cb + 1) * width], in_=PS[pi][0:2, 0:width]),
                     reads=[R_ps[pi]], writes=[R_mr])
            for cb in range(6 * D // width):
                items.append(lambda cb=cb: chunk(cb))
            items.append(lambda: ada_finish(l, PST, R_pst0, modrow, R_mr))
            if lazy:
                return items
            for it in items: it()

        def ada_finish(l, PST, R_pst0, modrow, R_mr):
            for j in range(48):
                P.op('pe', lambda e, j=j: e.transpose(out=PST[:, 2 * j:2 * j + 2], in_=modrow[:, j * 128:(j + 1) * 128], identity=ident_f[0:2, 0:2]),
                     reads=[R_mr, R_consts], writes=[R_pst0])
            P.op('dve', lambda e: e.tensor_tensor(
                mod[:, l, :, :], PST[:, 0:96].rearrange("p (j s) -> p j s", s=2),
                V('ada_b', l * 48, 48).unsqueeze(2).to_broadcast([128, 48, 2]), op=ALU.add),
                reads=[R_pst0, R_vecs], writes=[R_mod])
            P.op('dve', lambda e: e.scalar_tensor_tensor(
                out=der[:, l, 0], in0=mod[:, l, 8:16, :], scalar=1.0,
                in1=V('nmg', l * 8, 8).unsqueeze(2).to_broadcast([128, 8, 2]), op0=ALU.add, op1=ALU.mult),
                reads=[R_mod, R_vecs], writes=[R_der])
            P.op('dve', lambda e: e.scalar_tensor_tensor(
                out=der[:, l, 1], in0=mod[:, l, 32:40, :], scalar=1.0,
                in1=V('nfg', l * 8, 8).unsqueeze(2).to_broadcast([128, 8, 2]), op0=ALU.add, op1=ALU.mult),
                reads=[R_mod, R_vecs], writes=[R_der])
            bo = 'ev_bout' if l == 0 else 'od_bout'
            P.op('dve', lambda e: e.tensor_tensor(
                der[:, l, 2], mod[:, l, 16:24, :], V(bo, 0, 8).unsqueeze(2).to_broadcast([128, 8, 2]), op=ALU.mult),
                reads=[R_mod, R_vecs], writes=[R_der])

        with ExitStack() as ph:
            P.dma('sp', vecs[:], vecs_d[:, :], cs_ld, writes=[R_vecs])
            P.dma('sp', consts[:], consts_d[:, 0:128], cs_ld, writes=[R_consts])
            mtmp = SB("mtmp", [128, 256], F32, ph); R_mtmp = Res()
            P.dma('sp', mtmp[:], consts_d[:, 128:384], cs_ld, writes=[R_mtmp])
            for k in range(8):
                P.dma('sp', hT[:, k, :], xT[k * 128:(k + 1) * 128, :], cs_ld, writes=R_h[k])
            P.op('dve', lambda e: e.memset(ones_f[:], 1.0), writes=[R_k])
            P.op('dve', lambda e: e.memset(ones_bf[:], 1.0), writes=[R_k])
            P.op('dve', lambda e: e.tensor_copy(ident_bf[:], ident_f), reads=[R_consts], writes=[R_k])
            P.op('dve', lambda e: e.tensor_copy(maskb[:], mtmp[:]), reads=[R_mtmp], writes=[R_k])
            P.op('act', lambda e: e.activation(out=scg[:], in_=V('sc_in', 0, 16), func=AF.Silu), reads=[R_vecs], writes=[R_sc])
            ada_layer(0, ph, 3, 512)
            dump('mod', mod[:].rearrange("p l j s -> p (l j s)"), [R_mod])
            P.flush()

        def norm_mod(ph_tiles, src_fn, n_tok, A_fn, sh_fn, dst_fn, R_src, R_dst, ps_ss, R_ps_ss, tmp, also32=None):
            sq, R_sq = tmp['sq'], tmp['R_sq']
            for k in range(8):
                s_i = tmp['sqi'][0] % len(sq); tmp['sqi'][0] += 1
                P.op('act', lambda e, k=k, s_i=s_i: e.activation(out=sq[s_i][:, 0:n_tok], in_=src_fn(k), func=AF.Square),
                     reads=[R_src[k]], writes=[R_sq[s_i]])
                P.op('pe', lambda e, k=k, s_i=s_i: e.matmul(ps_ss[:, 0:n_tok], ones_bf[:], sq[s_i][:, 0:n_tok], start=(k == 0), stop=(k == 7)),
                     reads=[R_sq[s_i], R_k], writes=[R_ps_ss])
            rs, R_rs = tmp['rs'], tmp['R_rs']
            P.op('act', lambda e: e.activation(out=rs[:, 0:n_tok], in_=ps_ss[:, 0:n_tok], func=AF.Sqrt, scale=1.0 / D, bias=tmp['eps'][:, 0:1]),
                 reads=[R_ps_ss], writes=[R_rs])
            P.op('dve', lambda e: e.reciprocal(rs[:, 0:n_tok], rs[:, 0:n_tok]), reads=[R_rs], writes=[R_rs])
            for k in range(8):
                t_i = tmp['ti'][0] % len(tmp['t']); tmp['ti'][0] += 1
                tt_, R_tt = tmp['t'][t_i], tmp['R_t'][t_i]
                P.op('dve', lambda e, k=k, tt_=tt_: e.tensor_tensor(tt_[:, 0:n_tok], src_fn(k), rs[:, 0:n_tok], op=ALU.mult),
                     reads=[R_src[k], R_rs], writes=[R_tt])
                if also32 is None:
                    P.op('act', lambda e, k=k, tt_=tt_: e.activation(out=dst_fn(k), in_=tt_[:, 0:n_tok], func=AF.Identity, scale=A_fn(k), bias=sh_fn(k)),
                         reads=[R_tt, R_der, R_mod], writes=[R_dst[k]])
                else:
                    d32, R_d32 = also32
                    P.op('act', lambda e, k=k, tt_=tt_: e.activation(out=d32(k), in_=tt_[:, 0:n_tok], func=AF.Identity, scale=A_fn(k), bias=sh_fn(k)),
                         reads=[R_tt, R_der, R_mod], writes=[R_d32[k]])
                    P.op('pool', lambda e, k=k: e.tensor_copy(dst_fn(k), d32(k)), reads=[R_d32[k]], writes=[R_dst[k]])

        def mk_norm_tmp(ph, pfx):
            t = {'sq': [SB(f"{pfx}sq{i}", [128, 512], BF16, ph) for i in range(3)], 'R_sq': [Res() for _ in range(3)], 'sqi': [0],
                 'rs': SB(f"{pfx}rs", [128, 512], F32, ph), 'R_rs': Res(),
                 't': [SB(f"{pfx}t{i}", [128, 512], F32, ph) for i in range(2)], 'R_t': [Res() for _ in range(2)], 'ti': [0],
                 'eps': SB(f"{pfx}eps", [128, 1], F32, ph)}
            P.op('dve', lambda e: e.memset(t['eps'][:], EPS), writes=[t['R_rs']])
            return t

        def moe_phase(l):
            gtf = lambda m: mod[:, l, 40 + m, 0:1]
            with ExitStack() as pm:
                hf = SB("hf", [128, 8, S], BF16, pm); R_hf = [[Res() for _ in range(4)] for _ in range(8)]
                with ExitStack() as ph:
                    hf32 = SB("hf32", [128, 8, 512], F32, ph); R_hf32 = [Res() for _ in range(8)]
                    GT = SB("GT", [NE, S], F32, ph); R_GT = [Res() for _ in range(4)]
                    GTs = SB("GTs", [NE, S], F32, ph); R_GTs = Res()
                    rw = SB("rw", [128, 8, NE], F32, ph); R_rw = Res()
                    B2 = SB("B2", [NE, D], F32, ph); R_B2 = Res()
                    rb = SB("rb", [128, NE], F32, ph); R_rb = Res()
                    lg = SB("lg", [128, 16, NE], F32, ph); R_lg = [Res() for _ in range(16)]
                    Gt = SB("Gt", [128, 16, NE], F32, ph)
                    sm = SB("sm", [128, 16, 16], F32, ph)
                    ntmp = mk_norm_tmp(ph, f"m{l}")
                    PSx = ph.enter_context(nc.psum_tensor(f"L{P.phase}_psx", [128, 512], F32)); R_psx = Res()
                    PSy = [ph.enter_context(nc.psum_tensor(f"L{P.phase}_psy{i}", [128, 512], F32)) for i in range(2)]; R_psy = [Res() for _ in range(2)]
                    PSo = [ph.enter_context(nc.psum_tensor(f"L{P.phase}_m1pso{i}", [128, 512], F32)) for i in range(2)]; R_pso = [Res() for _ in range(2)]
                    P.dma('sp', rw[:], r_w[l].rearrange("(k p) n -> p k n", p=128), cs_ld, writes=[R_rw])
                    P.dma('sp', B2[:], b2[l], cs_ld, writes=[R_B2])
                    P.dma('sp', rb[:], rowsm_d[0:1, l * 32:(l + 1) * 32].partition_broadcast(128), cs_ld, writes=[R_rb])
                    ada_items = ada_layer(1, ph, 2, 256, lazy=True) if l == 0 else []
                    for tt in range(4):
                        tok = slice(tt * 512, (tt + 1) * 512)
                        norm_mod(ph, lambda k, tok=tok: hT[:, k, tok], 512,
                                 lambda k: der[:, l, 1, k, 0:1], lambda k: mod[:, l, 24 + k, 0:1],
                                 lambda k, tok=tok: hf[:, k, tok], [R_h[k][tt] for k in range(8)], [R_hf[k][tt] for k in range(8)],
                                 PSx, R_psx, ntmp, also32=(lambda k: hf32[:, k, :], R_hf32))
                        def sub_chain(sub):
                            i = tt * 4 + sub; yi = i % 2
                            for k in range(8):
                                P.op('pe', lambda e, k=k, sub=sub, yi=yi: e.matmul(PSy[yi][:, 0:NE], hf32[:, k, sub * 128:(sub + 1) * 128], rw[:, k, :],
                                                                                  start=(k == 0), stop=(k == 7)),
                                     reads=[R_hf32[k], R_rw], writes=[R_psy[yi]])
                            P.op('dve', lambda e, i=i, yi=yi: e.tensor_tensor(lg[:, i, :], PSy[yi][:, 0:NE], rb[:], op=ALU.add),
                                 reads=[R_psy[yi], R_rb], writes=[R_lg[i]])
                            P.op('dve', lambda e, i=i: e.max(out=sm[:, i, 0:8], in_=lg[:, i, :]), reads=[R_lg[i]], writes=[R_lg[i]])
                            P.op('dve', lambda e, i=i: e.tensor_scalar(sm[:, i, 8:9], sm[:, i, 0:1], -1.0, None, op0=ALU.mult),
                                 reads=[R_lg[i]], writes=[R_lg[i]])
                            P.op('act', lambda e, i=i: e.activation(out=Gt[:, i, :], in_=lg[:, i, :], func=AF.Exp, bias=sm[:, i, 8:9], scale=1.0),
                                 reads=[R_lg[i]], writes=[R_lg[i]])
                            P.op('dve', lambda e, i=i: e.scalar_tensor_tensor(out=Gt[:, i, :], in0=lg[:, i, :], scalar=sm[:, i, 3:4], in1=Gt[:, i, :],
                                                                                op0=ALU.is_ge, op1=ALU.mult),
                                 reads=[R_lg[i]], writes=[R_lg[i]])
                            P.op('dve', lambda e, i=i: e.reduce_sum(out=sm[:, i, 9:10], in_=Gt[:, i, :], axis=AX.X), reads=[R_lg[i]], writes=[R_lg[i]])
                            P.op('dve', lambda e, i=i: e.reciprocal(sm[:, i, 10:11], sm[:, i, 9:10]), reads=[R_lg[i]], writes=[R_lg[i]])
                            P.op('dve', lambda e, i=i: e.tensor_scalar(Gt[:, i, :], Gt[:, i, :], sm[:, i, 10:11], None, op0=ALU.mult),
                                 reads=[R_lg[i]], writes=[R_lg[i]])
                            P.op('pe', lambda e, i=i, yi=yi: e.transpose(out=PSy[yi][0:NE, 128:256], in_=Gt[:, i, :], identity=ident_f),
                                 reads=[R_lg[i], R_consts], writes=[R_psy[yi]])
                            P.op('act', lambda e, i=i, yi=yi: e.copy(out=GT[:, i * 128:(i + 1) * 128], in_=PSy[yi][0:NE, 128:256]),
                                 reads=[R_psy[yi]], writes=[R_GT[tt]])
                        for pair in ((0, 1), (2, 3)):
                            bufs = [P.record(lambda s_=s_: sub_chain(s_)) for s_ in pair]
                            for j in range(max(len(b) for b in bufs)):
                                for b in bufs:
                                    if j < len(b):
                                        P.op(b[j][0], b[j][1], reads=b[j][2], writes=b[j][3])
                            for _ in pair:
                                if ada_items: ada_items.pop(0)()
                    dump(f'hf{l}', hf[:], sum(R_hf, []))
                    dump(f'G{l}', Gt[:].rearrange("p i e -> p (i e)"), R_lg)
                    P.op('dve', lambda e: e.tensor_scalar(GTs[:], GT[:], 1.0 / 1.702, None, op0=ALU.mult), reads=R_GT, writes=[R_GTs])
                    P.dma('sp', gt_scr[l], GTs[:], reads=[R_GTs])
                    GTm = SB("GTm", [NE, S], BF16, ph); R_GTm = Res()
                    P.op('dve', lambda e: e.tensor_scalar(GTm[:], GT[:], 0.0, None, op0=ALU.is_gt), reads=R_GT, writes=[R_GTm])
                    P.dma('sp', mk_scr[l], GTm[:], reads=[R_GTm])
                    while ada_items: ada_items.pop(0)()
                    for tt in range(4):
                        tok = slice(tt * 512, (tt + 1) * 512)
                        for m in range(8):
                            oi = (tt * 8 + m) % 2
                            P.op('pe', lambda e, m=m, oi=oi, tok=tok: e.matmul(PSo[oi][:], B2[:, m * 128:(m + 1) * 128], GT[:, tok], start=True, stop=True),
                                 reads=[R_B2, R_GT[tt]], writes=[R_pso[oi]])
                            P.op('dve', lambda e, m=m, oi=oi, tok=tok: e.scalar_tensor_tensor(out=hT[:, m, tok], in0=PSo[oi][:], scalar=gtf(m), in1=hT[:, m, tok],
                                                                                               op0=ALU.mult, op1=ALU.add),
                                 reads=[R_pso[oi], R_mod, R_h[m][tt]], writes=[R_h[m][tt]])
                    P.flush()
                with ExitStack() as ph:
                    act = SB("actT", [128, 8, S], BF16, ph); R_act = [[Res() for _ in range(4)] for _ in range(8)]
                    W1s = [SB(f"W1s{i}", [128, 8, 2, 512], BF16, ph) for i in range(2)]; R_W1 = [[Res(), Res()] for _ in range(2)]
                    W2q = [SB(f"W2q{i}", [128, 8, 256], BF16, ph) for i in range(3)]; R_W2 = [Res() for _ in range(3)]
                    nq = [0]
                    w1sem = [None] * 2; w2sem = [None] * 2
                    hfm = [SB(f"hfm{i}", [128, 8, 512], BF16, ph) for i in range(2)]; R_hfm = [[Res() for _ in range(8)] for _ in range(2)]
                    maskt = SB("maskt", [128, 512], BF16, ph); R_maskt = Res()
                    sg = [SB(f"sg_{i}", [128, 512], F32, ph) for i in range(2)]; R_sg = [Res() for _ in range(2)]
                    u0 = [SB(f"u0_{i}", [128, 512], F32, ph) for i in range(2)]; R_u0 = [Res() for _ in range(2)]
                    b1e = [SB(f"b1e{i}", [128, 16], F32, ph) for i in range(2)]; R_b1e = [Res() for _ in range(2)]
                    PSg = [ph.enter_context(nc.psum_tensor(f"L{P.phase}_psg{i}", [128, 512], F32)) for i in range(2)]; R_psg = [Res() for _ in range(2)]
                    PSu = [ph.enter_context(nc.psum_tensor(f"L{P.phase}_psu{i}", [128, 512], F32)) for i in range(2)]; R_psu = [Res() for _ in range(2)]
                    PSo = [ph.enter_context(nc.psum_tensor(f"L{P.phase}_pso{i}", [128, 512], F32)) for i in range(4)]; R_pso = [Res() for _ in range(4)]

                    def load_w1(e, half):
                        for two in range(2):
                            src = w1[l, e].rearrange("(k p) (two h j) -> p k two h j", p=128, two=2, h=2)[:, :, two, half, :]
                            P.dma('pool', W1s[half][:, :, two, :], src, w1sem[half], writes=[R_W1[half][two]])
                    def load_w2q(e, q):
                        slot = (e * 4 + q) % 3
                        src = w2[l, e].rearrange("(k p) (q j) -> p k q j", p=128, q=4)[:, :, q, :]
                        P.dma('pool', W2q[slot][:], src, None, writes=[R_W2[slot]])

                    def load_b1(e):
                        eb_ = e % 2
                        P.dma('sp', b1e[eb_][:], b1c_d[l][:, e * 16:(e + 1) * 16], writes=[R_b1e[eb_]])
                        P.op('dve', lambda e_: e_.tensor_scalar(b1e[eb_][:, 8:16], b1e[eb_][:, 8:16], 1.0, None, op0=ALU.add), reads=[R_b1e[eb_]], writes=[R_b1e[eb_]])
                    load_b1(0)
                    load_w1(0, 0); load_w1(0, 1)
                    cnt = {'g': 0, 'o': 0, 'b': 0}
                    n_exp = DEBUG.get('n_exp', NE)
                    Gb = [SB(f"Gb{i}", [128, 512], F32, ph) for i in range(2)]; R_Gb = [Res() for _ in range(2)]
                    pend = [None]
                    def flush_tail():
                        if pend[0] is not None:
                            pend[0](); pend[0] = None
                    groups = [(e_, tt) for e_ in range(n_exp) for tt in range(4)]
                    def prep(n):
                        e_, tt = groups[n]; r = n % 2
                        tok = slice(tt * 512, (tt + 1) * 512)
                        P.dma('sp', maskt[:], mk_scr[l, e_:e_ + 1, tok].partition_broadcast(128), writes=[R_maskt])
                        return [lambda k=k: P.op('dve', lambda e: e.tensor_tensor(hfm[r][:, k, :], hf[:, k, tok], maskt[:], op=ALU.mult),
                                                 reads=[R_hf[k][tt], R_maskt], writes=[R_hfm[r][k]]) for k in range(8)]
                    for it in prep(0): it()
                    for n_, (e_, tt) in enumerate(groups):
                        if tt == 0:
                            load_w2q(e_, 0); load_w2q(e_, 1); load_w2q(e_, 2)
                            if e_ + 1 < n_exp: load_b1(e_ + 1)
                        pitems = prep(n_ + 1) if n_ + 1 < len(groups) else []
                        hr = n_ % 2
                        if True:
                            if True:
                                tok = slice(tt * 512, (tt + 1) * 512)
                                gi = cnt['b'] % 2; cnt['b'] += 1
                                P.dma('sp', Gb[gi][:], gt_scr[l, e_:e_ + 1, tok].partition_broadcast(128), writes=[R_Gb[gi]])
                                for ui in range(8):
                                    half = ui // 4; cc = ui % 4
                                    c = ui
                                    pi = cnt['g'] % 2; cnt['g'] += 1
                                    for it in pitems[ui:ui + 1]: it()
                                    for k in range(8):
                                        P.op('pe', lambda e, k=k, cc=cc, pi=pi, half=half, hr=hr: e.matmul(
                                            PSg[pi][:], W1s[half][:, k, 0, cc * 128:(cc + 1) * 128], hfm[hr][:, k, :], start=(k == 0), stop=(k == 7)),
                                            reads=[R_W1[half][0], R_hfm[hr][k]], writes=[R_psg[pi]])
                                    for k in range(8):
                                        P.op('pe', lambda e, k=k, cc=cc, pi=pi, half=half, hr=hr: e.matmul(
                                            PSu[pi][:], W1s[half][:, k, 1, cc * 128:(cc + 1) * 128], hfm[hr][:, k, :], start=(k == 0), stop=(k == 7)),
                                            reads=[R_W1[half][1], R_hfm[hr][k]], writes=[R_psu[pi]])
                                    eb = e_ % 2
                                    bg = b1e[eb][:, c:c + 1]
                                    bu = b1e[eb][:, 8 + c:9 + c]
                                    P.op('act', lambda e, pi=pi, bu=bu: e.activation(out=u0[pi][:], in_=PSu[pi][:], func=AF.Identity, bias=bu, scale=1.0),
                                         reads=[R_psu[pi], R_b1e[eb]], writes=[R_u0[pi]])
                                    P.op('dve', lambda e, pi=pi, bg=bg: e.tensor_scalar(sg[pi][:], PSg[pi][:], bg, 7.0, op0=ALU.add, op1=ALU.min),
                                         reads=[R_psg[pi], R_b1e[eb]], writes=[R_sg[pi]])
                                    P.op('act', lambda e, pi=pi: e.activation(out=sg[pi][:], in_=sg[pi][:], func=AF.Silu, scale=1.702),
                                         reads=[R_sg[pi]], writes=[R_sg[pi]])
                                    P.op('dve', lambda e, pi=pi: e.tensor_scalar(u0[pi][:], u0[pi][:], -6.0, 8.0, op0=ALU.max, op1=ALU.min),
                                         reads=[R_u0[pi]], writes=[R_u0[pi]])
                                    flush_tail()
                                    def tail(pi=pi, gi=gi, c=c, tok=tok, tt=tt):
                                        P.op('dve', lambda e: e.tensor_tensor(u0[pi][:], u0[pi][:], sg[pi][:], op=ALU.mult),
                                             reads=[R_sg[pi], R_u0[pi]], writes=[R_u0[pi]])
                                        P.op('dve', lambda e: e.tensor_tensor(act[:, c, tok], u0[pi][:], Gb[gi][:], op=ALU.mult),
                                             reads=[R_u0[pi], R_Gb[gi]], writes=[R_act[c][tt]])
                                    pend[0] = tail
                                    if tt == 3 and cc == 3 and e_ + 1 < n_exp:
                                        load_w1(e_ + 1, half)
                        if tt != 3: continue
                        flush_tail()
                        for q in range(4):
                            slot = (e_ * 4 + q) % 3
                            for tt2 in range(4):
                                tok = slice(tt2 * 512, (tt2 + 1) * 512)
                                for mm_ in range(2):
                                    m = q * 2 + mm_
                                    oi = cnt['o'] % 4; cnt['o'] += 1
                                    for k in range(8):
                                        P.op('pe', lambda e, k=k, mm_=mm_, oi=oi, slot=slot, tok=tok: e.matmul(
                                            PSo[oi][:], W2q[slot][:, k, mm_ * 128:(mm_ + 1) * 128], act[:, k, tok], start=(k == 0), stop=(k == 7)),
                                            reads=[R_W2[slot], R_act[k][tt2]], writes=[R_pso[oi]])
                                    P.op('dve', lambda e, m=m, oi=oi, tok=tok: e.scalar_tensor_tensor(out=hT[:, m, tok], in0=PSo[oi][:], scalar=gtf(m), in1=hT[:, m, tok],
                                                                                                       op0=ALU.mult, op1=ALU.add),
                                         reads=[R_pso[oi], R_mod, R_h[m][tt2]], writes=[R_h[m][tt2]])
                            if q == 0: load_w2q(e_, 3)
                    dump(f'h_moe{l}', hT[:], sum(R_h, []))
                    P.flush()

        def mixer0():
            with ExitStack() as pa:
                u_bf = SB("u_bf", [128, 4, S + 30], BF16, pa); R_u = [[Res() for _ in range(4)] for _ in range(4)]; R_upad = Res()
                qT = SB("qT", [128, 16, 512], BF16, pa); R_qT = [Res() for _ in range(16)]
                kT = SB("kT", [128, S + L], BF16, pa); R_kT = [Res() for _ in range(18)]
                vtok = SB("vtok", [128, 18, 128], BF16, pa); R_v = [Res() for _ in range(18)]
                rows0 = SB("rows0", [128, NROW0], F32, pa); R_rows0 = Res()
                es_t = SB("es_t", [128, 4], F32, pa); R_es = Res()
                with ExitStack() as ph:
                    hm = [SB(f"hm{i}", [128, 8, 512], BF16, ph) for i in range(2)]; R_hm = [[Res() for _ in range(8)] for _ in range(2)]
                    cT = SB("cT", [128, 8, L], F32, ph); R_cT = [Res() for _ in range(8)]
                    w_in = SB("w_in", [128, 8, 1792], BF16, ph); R_win = Res()
                    cs_t = SB("cs_t", [128, NCS], F32, ph); R_cs = Res()
                    G10 = SB("G10", [128, 10, 64], F32, ph); R_G10 = Res()
                    cos_t = cs_t[:, 0:512].rearrange("p (i f) -> p i f", f=32)
                    sin_t = cs_t[:, 512:1024].rearrange("p (i f) -> p i f", f=32)
                    ntmp = mk_norm_tmp(ph, "a1")
                    qkv = SB("qkv", [128, 768], F32, ph); R_qkv = Res()
                    sqq = SB("sqq", [128, 640], F32, ph); R_sqq = Res()
                    st10 = SB("st10", [128, 16], F32, ph); R_st10 = Res()
                    qn = SB("qn", [128, 10, 64], F32, ph); R_qn = Res()
                    ra = SB("ra", [128, 10, 32], F32, ph); R_ra = Res()
                    rbb = SB("rbb", [128, 10, 32], F32, ph); R_rbb = Res()
                    qr = SB("qr", [128, 10, 64], F32, ph); R_qr = Res()
                    sgl = [SB(f"sgl{i}", [128, 512], F32, ph) for i in range(2)]; R_sgl = [Res() for _ in range(2)]
                    PSs = ph.enter_context(nc.psum_tensor(f"L{P.phase}_a1pss", [128, 512], F32)); R_pss = Res()
                    PSa = [ph.enter_context(nc.psum_tensor(f"L{P.phase}_a1psa{i}", [128, 512], F32)) for i in range(4)]; R_psa = [Res() for _ in range(4)]
                    PSq = ph.enter_context(nc.psum_tensor(f"L{P.phase}_a1psq", [128, 512], F32)); R_psq = Res()
                    PSk = ph.enter_context(nc.psum_tensor(f"L{P.phase}_a1psk", [128, 512], F32)); R_psk = Res()
                    PSt = ph.enter_context(nc.psum_tensor(f"L{P.phase}_a1pst", [128, 512], F32)); R_pst = Res()
                    for k in range(8):
                        P.dma('sp', cT[:, k, :], ctxT[k * 128:(k + 1) * 128, :], cs_ld, writes=[R_cT[k]])
                    P.dma('sp', rows0[:], rows0_d[0:1, :].partition_broadcast(128), cs_ld, writes=[R_rows0])
                    P.dma('sp', cs_t[:], cs_d[:, :], cs_ld, writes=[R_cs])
                    P.dma('pool', w_in[:], ev_w_in.rearrange("(k p) n -> p k n", p=128), wA, writes=[R_win])
                    P.op('dve', lambda e: e.memset(u_bf[:, :, 0:15], 0.0), writes=[R_upad])
                    P.op('dve', lambda e: e.memset(u_bf[:, :, S + 15:S + 30], 0.0), writes=[R_upad])
                    P.op('dve', lambda e: e.tensor_copy(G10[:, 0:8, :], rows0[:, 768:832].unsqueeze(1).to_broadcast([128, 8, 64])), reads=[R_rows0], writes=[R_G10])
                    P.op('dve', lambda e: e.tensor_copy(G10[:, 8:10, :], rows0[:, 832:896].unsqueeze(1).to_broadcast([128, 2, 64])), reads=[R_rows0], writes=[R_G10])
                    P.op('act', lambda e: e.activation(out=es_t[0:64, :], in_=rows0[0:64, 896:900], func=AF.Exp), reads=[R_rows0], writes=[R_es])
                    P.op('act', lambda e: e.activation(out=es_t[64:128, :], in_=rows0[64:128, 900:904], func=AF.Exp), reads=[R_rows0], writes=[R_es])
                    na = 0
                    A1C = DEBUG.get('a1', 99)
                    for tt in range(5 if A1C >= 9 else (1 if A1C >= 1 else 0)):
                        hb = tt % 2
                        hmt = hm[hb]
                        if tt < 4:
                            tok = slice(tt * 512, (tt + 1) * 512)
                            norm_mod(ph, lambda k, tok=tok: hT[:, k, tok], 512, lambda k: der[:, 0, 0, k, 0:1], lambda k: mod[:, 0, k, 0:1],
                                     lambda k, hmt=hmt: hmt[:, k, :], [R_h[k][tt] for k in range(8)], R_hm[hb], PSs, R_pss, ntmp)
                        else:
                            norm_mod(ph, lambda k: cT[:, k, :], L, lambda k: der[:, 0, 0, k, 1:2], lambda k: mod[:, 0, k, 1:2],
                                     lambda k, hmt=hmt: hmt[:, k, 0:L], R_cT, R_hm[hb], PSs, R_pss, ntmp)
                        if tt == 0: dump('hm0', hmt[:], R_hm[hb])
                        if A1C == 1: break
                        if tt < 4:
                            for c in range(4):
                                pu = na % 4; pg = (na + 1) % 4; si = (na // 2) % 2; na += 2
                                for k in range(8):
                                    P.op('pe', lambda e, k=k, c=c, pu=pu, hmt=hmt: e.matmul(PSa[pu][:], w_in[:, k, c * 128:(c + 1) * 128], hmt[:, k, :], start=(k == 0), stop=(k == 7)),
                                         reads=[R_win, R_hm[hb][k]], writes=[R_psa[pu]])
                                for k in range(8):
                                    P.op('pe', lambda e, k=k, c=c, pg=pg, hmt=hmt: e.matmul(PSa[pg][:], w_in[:, k, 512 + c * 128:512 + (c + 1) * 128], hmt[:, k, :], start=(k == 0), stop=(k == 7)),
                                         reads=[R_win, R_hm[hb][k]], writes=[R_psa[pg]])
                                P.op('act', lambda e, c=c, pg=pg, si=si: e.activation(out=sgl[si][:], in_=PSa[pg][:], func=AF.Sigmoid, bias=V('ev_bin_a', 4 + c, 1), scale=1.0),
                                     reads=[R_psa[pg], R_vecs], writes=[R_sgl[si]])
                                P.op('dve', lambda e, c=c, pu=pu, si=si, tt=tt: e.scalar_tensor_tensor(
                                    out=u_bf[:, c, 15 + tt * 512:15 + (tt + 1) * 512], in0=PSa[pu][:], scalar=V('ev_bin_a', c, 1), in1=sgl[si][:], op0=ALU.add, op1=ALU.mult),
                                    reads=[R_psa[pu], R_sgl[si], R_vecs], writes=[R_u[c][tt]])
                        nsub = 4 if tt < 4 else 2
                        if A1C == 2: break
                        if A1C < 9: nsub = 1
                        for sub in range(nsub):
                            i = tt * 4 + sub
                            tokc = slice(i * 128, (i + 1) * 128)
                            lc = slice(sub * 128, (sub + 1) * 128)
                            main = tt < 4
                            if main:
                                for k in range(8):
                                    P.op('pe', lambda e, k=k, lc=lc, hmt=hmt: e.matmul(PSq[:], hmt[:, k, lc], w_in[:, k, 1024:1536], start=(k == 0), stop=(k == 7)),
                                         reads=[R_win, R_hm[hb][k]], writes=[R_psq])
                            for k in range(8):
                                P.op('pe', lambda e, k=k, lc=lc, hmt=hmt: e.matmul(PSk[:, 0:256], hmt[:, k, lc], w_in[:, k, 1536:1792], start=(k == 0), stop=(k == 7)),
                                     reads=[R_win, R_hm[hb][k]], writes=[R_psk])
                            if main:
                                P.op('dve', lambda e: e.tensor_tensor(qkv[:, 0:512], PSq[:], rows0[:, 0:512], op=ALU.add),
                                     reads=[R_psq, R_rows0], writes=[R_qkv])
                            P.op('dve', lambda e: e.tensor_tensor(qkv[:, 512:768], PSk[:, 0:256], rows0[:, 512:768], op=ALU.add),
                                 reads=[R_psk, R_rows0], writes=[R_qkv])
                            P.op('act', lambda e, i=i: e.copy(out=vtok[:, i, :], in_=qkv[:, 640:768]), reads=[R_qkv], writes=[R_v[i]])
                            h0 = 0 if main else 8
                            nh = 10 - h0
                            c0 = h0 * 64
                            P.op('pool', lambda e, c0=c0: e.tensor_tensor(sqq[:, c0:640], qkv[:, c0:640], qkv[:, c0:640], op=ALU.mult),
                                 reads=[R_qkv], writes=[R_sqq])
                            P.op('dve', lambda e, h0=h0, c0=c0: e.tensor_reduce(out=st10[:, h0:10], in_=sqq[:, c0:640].rearrange("p (h d) -> p h d", d=64),
                                                                                 axis=AX.X, op=ALU.add),
                                 reads=[R_sqq], writes=[R_st10])
                            P.op('act', lambda e, h0=h0: e.activation(out=st10[:, h0:10], in_=st10[:, h0:10], func=AF.Sqrt, scale=1.0 / 64, bias=ntmp['eps'][:, 0:1]),
                                 reads=[R_st10], writes=[R_st10])
                            P.op('dve', lambda e, h0=h0: e.reciprocal(st10[:, h0:10], st10[:, h0:10]), reads=[R_st10], writes=[R_st10])
                            P.op('dve', lambda e, h0=h0, nh=nh, c0=c0: e.tensor_tensor(
                                qn[:, h0:10, :], qkv[:, c0:640].rearrange("p (h d) -> p h d", d=64),
                                st10[:, h0:10].unsqueeze(2).to_broadcast([128, nh, 64]), op=ALU.mult),
                                reads=[R_qkv, R_st10], writes=[R_qn])
                            if A1C == 3: break
                            if main:
                                P.op('pool', lambda e: e.tensor_tensor(qn[:], qn[:], G10[:], op=ALU.mult), reads=[R_qn, R_G10], writes=[R_qn])
                                cb_ = lambda i=i: cos_t[:, i, :].unsqueeze(1).to_broadcast([128, 10, 32])
                                sb_ = lambda i=i: sin_t[:, i, :].unsqueeze(1).to_broadcast([128, 10, 32])
                                P.op('dve', lambda e, cb_=cb_: e.tensor_tensor(ra[:], qn[:, :, 0:32], cb_(), op=ALU.mult), reads=[R_qn, R_cs], writes=[R_ra])
                                P.op('pool', lambda e, sb_=sb_: e.tensor_tensor(rbb[:], qn[:, :, 32:64], sb_(), op=ALU.mult), reads=[R_qn, R_cs], writes=[R_rbb])
                                qperm = lambda lo, hi: qr[:, 0:8, lo:hi].rearrange("p (g kv) d -> p kv g d", kv=2)
                                v4 = lambda t: t[:, 0:8, :].rearrange("p (kv g) d -> p kv g d", kv=2)
                                P.op('dve', lambda e, qperm=qperm, v4=v4: e.tensor_tensor(qperm(0, 32), v4(ra), v4(rbb), op=ALU.subtract), reads=[R_ra, R_rbb], writes=[R_qr])
                                P.op('dve', lambda e: e.tensor_tensor(qr[:, 8:10, 0:32], ra[:, 8:10, :], rbb[:, 8:10, :], op=ALU.subtract), reads=[R_ra, R_rbb], writes=[R_qr])
                                P.op('dve', lambda e, cb_=cb_: e.tensor_tensor(ra[:], qn[:, :, 32:64], cb_(), op=ALU.mult), reads=[R_qn, R_cs], writes=[R_ra])
                                P.op('pool', lambda e, sb_=sb_: e.tensor_tensor(rbb[:], qn[:, :, 0:32], sb_(), op=ALU.mult), reads=[R_qn, R_cs], writes=[R_rbb])
                                P.op('dve', lambda e, qperm=qperm, v4=v4: e.tensor_tensor(qperm(32, 64), v4(ra), v4(rbb), op=ALU.add), reads=[R_ra, R_rbb], writes=[R_qr])
                                P.op('dve', lambda e: e.tensor_tensor(qr[:, 8:10, 32:64], ra[:, 8:10, :], rbb[:, 8:10, :], op=ALU.add), reads=[R_ra, R_rbb], writes=[R_qr])
                                if A1C == 4: break
                                for g in range(4):
                                    P.op('pe', lambda e, g=g: e.transpose(
                                        out=PSt[:, g * 128:(g + 1) * 128],
                                        in_=qr[:, 2 * g:2 * g + 2, :], identity=ident_f),
                                        reads=[R_qr, R_consts], writes=[R_pst])
                                P.op('pe', lambda e: e.transpose(out=PSk[:, 384:512], in_=qr[:, 8:10, :], identity=ident_f),
                                     reads=[R_qr, R_consts], writes=[R_psk])
                                P.op('act', lambda e, i=i: e.copy(out=qT[:, i, :], in_=PSt[:, 0:512]),
                                     reads=[R_pst], writes=[R_qT[i]])
                                P.op('dve', lambda e, tokc=tokc: e.tensor_copy(kT[:, tokc], PSk[:, 384:512]), reads=[R_psk], writes=[R_kT[i]])
                            else:
                                P.op('pool', lambda e: e.tensor_tensor(qr[:, 8:10, :], qn[:, 8:10, :], G10[:, 8:10, :], op=ALU.mult),
                                     reads=[R_qn, R_G10], writes=[R_qr])
                                P.op('pe', lambda e: e.transpose(out=PSk[:, 384:512], in_=qr[:, 8:10, :], identity=ident_f),
                                     reads=[R_qr, R_consts], writes=[R_psk])
                                P.op('dve', lambda e, tokc=tokc: e.tensor_copy(kT[:, tokc], PSk[:, 384:512]), reads=[R_psk], writes=[R_kT[i]])
                    dump('u', u_bf[:], sum(R_u, []))
                    dump('qT', qT[:].rearrange('p i f -> p (i f)'), R_qT); dump('kT', kT[:], R_kT); dump('v', vtok[:], R_v)
                    P.flush()
                if DEBUG.get('sub', 9) <= 1: return
                with ExitStack() as pb:
                    chT = SB("chT", [128, 4, S], BF16, pb); R_ch = [[Res() for _ in range(4)] for _ in range(4)]
                    atT = SB("atT", [128, 4, S], BF16, pb); R_at = [Res() for _ in range(16)]
                    with ExitStack() as ph:
                        acc = SB("acc", [128, 4, 512], F32, ph); R_acc = [Res() for _ in range(4)]
                        sq4 = SB("sq4", [128, 4, 512], F32, ph); R_sq4 = [Res() for _ in range(4)]
                        mean = SB("mean", [128, 512], F32, ph); R_mean = Res()
                        var = SB("var", [128, 512], F32, ph); R_var = Res()
                        m2 = SB("m2", [128, 512], F32, ph); R_m2 = Res()
                        tl = [SB(f"tl{i}", [128, 512], F32, ph) for i in range(2)]; R_tl = [Res() for _ in range(2)]
                        epsb = SB("epsb", [128, 1], F32, ph); R_epsb = Res()
                        Eb = [SB(f"Eb{i}", [128, 512], BF16, ph) for i in range(6)]; R_Eb = [Res() for _ in range(6)]
                        den = SB("den", [128, 512], F32, ph); R_den = Res()
                        PSm = ph.enter_context(nc.psum_tensor(f"L{P.phase}_a2psm", [128, 512], F32)); R_psm = Res()
                        PSv = ph.enter_context(nc.psum_tensor(f"L{P.phase}_a2psv", [128, 512], F32)); R_psv = Res()
                        PSs2 = [ph.enter_context(nc.psum_tensor(f"L{P.phase}_a2pss{i}", [128, 512], F32)) for i in range(2)]; R_pss2 = [Res() for _ in range(2)]
                        PSo2 = [ph.enter_context(nc.psum_tensor(f"L{P.phase}_a2pso{i}", [128, 512], F32)) for i in range(2)]; R_pso2 = [Res() for _ in range(2)]
                        PSd2 = [ph.enter_context(nc.psum_tensor(f"L{P.phase}_a2psd{i}", [128, 512], F32)) for i in range(2)]; R_psd2 = [Res() for _ in range(2)]
                        P.op('dve', lambda e: e.memset(epsb[:], EPS), writes=[R_epsb])
                        jobs = []
                        for i in range(16):
                            blocks = [(jb, m_) for jb, m_ in ((i - 1, 0), (i, None), (i + 1, 1)) if 0 <= jb < 16] + [(16, None), (17, None)]
                            for kv in range(2):
                                for bi_, (jb, m_) in enumerate(blocks):
                                    jobs.append(dict(i=i, kv=kv, jb=jb, m=m_, first=(bi_ == 0), last=(bi_ == len(blocks) - 1), n=len(jobs)))
                        def job_scores(J):
                            i, kv, jb = J['i'], J['kv'], J['jb']; si = J['n'] % 2
                            pr = slice(kv * 64, (kv + 1) * 64)
                            P.op('pe', lambda e: e.matmul(PSs2[si][:], kT[pr, jb * 128:(jb + 1) * 128], qT[pr, i, :], start=True, stop=True),
                                 reads=[R_kT[jb], R_qT[i]], writes=[R_pss2[si]])
                        def job_rest(J):
                            i, kv, jb, m_ = J['i'], J['kv'], J['jb'], J['m']; si = J['n'] % 2; ei = J['n'] % 6; pi = i % 2
                            pr = slice(kv * 64, (kv + 1) * 64)
                            first, last = J['first'], J['last']
                            P.op('act', lambda e: e.activation(out=Eb[ei][:], in_=PSs2[si][:], func=AF.Exp, scale=0.125),
                                 reads=[R_pss2[si]], writes=[R_Eb[ei]])
                            if m_ is not None:
                                P.op('pool', lambda e: e.tensor_tensor(
                                    Eb[ei][:].rearrange("p (g t) -> p g t", g=4), Eb[ei][:].rearrange("p (g t) -> p g t", g=4),
                                    maskb[:, m_ * 128:(m_ + 1) * 128].unsqueeze(1).to_broadcast([128, 4, 128]), op=ALU.mult),
                                    reads=[R_Eb[ei], R_k], writes=[R_Eb[ei]])
                            P.op('pe', lambda e: e.matmul(PSo2[pi][pr, :], vtok[:, jb, kv * 64:(kv + 1) * 64], Eb[ei][:], start=first, stop=last),
                                 reads=[R_v[jb], R_Eb[ei]], writes=[R_pso2[pi]])
                            P.op('pe', lambda e: e.matmul(PSd2[pi][pr, :], ones_bf[:, 0:64], Eb[ei][:], start=first, stop=last),
                                 reads=[R_k, R_Eb[ei]], writes=[R_psd2[pi]])
                            if last and kv == 1:
                                tokq = slice(i * 128, (i + 1) * 128)
                                P.op('dve', lambda e: e.tensor_tensor(den[:].rearrange("p (g t) -> p g t", g=4), PSd2[pi][:].rearrange("p (g t) -> p g t", g=4),
                                                                       es_t[:].unsqueeze(2).to_broadcast([128, 4, 128]), op=ALU.add),
                                     reads=[R_psd2[pi], R_es], writes=[R_den])
                                P.op('dve', lambda e: e.reciprocal(den[:], den[:]), reads=[R_den], writes=[R_den])
                                P.op('dve', lambda e: e.tensor_tensor(atT[:, :, tokq], PSo2[pi][:].rearrange("p (g t) -> p g t", g=4),
                                                                      den[:].rearrange("p (g t) -> p g t", g=4), op=ALU.mult),
                                     reads=[R_pso2[pi], R_den], writes=[R_at[i]])
                                return True
                            return False

                        ptmp = SB("ptmp", [128, 512], F32, ph); R_ptmp = Res()
                        def conv_taps(tt):
                            ops = []
                            for tap in range(31):
                                for c in range(4):
                                    src = u_bf[:, c, tt * 512 + tap: tt * 512 + tap + 512]
                                    wcol = V('conv_w', c * 31 + tap, 1)
                                    rd = [R_u[c][t2] for t2 in range(max(0, tt - 1), min(4, tt + 2))] + [R_upad, R_vecs]
                                    eng = 'dve'
                                    if tap == 0:
                                        ops.append(lambda eng=eng, c=c, src=src, wcol=wcol, rd=rd: P.op(eng, lambda e: e.tensor_scalar(
                                            acc[:, c, :], src, wcol, V('conv_b', c, 1), op0=ALU.mult, op1=ALU.add),
                                            reads=rd, writes=[R_acc[c]]))
                                    elif True:
                                        ops.append(lambda c=c, src=src, wcol=wcol, rd=rd: P.op('dve', lambda e: e.scalar_tensor_tensor(
                                            out=acc[:, c, :], in0=src, scalar=wcol, in1=acc[:, c, :], op0=ALU.mult, op1=ALU.add),
                                            reads=rd + [R_acc[c]], writes=[R_acc[c]]))
                                    else:
                                        def two(c=c, src=src, wcol=wcol, rd=rd):
                                            P.op('pool', lambda e: e.tensor_scalar(ptmp[:], src, wcol, None, op0=ALU.mult), reads=rd, writes=[R_ptmp])
                                            P.op('pool', lambda e: e.tensor_tensor(acc[:, c, :], acc[:, c, :], ptmp[:], op=ALU.add), reads=[R_ptmp, R_acc[c]], writes=[R_acc[c]])
                                        ops.append(two)
                            return ops

                        def conv_tile(tt):
                            for c in range(4):
                                P.op('act', lambda e, c=c: e.activation(out=sq4[:, c, :], in_=acc[:, c, :], func=AF.Square),
                                     reads=[R_acc[c]], writes=[R_sq4[c]])
                            for c in range(4):
                                P.op('pe', lambda e, c=c: e.matmul(PSm[:], ones_f[:], acc[:, c, :], start=(c == 0), stop=(c == 3)),
                                     reads=[R_acc[c], R_k], writes=[R_psm])
                            for c in range(4):
                                P.op('pe', lambda e, c=c: e.matmul(PSv[:], ones_f[:], sq4[:, c, :], start=(c == 0), stop=(c == 3)),
                                     reads=[R_sq4[c], R_k], writes=[R_psv])
                            P.op('act', lambda e: e.activation(out=mean[:], in_=PSm[:], func=AF.Identity, scale=1.0 / 512), reads=[R_psm], writes=[R_mean])
                            P.op('pool', lambda e: e.tensor_tensor(m2[:], mean[:], mean[:], op=ALU.mult), reads=[R_mean], writes=[R_m2])
                            P.op('dve', lambda e: e.scalar_tensor_tensor(out=var[:], in0=PSv[:], scalar=1.0 / 512, in1=m2[:], op0=ALU.mult, op1=ALU.subtract),
                                 reads=[R_psv, R_m2], writes=[R_var])
                            P.op('act', lambda e: e.activation(out=var[:], in_=var[:], func=AF.Sqrt, bias=epsb[:, 0:1], scale=1.0), reads=[R_var, R_epsb], writes=[R_var])
                            P.op('dve', lambda e: e.reciprocal(var[:], var[:]), reads=[R_var], writes=[R_var])
                            for c in range(4):
                                ti = c % 2
                                P.op('dve', lambda e, c=c, ti=ti: e.tensor_tensor(tl[ti][:], acc[:, c, :], mean[:], op=ALU.subtract),
                                     reads=[R_acc[c], R_mean], writes=[R_tl[ti]])
                                P.op('pool', lambda e, ti=ti: e.tensor_tensor(tl[ti][:], tl[ti][:], var[:], op=ALU.mult), reads=[R_tl[ti], R_var], writes=[R_tl[ti]])
                                P.op('act', lambda e, c=c, ti=ti, tt=tt: e.activation(out=chT[:, c, tt * 512:(tt + 1) * 512], in_=tl[ti][:], func=AF.Silu,
                                                                                      scale=V('cln_g', c, 1), bias=V('cln_b', c, 1)),
                                     reads=[R_tl[ti], R_vecs], writes=[R_ch[c][tt]])
                        conv_q = []
                        for tt in range(4):
                            ops = conv_taps(tt)
                            per = (len(ops) + 3) // 4
                            for qi in range(4):
                                conv_q.append((tt, qi, ops[qi * per:(qi + 1) * per]))
                        def run_conv(item):
                            tt, qi, ops = item
                            for o in ops: o()
                            if qi == 3: conv_tile(tt)
                        job_scores(jobs[0])
                        done_tiles = 0
                        for n in range(len(jobs)):
                            if n + 1 < len(jobs): job_scores(jobs[n + 1])
                            if job_rest(jobs[n]):
                                run_conv(conv_q[done_tiles]); done_tiles += 1
                        dump('chT', chT[:], sum(R_ch, [])); dump('atT', atT[:], R_at)
                        P.flush()
                    if DEBUG.get('sub', 9) <= 2: return
                    with ExitStack() as ph:
                        w_out = SB("w_out", [128, 8, D], BF16, ph); R_wo = [Res() for _ in range(3)]
                        tmp = [SB(f"a3t{i}", [128, 512], F32, ph) for i in range(2)]; R_tmp = [Res() for _ in range(2)]
                        PSo3 = [ph.enter_context(nc.psum_tensor(f"L{P.phase}_a3ps{i}", [128, 512], F32)) for i in range(2)]; R_pso3 = [Res() for _ in range(2)]
                        P.dma('pool', w_out[:, 0:4, :], ev_w_out[0:512, :].rearrange("(k p) n -> p k n", p=128), wB, writes=[R_wo[0]])
                        P.dma('pool', w_out[0:64, 4:8, :], ev_w_out[512:768, :].rearrange("(g d) n -> d g n", d=64), wB, writes=[R_wo[1]])
                        P.dma('pool', w_out[64:128, 4:8, :], ev_w_out[768:1024, :].rearrange("(g d) n -> d g n", d=64), wB, writes=[R_wo[2]])
                        n3 = 0
                        for tt in range(4):
                            tok = slice(tt * 512, (tt + 1) * 512)
                            for m in range(8):
                                oi = n3 % 2; n3 += 1
                                for c in range(4):
                                    P.op('pe', lambda e, c=c, m=m, oi=oi, tok=tok: e.matmul(PSo3[oi][:], w_out[:, c, m * 128:(m + 1) * 128], chT[:, c, tok], start=(c == 0), stop=False),
                                         reads=R_wo + [R_ch[c][tt]], writes=[R_pso3[oi]])
                                for g in range(4):
                                    P.op('pe', lambda e, g=g, m=m, oi=oi, tok=tok: e.matmul(PSo3[oi][:], w_out[:, 4 + g, m * 128:(m + 1) * 128], atT[:, g, tok], start=False, stop=(g == 3)),
                                         reads=R_wo + [R_at[tt * 4 + q_] for q_ in range(4)], writes=[R_pso3[oi]])
                                P.op('act', lambda e, m=m, oi=oi: e.activation(out=tmp[oi][:], in_=PSo3[oi][:], func=AF.Identity, scale=mod[:, 0, 16 + m, 0:1], bias=der[:, 0, 2, m, 0:1]),
                                     reads=[R_pso3[oi], R_mod, R_der], writes=[R_tmp[oi]])
                                P.op('dve', lambda e, m=m, oi=oi, tok=tok: e.tensor_tensor(hT[:, m, tok], hT[:, m, tok], tmp[oi][:], op=ALU.add),
                                     reads=[R_tmp[oi], R_h[m][tt]], writes=[R_h[m][tt]])
                        dump('h_mix0', hT[:], sum(R_h, []))
                        P.flush()

        def mixer1():
            with ExitStack() as pa:
                uT = SB("uT", [128, 8, S], BF16, pa); R_uT = [[Res() for _ in range(4)] for _ in range(8)]
                vt = SB("vt", [128, 16, D], BF16, pa); R_vt = [Res() for _ in range(16)]
                wsT = SB("wsT", [128, 8, 128], BF16, pa); R_ws = Res()
                with ExitStack() as ph:
                    wsf = SB("wsf", [128, 8, 128], F32, ph); R_wsf = Res()
                    PSt = [ph.enter_context(nc.psum_tensor(f"L{P.phase}_b0pst{i}", [128, 512], F32)) for i in range(2)]; R_pst = [Res(), Res()]
                    P.dma('sp', wsf[:], od_w_s.rearrange("g p q -> p g q"), cs_ld, writes=[R_wsf])
                    for g in range(8):
                        P.op('pe', lambda e, g=g: e.transpose(out=PSt[g // 4][:, (g % 4) * 128:(g % 4 + 1) * 128], in_=wsf[:, g, :], identity=ident_f),
                             reads=[R_wsf, R_consts], writes=[R_pst[g // 4]])
                    for hh in range(2):
                        P.op('act', lambda e, hh=hh: e.copy(out=wsT[:, hh * 4:(hh + 1) * 4, :].rearrange("p g q -> p (g q)"), in_=PSt[hh][:]), reads=[R_pst[hh]], writes=[R_ws])
                    P.flush()
                with ExitStack() as ph:
                    hm = [SB(f"hm1_{i}", [128, 8, 512], BF16, ph) for i in range(2)]; R_hm = [[Res() for _ in range(8)] for _ in range(2)]
                    w_in = SB("w_in1", [128, 8, D], BF16, ph); R_win = Res()
                    rows1 = SB("rows1", [128, 3 * D], F32, ph); R_rows1 = Res()
                    ntmp = mk_norm_tmp(ph, "b1")
                    vzs = [SB(f"vz{i}", [128, D], F32, ph) for i in range(2)]; R_vzs = [Res() for _ in range(2)]
                    bsts = [SB(f"bst{i}", [128, 2, 6], F32, ph) for i in range(2)]; R_bsts = [Res() for _ in range(2)]
                    mvs = [SB(f"mv{i}", [128, 2], F32, ph) for i in range(2)]; R_mvs = [Res() for _ in range(2)]
                    PSs = ph.enter_context(nc.psum_tensor(f"L{P.phase}_b1pss", [128, 512], F32)); R_pss = Res()
                    PSa = [ph.enter_context(nc.psum_tensor(f"L{P.phase}_b1psa{i}", [128, 512], F32)) for i in range(2)]; R_psa = [Res() for _ in range(2)]
                    PSv = [ph.enter_context(nc.psum_tensor(f"L{P.phase}_b1psv{i}", [128, 512], F32)) for i in range(4)]; R_psv = [Res() for _ in range(4)]
                    P.dma('sp', rows1[:], rows1_d[0:1, 0:3 * D].partition_broadcast(128), cs_ld, writes=[R_rows1])
                    for pas in range(2):
                        P.dma('pool', w_in[:], od_w_in[:, pas * D:(pas + 1) * D].rearrange("(k p) n -> p k n", p=128), wA, writes=[R_win])
                        na = 0
                        for tt in range(4):
                            hb = tt % 2; hmt = hm[hb]
                            tok = slice(tt * 512, (tt + 1) * 512)
                            norm_mod(ph, lambda k, tok=tok: hT[:, k, tok], 512, lambda k: der[:, 1, 0, k, 0:1], lambda k: mod[:, 1, k, 0:1],
                                     lambda k, hmt=hmt: hmt[:, k, :], [R_h[k][tt] for k in range(8)], R_hm[hb], PSs, R_pss, ntmp)
                            if pas == 0 and tt == 0: dump('hm1', hmt[:], R_hm[hb])
                            if pas == 0:
                                for c in range(8):
                                    pi = na % 2; na += 1
                                    for k in range(8):
                                        P.op('pe', lambda e, k=k, c=c, pi=pi, hmt=hmt: e.matmul(PSa[pi][:], w_in[:, k, c * 128:(c + 1) * 128], hmt[:, k, :], start=(k == 0), stop=(k == 7)),
                                             reads=[R_win, R_hm[hb][k]], writes=[R_psa[pi]])
                                    P.op('act', lambda e, c=c, pi=pi, tok=tok: e.activation(out=uT[:, c, tok], in_=PSa[pi][:], func=AF.Gelu, bias=V('od_bin_u', c, 1), scale=1.0),
                                         reads=[R_psa[pi], R_vecs], writes=[R_uT[c][tt]])
                            else:
                                for sub in range(4):
                                    i = tt * 4 + sub
                                    lc = slice(sub * 128, (sub + 1) * 128)
                                    vz = vzs[i % 2]; R_vz = R_vzs[i % 2]; bst = bsts[i % 2]; R_bst = R_bsts[i % 2]; mv = mvs[i % 2]; R_mv = R_mvs[i % 2]
                                    for hh in range(2):
                                        pv = (i * 2 + hh) % 4
                                        for k in range(8):
                                            P.op('pe', lambda e, k=k, hh=hh, pv=pv, lc=lc, hmt=hmt: e.matmul(PSv[pv][:], hmt[:, k, lc], w_in[:, k, hh * 512:(hh + 1) * 512], start=(k == 0), stop=(k == 7)),
                                                 reads=[R_win, R_hm[hb][k]], writes=[R_psv[pv]])
                                        P.op('dve', lambda e, hh=hh, pv=pv, vz=vz: e.tensor_tensor(vz[:, hh * 512:(hh + 1) * 512], PSv[pv][:], rows1[:, hh * 512:(hh + 1) * 512], op=ALU.add),
                                             reads=[R_psv[pv], R_rows1], writes=[R_vz])
                                    P.op('act', lambda e, vz=vz: e.activation(out=vz[:], in_=vz[:], func=AF.Gelu), reads=[R_vz], writes=[R_vz])
                                    for hh in range(2):
                                        P.op('dve', lambda e, hh=hh, vz=vz, bst=bst: e.bn_stats(out=bst[:, hh, :], in_=vz[:, hh * 512:(hh + 1) * 512]), reads=[R_vz], writes=[R_bst])
                                    P.op('dve', lambda e, mv=mv, bst=bst: e.bn_aggr(out=mv[:], in_=bst[:]), reads=[R_bst], writes=[R_mv])
                                    P.op('act', lambda e, mv=mv: e.activation(out=mv[:, 1:2], in_=mv[:, 1:2], func=AF.Sqrt, bias=ntmp['eps'][:, 0:1], scale=1.0), reads=[R_mv], writes=[R_mv])
                                    P.op('dve', lambda e, mv=mv: e.reciprocal(mv[:, 1:2], mv[:, 1:2]), reads=[R_mv], writes=[R_mv])
                                    P.op('dve', lambda e, vz=vz, mv=mv: e.tensor_scalar(vz[:], vz[:], mv[:, 0:1], mv[:, 1:2], op0=ALU.subtract, op1=ALU.mult),
                                         reads=[R_vz, R_mv], writes=[R_vz])
                                    P.op('pool', lambda e, vz=vz: e.tensor_tensor(vz[:], vz[:], rows1[:, 1024:2048], op=ALU.mult), reads=[R_vz, R_rows1], writes=[R_vz])
                                    P.op('pool', lambda e, i=i, vz=vz: e.tensor_tensor(vt[:, i, :], vz[:], rows1[:, 2048:3072], op=ALU.add), reads=[R_vz, R_rows1], writes=[R_vt[i]])
                    dump('uT', uT[:], sum(R_uT, [])); dump('vt', vt[:], R_vt)
                    P.flush()
                with ExitStack() as ph:
                    w_out = SB("w_out1", [128, 8, D], BF16, ph); R_wo = Res()
                    bsr = SB("bsr", [128, D], F32, ph); R_bsr = Res()
                    tmp = [SB(f"b3t{i}", [128, 512], F32, ph) for i in range(2)]; R_tmp = [Res() for _ in range(2)]
                    svb = [SB(f"svb{i}", [128, 128], F32, ph) for i in range(2)]; R_svb = [Res() for _ in range(2)]
                    PSg_ = [ph.enter_context(nc.psum_tensor(f"L{P.phase}_b2ps{i}", [128, 512], F32)) for i in range(2)]; R_psg_ = [Res() for _ in range(2)]
                    PSo3 = [ph.enter_context(nc.psum_tensor(f"L{P.phase}_b3ps{i}", [128, 512], F32)) for i in range(2)]; R_pso3 = [Res() for _ in range(2)]
                    P.dma('pool', w_out[:], od_w_out.rearrange("(k p) n -> p k n", p=128), wB, writes=[R_wo])
                    P.dma('sp', bsr[:], rows1_d[0:1, 3 * D:4 * D].partition_broadcast(128), cs_ld, writes=[R_bsr])
                    n2 = 0
                    for i in range(16):
                        tokc = slice(i * 128, (i + 1) * 128); tt = i // 4
                        for g in range(8):
                            pi = n2 % 2; n2 += 1
                            P.op('pe', lambda e, g=g, i=i, pi=pi: e.matmul(PSg_[pi][:, 0:128], vt[:, i, g * 128:(g + 1) * 128], wsT[:, g, :], start=True, stop=True),
                                 reads=[R_vt[i], R_ws], writes=[R_psg_[pi]])
                            P.op('dve', lambda e, g=g, pi=pi: e.tensor_tensor(svb[pi][:], PSg_[pi][:, 0:128], bsr[:, g * 128:(g + 1) * 128], op=ALU.add),
                                 reads=[R_psg_[pi], R_bsr], writes=[R_svb[pi]])
                            P.op('pool', lambda e, g=g, pi=pi, tokc=tokc: e.tensor_tensor(uT[:, g, tokc], uT[:, g, tokc], svb[pi][:], op=ALU.mult),
                                 reads=[R_svb[pi], R_uT[g][tt]], writes=[R_uT[g][tt]])
                    dump('prod', uT[:], sum(R_uT, []))
                    n3 = 0
                    for tt in range(4):
                        tok = slice(tt * 512, (tt + 1) * 512)
                        for m in range(8):
                            oi = n3 % 2; n3 += 1
                            for c in range(8):
                                P.op('pe', lambda e, c=c, m=m, oi=oi, tok=tok: e.matmul(PSo3[oi][:], w_out[:, c, m * 128:(m + 1) * 128], uT[:, c, tok], start=(c == 0), stop=(c == 7)),
                                     reads=[R_wo, R_uT[c][tt]], writes=[R_pso3[oi]])
                            P.op('act', lambda e, m=m, oi=oi: e.activation(out=tmp[oi][:], in_=PSo3[oi][:], func=AF.Identity, scale=mod[:, 1, 16 + m, 0:1], bias=der[:, 1, 2, m, 0:1]),
                                 reads=[R_pso3[oi], R_mod, R_der], writes=[R_tmp[oi]])
                            P.op('dve', lambda e, m=m, oi=oi, tok=tok: e.tensor_tensor(hT[:, m, tok], hT[:, m, tok], tmp[oi][:], op=ALU.add),
                                 reads=[R_tmp[oi], R_h[m][tt]], writes=[R_h[m][tt]])
                    dump('h_mix1', hT[:], sum(R_h, []))
                    P.flush()

        stages = dbg.get('_stages', None)
        stop_after = DEBUG.get('stop_after', 99)
        if stop_after >= 1 and DEBUG.get('sub', 9) >= 1: mixer0()
        if stop_after >= 2: moe_phase(0)
        if stop_after >= 3: mixer1()
        if stop_after >= 4: moe_phase(1)
        for k in range(8):
            P.dma('sp', outT[k * 128:(k + 1) * 128, :], hT[:, k, :], out_st, reads=R_h[k])
        P.flush()
        P.final_wait()
    return nc


def _rope_tables():
    rows = S // 64
    r = np.repeat(np.arange(rows), 64).astype(np.float32)
    col = np.tile(np.arange(64), rows).astype(np.float32)
    inv_freq = (np.float32(10000.0) ** (-np.arange(16, dtype=np.float32) / np.float32(16))).astype(np.float32)
    ang = np.concatenate([r[:, None] * inv_freq, col[:, None] * inv_freq], axis=-1).astype(np.float32)
    return np.cos(ang).astype(np.float32), np.sin(ang).astype(np.float32)


def _consts():
    c = np.zeros((128, NCONST), np.float32)
    c[:, 0:128] = np.eye(128, dtype=np.float32)
    j = np.arange(128)[:, None]; i = np.arange(128)[None, :]
    c[:, 128:256] = (j >= i).astype(np.float32)
    c[:, 256:384] = (j <= i).astype(np.float32)
    return c


def _cossin():
    c = np.zeros((128, NCS), np.float32)
    cos, sin = _rope_tables()
    c[:, 0:512] = cos.reshape(16, 128, 32).transpose(1, 0, 2).reshape(128, 512)
    c[:, 512:1024] = sin.reshape(16, 128, 32).transpose(1, 0, 2).reshape(128, 512)
    return c


def _cols(v):
    v = np.asarray(v, np.float32).reshape(-1, 128)
    return v.T


def _build_vecs(inp, b):
    f = lambda a: np.asarray(a, np.float32)
    parts = []
    sc = np.stack([_cols(f(inp['c'])[b]), _cols(f(inp['c_ctx']))], axis=2).reshape(128, 16)
    parts.append(sc)
    parts.append(_cols(f(inp['ada_b'])))
    parts.append(_cols(f(inp['norm_mix_g']))); parts.append(_cols(f(inp['norm_ffn_g'])))
    parts.append(_cols(f(inp['ev_b_in'])[0, :1024]))
    cw = f(inp['ev_conv_w'])[0]
    parts.append(cw.reshape(31, 4, 128).transpose(2, 1, 0).reshape(128, 124))
    parts.append(_cols(f(inp['ev_conv_b'])[0])); parts.append(_cols(f(inp['ev_conv_ln_g'])[0])); parts.append(_cols(f(inp['ev_conv_ln_b'])[0]))
    parts.append(_cols(f(inp['ev_b_out'])[0]))
    parts.append(_cols(f(inp['od_b_in'])[0, :1024]))
    parts.append(_cols(f(inp['od_b_out'])[0]))
    v = np.concatenate(parts, axis=1)
    assert v.shape == (128, NV), v.shape
    return np.ascontiguousarray(v)


_NC_CACHE = {}

def kernel(**inp):
    f = lambda a: np.ascontiguousarray(np.asarray(a, np.float32))
    key = (tuple(sorted(DEBUG.get('dbg', {}).items())), DEBUG.get('stop_after', 99), DEBUG.get('sub', 9), DEBUG.get('n_exp', 32), DEBUG.get('a1', 99))
    if key not in _NC_CACHE:
        _NC_CACHE[key] = build_program(DEBUG.get('dbg'))
    nc = _NC_CACHE[key]
    n = DEBUG.get('n_cores', 8)
    consts = _consts()
    x = f(inp['x']); ctx = f(inp['ctx'])
    rows0 = np.concatenate([f(inp['ev_b_in'])[0, 1024:], f(inp['ev_q_norm_g'])[0], f(inp['ev_k_norm_g'])[0], f(inp['ev_sink'])[0]])[None, :]
    rows1 = np.concatenate([f(inp['od_b_in'])[0, 1024:], f(inp['od_v_ln_g'])[0], f(inp['od_v_ln_b'])[0], f(inp['od_b_s'])[0].reshape(-1)])[None, :]
    rowsm = f(inp['moe_router_b']).reshape(1, 64)
    b1 = f(inp['moe_b1'])
    b1cols = np.ascontiguousarray(np.stack([_cols(b1[l]) for l in range(2)], axis=0))
    shared = dict(consts=consts, cossin=_cossin(), b1cols=b1cols, rows0=np.ascontiguousarray(rows0), rows1=np.ascontiguousarray(rows1), rowsm=rowsm,
                  ada_w=f(inp['ada_w']), ev_w_in=f(inp['ev_w_in'])[0], ev_w_out=f(inp['ev_w_out'])[0],
                  od_w_in=f(inp['od_w_in'])[0], od_w_s=f(inp['od_w_s'])[0], od_w_out=f(inp['od_w_out'])[0],
                  r_w=f(inp['moe_router_w']), w1=f(inp['moe_w1']), w2=f(inp['moe_w2']), b2=f(inp['moe_b2']))
    in_maps = []
    for b in range(n):
        m = dict(shared)
        m['xT'] = np.ascontiguousarray(x[b].T); m['ctxT'] = np.ascontiguousarray(ctx[b].T)
        m['vecs'] = _build_vecs(inp, b)
        in_maps.append(m)
    res = run_bass_kernel_spmd(nc, in_maps, core_ids=list(range(n)))
    DEBUG['last'] = res.results
    out = np.stack([np.ascontiguousarray(res.results[b]['outT'].T) for b in range(n)], axis=0)
    return out.astype(np.float32)
```
